# Optimizing a Trainium2 kernel written in Bass

```python
import math
import jax, jax.numpy as jnp
from jax import lax
import numpy as np

D_MODEL = 2048
BATCH = 4
SEQ = 4096
DEPTH = 1

POOL_WIDTH = D_MODEL // 2
POOL_GROUPS = 4
POOL_WINDOWS = (2, 4, 8, 16)
POOL_GROUP_DIM = POOL_WIDTH // POOL_GROUPS
GLA_V_WIDTH = D_MODEL - POOL_WIDTH
GLA_HEADS = 4
GLA_HEAD_V = GLA_V_WIDTH // GLA_HEADS
GLA_K_WIDTH = GLA_V_WIDTH // 2
GLA_HEAD_K = GLA_K_WIDTH // GLA_HEADS
GLA_GATE_RANK = 16
GLA_GATE_TAU = 16.0
GLA_CHUNK = 64
GLA_NORM_EPS = 1e-6
SPLITS = (POOL_WIDTH,
          POOL_WIDTH + GLA_K_WIDTH,
          POOL_WIDTH + 2 * GLA_K_WIDTH,
          POOL_WIDTH + 2 * GLA_K_WIDTH + GLA_V_WIDTH,
          POOL_WIDTH + 2 * GLA_K_WIDTH + GLA_V_WIDTH + GLA_GATE_RANK)
IN_WIDTH = SPLITS[-1] + GLA_V_WIDTH
N_EXPERTS = 32
TOP_K = 4
D_FF = D_MODEL
SWIGLU_ALPHA = 1.702
SWIGLU_LIMIT = 7.0
MOE_BLOCK = 128
DEEPNORM_ALPHA = (2.0 * DEPTH) ** 0.25
DEEPNORM_BETA = (8.0 * DEPTH) ** -0.25
LN_EPS = 1e-5

kernel_name = "hymba_pool_gla_moe_adaln_deepnorm"


def _layer_norm(x):
    xf = x.astype(jnp.float32)
    mu = jnp.mean(xf, axis=-1, keepdims=True)
    var = jnp.mean(jnp.square(xf - mu), axis=-1, keepdims=True)
    return (xf - mu) * lax.rsqrt(var + LN_EPS)


def _post_ln(y, g, b, dtype):
    return (_layer_norm(y) * g + b).astype(dtype)


def _modulate(x, shift, scale):
    return (_layer_norm(x) * (1.0 + scale) + shift).astype(x.dtype)


def _pool_mixer(u, w_pool, b_pool, pool_scale):
    bsz, s, _ = u.shape
    uf = u.astype(jnp.float32).reshape(bsz, s, POOL_GROUPS, POOL_GROUP_DIM)
    cs = jnp.cumsum(uf, axis=1)
    pos = jnp.arange(1, s + 1)
    outs = []
    for g, w in enumerate(POOL_WINDOWS):
        csg = cs[:, :, g]
        lagged = jnp.pad(csg[:, : s - w], ((0, 0), (w, 0), (0, 0)))
        count = jnp.minimum(pos, w).astype(jnp.float32)[None, :, None]
        outs.append((csg - lagged) / count - uf[:, :, g])
    pooled = jnp.stack(outs, axis=2)
    mixed = jnp.einsum('bsgc,gcd->bsgd', pooled, w_pool.astype(jnp.float32))
    mixed = mixed.reshape(bsz, s, POOL_WIDTH) + b_pool
    return (mixed * pool_scale).astype(u.dtype)


def _gla_mixer(q, k, v, gk_lowrank, g, w_gk, b_gk, norm_w):
    bsz, s, _ = q.shape
    n_chunks = s // GLA_CHUNK
    f32 = jnp.float32

    def to_chunks(a, dh):
        a = a.astype(f32).reshape(bsz, n_chunks, GLA_CHUNK, GLA_HEADS, dh)
        return a.transpose(1, 0, 3, 2, 4)

    log_alpha = jax.nn.log_sigmoid(gk_lowrank.astype(f32) @ w_gk.astype(f32) + b_gk) / GLA_GATE_TAU
    qc = to_chunks(q, GLA_HEAD_K) * (GLA_HEAD_K ** -0.5)
    kc = to_chunks(k, GLA_HEAD_K)
    vc = to_chunks(v, GLA_HEAD_V)
    ac = to_chunks(log_alpha, GLA_HEAD_K)
    causal = jnp.tril(jnp.ones((GLA_CHUNK, GLA_CHUNK), dtype=bool))[None, None, :, :, None]

    def chunk_step(state, inp):
        qb, kb, vb, ab = inp
        bcum = jnp.cumsum(ab, axis=2)
        blast = bcum[:, :, -1:, :]
        o_inter = jnp.einsum('bhtd,bhdv->bhtv', qb * jnp.exp(bcum), state)
        rel = jnp.where(causal, bcum[:, :, :, None, :] - bcum[:, :, None, :, :], -jnp.inf)
        scores = jnp.einsum('bhtd,bhsd,bhtsd->bhts', qb, kb, jnp.exp(rel))
        o_intra = jnp.einsum('bhts,bhsv->bhtv', scores, vb)
        new_state = jnp.exp(blast[:, :, 0, :])[..., None] * state + jnp.einsum(
            'bhsd,bhsv->bhdv', kb * jnp.exp(blast - bcum), vb)
        return new_state, o_inter + o_intra

    state0 = jnp.zeros((bsz, GLA_HEADS, GLA_HEAD_K, GLA_HEAD_V), f32)
    _, o = lax.scan(chunk_step, state0, (qc, kc, vc, ac))
    o = o.transpose(1, 0, 3, 2, 4).reshape(bsz, s, GLA_HEADS, GLA_HEAD_V)
    o = o * lax.rsqrt(jnp.mean(o * o, axis=-1, keepdims=True) + GLA_NORM_EPS) * norm_w
    gate = jax.nn.silu(g.astype(f32)).reshape(bsz, s, GLA_HEADS, GLA_HEAD_V)
    return (o * gate).reshape(bsz, s, GLA_V_WIDTH).astype(q.dtype)


def _mixer(h, w_in, w_gk, b_gk, w_pool, b_pool, pool_scale, gla_norm_w, w_out):
    proj = h @ w_in
    u, q, k, v, gk_lr, g = jnp.split(proj, list(SPLITS), axis=-1)
    y_pool = _pool_mixer(u, w_pool, b_pool, pool_scale)
    y_gla = _gla_mixer(q, k, v, gk_lr, g, w_gk, b_gk, gla_norm_w)
    return jnp.concatenate([y_pool, y_gla], axis=-1) @ w_out


def _moe(h, w_router, b_router, w_gate, b_gate, w_up, b_up, w_down, b_down):
    bsz, s, d = h.shape
    n_tok = bsz * s
    xf = h.reshape(n_tok, d)
    logits = (xf @ w_router + b_router).astype(jnp.float32)
    top_vals, top_idx = lax.top_k(logits, TOP_K)
    top_w = jax.nn.softmax(top_vals, axis=-1)
    n_assign = n_tok * TOP_K
    flat_e = top_idx.reshape(-1)
    flat_tok = jnp.repeat(jnp.arange(n_tok, dtype=jnp.int32), TOP_K)
    flat_w = top_w.reshape(-1)
    order = jnp.argsort(flat_e)
    sorted_e = flat_e[order]
    sorted_tok = flat_tok[order]
    sorted_w = flat_w[order]
    counts = jnp.bincount(flat_e, length=N_EXPERTS)
    padded = (counts + MOE_BLOCK - 1) // MOE_BLOCK * MOE_BLOCK
    group_start = jnp.cumsum(counts) - counts
    pad_end = jnp.cumsum(padded)
    pad_start = pad_end - padded
    rank = jnp.arange(n_assign, dtype=jnp.int32) - group_start[sorted_e]
    dest = pad_start[sorted_e] + rank
    n_slots = (n_assign + MOE_BLOCK - 1) // MOE_BLOCK * MOE_BLOCK + N_EXPERTS * MOE_BLOCK
    n_blocks = n_slots // MOE_BLOCK
    slot_tok = jnp.zeros((n_slots,), jnp.int32).at[dest].set(sorted_tok)
    slot_w = jnp.zeros((n_slots,), jnp.float32).at[dest].set(sorted_w)
    block_e = jnp.minimum(
        jnp.searchsorted(pad_end, jnp.arange(n_blocks) * MOE_BLOCK, side='right'), N_EXPERTS - 1)

    def expert_block(args):
        tok, e = args
        xb = xf[tok]
        gate = jnp.minimum(xb @ w_gate[e] + b_gate[e], SWIGLU_LIMIT)
        up = jnp.clip(xb @ w_up[e] + b_up[e], -SWIGLU_LIMIT, SWIGLU_LIMIT)
        act = gate * jax.nn.sigmoid(SWIGLU_ALPHA * gate) * (up + 1.0)
        return act @ w_down[e] + b_down[e]

    y = lax.map(expert_block, (slot_tok.reshape(n_blocks, MOE_BLOCK), block_e))
    y = y.reshape(n_slots, d) * slot_w[:, None].astype(y.dtype)
    out = jnp.zeros((n_tok, d), y.dtype).at[slot_tok].add(y)
    return out.reshape(bsz, s, d)


def setup_inputs(seed: int = 0) -> dict:
    key = jax.random.key(seed)
    ks = jax.random.split(key, 24)
    L, D, E, F = DEPTH, D_MODEL, N_EXPERTS, D_FF

    def nrm(k, shape, scale):
        return jax.random.normal(k, shape, jnp.float32) * scale

    col_scale = jnp.concatenate([
        jnp.ones((SPLITS[2],), jnp.float32),
        jnp.full((GLA_V_WIDTH,), DEEPNORM_BETA, jnp.float32),
        jnp.ones((IN_WIDTH - SPLITS[3],), jnp.float32)])
    return {
        "x": nrm(ks[0], (BATCH, SEQ, D), 1.0),
        "c": nrm(ks[1], (BATCH, D), 1.0),
        "w_ada": nrm(ks[2], (L, D, 6 * D), 0.5 * D ** -0.5),
        "b_ada": nrm(ks[3], (L, 6 * D), 0.02),
        "w_in": nrm(ks[4], (L, D, IN_WIDTH), D ** -0.5) * col_scale,
        "w_gk": nrm(ks[5], (L, GLA_GATE_RANK, GLA_K_WIDTH), GLA_GATE_RANK ** -0.5),
        "b_gk": nrm(ks[6], (L, GLA_K_WIDTH), 0.1),
        "w_pool": nrm(ks[7], (L, POOL_GROUPS, POOL_GROUP_DIM, POOL_GROUP_DIM), POOL_GROUP_DIM ** -0.5),
        "b_pool": nrm(ks[8], (L, POOL_WIDTH), 0.02),
        "pool_scale": 1.0 + nrm(ks[9], (L, POOL_WIDTH), 0.05),
        "gla_norm_w": 1.0 + nrm(ks[10], (L, GLA_HEAD_V), 0.05),
        "w_out": nrm(ks[11], (L, D, D), DEEPNORM_BETA * D ** -0.5),
        "ln1_g": 1.0 + nrm(ks[12], (L, D), 0.05),
        "ln1_b": nrm(ks[13], (L, D), 0.02),
        "w_router": nrm(ks[14], (L, D, E), D ** -0.5),
        "b_router": nrm(ks[15], (L, E), 0.01),
        "w_gate": nrm(ks[16], (L, E, D, F), D ** -0.5),
        "b_gate": nrm(ks[17], (L, E, F), 0.02),
        "w_up": nrm(ks[18], (L, E, D, F), D ** -0.5),
        "b_up": nrm(ks[19], (L, E, F), 0.02),
        "w_down": nrm(ks[20], (L, E, F, D), DEEPNORM_BETA * F ** -0.5),
        "b_down": nrm(ks[21], (L, E, D), 0.02),
        "ln2_g": 1.0 + nrm(ks[22], (L, D), 0.05),
        "ln2_b": nrm(ks[23], (L, D), 0.02),
    }


def reference(x, c, w_ada, b_ada, w_in, w_gk, b_gk, w_pool, b_pool, pool_scale, gla_norm_w, w_out,
              ln1_g, ln1_b, w_router, b_router, w_gate, b_gate, w_up, b_up, w_down, b_down,
              ln2_g, ln2_b):
    for l in range(DEPTH):
        mod = jax.nn.silu(c) @ w_ada[l] + b_ada[l]
        sh1, sc1, g1, sh2, sc2, g2 = jnp.split(mod[:, None, :], 6, axis=-1)
        h = _modulate(x, sh1, sc1)
        y = _mixer(h, w_in[l], w_gk[l], b_gk[l], w_pool[l], b_pool[l], pool_scale[l],
                   gla_norm_w[l], w_out[l])
        x = _post_ln(DEEPNORM_ALPHA * x + g1 * y, ln1_g[l], ln1_b[l], x.dtype)
        h = _modulate(x, sh2, sc2)
        y = _moe(h, w_router[l], b_router[l], w_gate[l], b_gate[l], w_up[l], b_up[l],
                 w_down[l], b_down[l])
        x = _post_ln(DEEPNORM_ALPHA * x + g2 * y, ln2_g[l], ln2_b[l], x.dtype)
    return x
```

```python
from contextlib import ExitStack
import numpy as np
import concourse.bass as bass
import concourse.mybir as mybir
from concourse.bass_utils import run_bass_kernel_spmd

F32 = mybir.dt.float32
BF16 = mybir.dt.bfloat16
I32 = mybir.dt.int32
U32 = mybir.dt.uint32
AF = mybir.ActivationFunctionType
ALU = mybir.AluOpType
AX = mybir.AxisListType

D = 2048
T = 2048
SEG = 512
NSEG = T // SEG
NE = 32
CAP = 2048
GRP = 512
ALPHA = 2.0 ** 0.25
LN_EPS = 1e-5
IN_W = 4112


class _Stop(Exception):
    pass


class Sched:
    def __init__(self, nc, stack):
        self.nc = nc
        self.eng = {"pe": nc.tensor, "act": nc.scalar, "dve": nc.vector,
                    "pool": nc.gpsimd, "sp": nc.sync}
        self.sem = {}
        self.cnt = {}
        self.stack = stack
        for e in self.eng:
            self.sem[e] = stack.enter_context(nc.semaphore("prog_" + e))
            self.cnt[e] = 0
        self.dsem = {}
        self.dcnt = {}
        self.dq = {}
        self.seen = {e: {} for e in self.eng}
        self.lastw = {}
        self.readers = {}
        self.n_wait = 0
        self.n_inst = 0

    def _chan(self, key):
        if key not in self.dsem:
            self.dsem[key] = self.stack.enter_context(
                self.nc.semaphore("d_" + str(len(self.dsem))))
            self.dcnt[key] = 0
        return self.dsem[key]

    def _wait(self, e, tok):
        kind, k, c = tok
        semkey = (kind, k)
        if self.seen[e].get(semkey, 0) >= c:
            return
        sem = self.sem[k] if kind == "eng" else self.dsem[k]
        self.eng[e].wait_ge(sem, c)
        self.seen[e][semkey] = c
        self.n_wait += 1

    def _deps(self, e, reads, writes, skip_same=False):
        toks = []
        for r in reads:
            w = self.lastw.get(r)
            if w is not None:
                toks.append(w)
        for w_ in writes:
            w = self.lastw.get(w_)
            if w is not None:
                toks.append(w)
            toks.extend(self.readers.get(w_, []))
        for t in toks:
            if skip_same and t[0] == "eng" and t[1] == e:
                continue
            self._wait(e, t)

    def _record(self, tok, reads, writes):
        for r in reads:
            self.readers.setdefault(r, []).append(tok)
        for w in writes:
            self.lastw[w] = tok
            self.readers[w] = []

    def op(self, e, fn, reads=(), writes=()):
        self._deps(e, reads, writes, skip_same=(e == "pe"))
        ins = fn(self.eng[e])
        self.cnt[e] += 1
        ins.then_inc(self.sem[e], 1)
        self._record(("eng", e, self.cnt[e]), reads, writes)
        self.n_inst += 1
        return ins

    def dma(self, q, out, in_, chan, reads=(), writes=(), indirect=None, **kw):
        self._deps(q, reads, writes)
        sem = self._chan(chan)
        if indirect is None:
            ins = self.eng[q].dma_start(out=out, in_=in_, **kw)
        else:
            ins = self.eng[q].indirect_dma_start(out=out, in_=in_, **indirect)
        self.dcnt[chan] += 16
        self.dq[chan] = q
        ins.then_inc(sem, 16)
        self._record(("dma", chan, self.dcnt[chan]), reads, writes)
        self.n_inst += 1
        return ins

    def barrier(self):
        for e in self.eng:
            for o in self.eng:
                if self.cnt[o] > 0:
                    self._wait(e, ("eng", o, self.cnt[o]))
            for k, c in self.dcnt.items():
                if c > 0:
                    self._wait(e, ("dma", k, c))

    def finish(self, e="sp"):
        for k, w in list(self.lastw.items()):
            if w is not None:
                self._wait(e, w)
            for r in self.readers.get(k, []):
                self._wait(e, r)


def build_program(debug=False, n_groups=4, stop_after=None):
    nc = bass.Bass("TRN2", target_bir_lowering=False)

    in_names = []
    lean = stop_after in ("A0", "A1", "A1s", "A1a", "A1b", "A1c", "A1d", "A2")

    def din(name, shape, dt=F32):
        if lean and name in ("w_gate", "w_up", "w_down"):
            return None
        in_names.append(name)
        return nc.dram_tensor(name, list(shape), dt, kind="ExternalInput").ap()

    x_own = din("x_own", [T, D])
    x_pre = din("x_pre", [T, D])
    c_l = din("c_l", [128, 16])
    flag = din("flag", [128, 1])
    invcnt = din("invcnt", [128, 4, 16])
    ident_f = din("ident_f", [128, 128])
    triu2 = din("triu2", [128, 128])
    rmask = din("rmask", [128, SEG])
    ltri = din("ltri", [128, 128])
    iota_e = din("iota_e", [128, NE])
    tokid = din("tokid", [128, 16], I32)
    iota_n = din("iota_n", [128, NE])
    list_init = din("list_init", [NE * CAP, 2], I32)
    w_ada = din("w_ada", [D, 6 * D])
    b_ada = din("b_ada", [1, 6 * D])
    w_in = din("w_in", [D, IN_W])
    w_gk = din("w_gk", [16, 512])
    b_gk = din("b_gk", [128, 4])
    w_pool = din("w_pool", [4, 256, 256])
    b_pool = din("b_pool", [128, 8])
    pool_scale = din("pool_scale", [128, 8])
    gla_norm_w = din("gla_norm_w", [1, 256])
    w_out = din("w_out", [D, D])
    ln1_g = din("ln1_g", [1, D])
    ln1_b = din("ln1_b", [1, D])
    w_router = din("w_router", [D, NE])
    b_router = din("b_router", [1, NE])
    w_gate = din("w_gate", [NE, D, D])
    b_gate = din("b_gate", [NE, 128, 16])
    w_up = din("w_up", [NE, D, D])
    b_up = din("b_up", [NE, 128, 16])
    w_down = din("w_down", [NE, D, D])
    b_down = din("b_down", [NE, D])
    ln2_g = din("ln2_g", [1, D])
    ln2_b = din("ln2_b", [1, D])

    out = nc.dram_tensor("out", [T, D], F32, kind="ExternalOutput").ap()

    def dscratch(name, shape, dt):
        return nc.dram_tensor(name, list(shape), dt, kind="Internal").ap()

    ybufT = dscratch("ybufT", [16, 128, T], BF16)
    x1buf = dscratch("x1buf", [T, D], F32)
    h2buf = dscratch("h2buf", [T + 128, D], BF16)
    lists = dscratch("lists", [NE * CAP, 2], I32)
    yacc = dscratch("yacc", [T + 128, D], F32)
    moddram = dscratch("moddram", [1, 6 * D], F32)
    dbg = None
    if debug:
        dbg = {
            "x1": nc.dram_tensor("dbg_x1", [T, D], F32, kind="ExternalOutput").ap(),
            "yT": nc.dram_tensor("dbg_yT", [16, 128, T], BF16, kind="ExternalOutput").ap(),
            "lg": nc.dram_tensor("dbg_lg", [T, NE], F32, kind="ExternalOutput").ap(),
        }

    nc.in_names = in_names
    with ExitStack() as st:
        S = Sched(nc, st)

        def sb(name, shape, dt=F32):
            return st.enter_context(nc.sbuf_tensor(name, list(shape), dt))

        def pst(name, shape, dt=F32):
            return st.enter_context(nc.psum_tensor(name, list(shape), dt))

        ident32 = sb("ident32", [128, 128])
        identb = sb("identb", [128, 128], BF16)
        triu_sb = sb("triu_sb", [128, 128])
        rmask_sb = sb("rmask_sb", [128, SEG])
        flag_sb = sb("flag_sb", [128, 1])
        invc_sb = sb("invc_sb", [128, 4, 16])
        ones_row = sb("ones_row", [1, 128])
        one11 = sb("one11", [1, 1])
        eps_sb = sb("eps_sb", [128, 1])
        S.dma("sp", ident32[:], ident_f, chan="ident32", writes=["ident32"])
        S.dma("pool", identb[:], ident_f, chan="identb", writes=["identb"])
        S.dma("sp", triu_sb[:], triu2, chan="triu", writes=["triu"])
        S.dma("sp", rmask_sb[:], rmask, chan="rmask", writes=["rmask"])
        S.dma("sp", flag_sb[:], flag, chan="flag", writes=["flag"])
        S.dma("sp", invc_sb[:], invcnt, chan="invc", writes=["invc"])
        S.op("dve", lambda e: e.memset(ones_row[:], 1.0), writes=["ones_row"])
        S.op("dve", lambda e: e.memset(one11[:], 1.0), writes=["one11"])
        S.op("dve", lambda e: e.memset(eps_sb[:], LN_EPS), writes=["eps"])

        PG = [pst("pg%d" % i, [128, 512]) for i in range(2)]
        PTS = [pst("ptr%d" % i, [128, 1024], BF16) for i in range(2)]
        PSC = pst("psc", [128, 512])
        PO = [pst("po%d" % i, [128, 512]) for i in range(2)]
        PS_ = pst("pss", [128, 512])
        pg_i = [0]

        def next_pg():
            i = pg_i[0] % 2
            pg_i[0] += 1
            return PG[i], "pg%d" % i

        flg_i = sb("flg_i", [1, 4 * NE], I32)
        cur = [st]

        def sb(name, shape, dt=F32):
            return cur[0].enter_context(nc.sbuf_tensor(name, list(shape), dt))

        ph = ExitStack()
        cur[0] = ph
        c_sb = sb("c_sb", [128, 16])
        sc_sb = sb("sc_sb", [128, 16])
        S.dma("sp", c_sb[:], c_l, chan="c_sb", writes=["c_sb"])
        S.op("act", lambda e: e.activation(out=sc_sb[:], in_=c_sb[:], func=AF.Silu),
             reads=["c_sb"], writes=["sc_sb"])
        WF = [sb("wf%d" % i, [128, 16, 512]) for i in range(2)]
        brow = [sb("brow%d" % i, [1, 512]) for i in range(2)]
        mrow = [sb("mrow%d" % i, [1, 512]) for i in range(2)]
        for j in range(24):
            i = j % 2
            wk = "wf%d" % i
            S.dma("sp", WF[i][:], w_ada[:, j * 512:(j + 1) * 512].rearrange("(kc p) n -> p kc n", p=128),
                  chan=wk, writes=[wk])
            S.dma("sp", brow[i][:], b_ada[0:1, j * 512:(j + 1) * 512], chan="brow%d" % i, writes=["brow%d" % i])
            ps, pk = next_pg()
            for kc in range(16):
                S.op("pe", lambda e, kc=kc, i=i, ps=ps: e.matmul(ps[0:1, :], lhsT=sc_sb[:, kc:kc + 1], rhs=WF[i][:, kc, :],
                                                                 start=(kc == 0), stop=(kc == 15)),
                     reads=[wk, "sc_sb"], writes=[pk])
            S.op("dve", lambda e, i=i, ps=ps: e.tensor_tensor(out=mrow[i][:], in0=ps[0:1, :], in1=brow[i][:], op=ALU.add),
                 reads=[pk, "brow%d" % i], writes=["mrow%d" % i])
            S.dma("sp", moddram[0:1, j * 512:(j + 1) * 512], mrow[i][:], chan="mrow%d" % i, reads=["mrow%d" % i],
                  writes=["moddram"])
        S.barrier()
        ph.close()
        if stop_after == "A0":
            print("sems", len(S.dsem), "inst", S.n_inst, "waits", S.n_wait)
            return nc

        ph = ExitStack()
        cur[0] = ph
        sh1_fm = sb("sh1_fm", [128, 16])
        sc1p_fm = sb("sc1p_fm", [128, 16])
        with nc.allow_non_contiguous_dma(reason="tiny strided vector load"):
            S.dma("sp", sh1_fm[:], moddram[0, 0:D].rearrange("(kc p) -> p kc", p=128), chan="sh1_fm",
                  reads=["moddram"], writes=["sh1_fm"])
            S.dma("sp", sc1p_fm[:], moddram[0, D:2 * D].rearrange("(kc p) -> p kc", p=128), chan="sc1p_fm",
                  reads=["moddram"], writes=["sc1p_0"])
        S.op("dve", lambda e: e.tensor_scalar(out=sc1p_fm[:], in0=sc1p_fm[:], scalar1=1.0, scalar2=None, op0=ALU.add),
             reads=["sc1p_0"], writes=["sc1p_fm"])
        wgkin = sb("wgkin", [128, 16, 16], BF16)
        S.dma("pool", wgkin[:], w_in[:, 3072:3088].rearrange("(kc p) n -> p kc n", p=128),
              chan="wgkin", writes=["wgkin"])
        wgk_sb = sb("wgk_sb", [16, 512])
        S.dma("sp", wgk_sb[:], w_gk, chan="wgk", writes=["wgk"])
        nbgk = sb("nbgk", [128, 4])
        S.dma("sp", nbgk[:], b_gk, chan="nbgk", writes=["nbgk0"])
        S.op("dve", lambda e: e.tensor_scalar(out=nbgk[:], in0=nbgk[:], scalar1=-1.0, scalar2=None, op0=ALU.mult),
             reads=["nbgk0"], writes=["nbgk"])
        wpool_sb = sb("wpool_sb", [128, 4, 2, 256], BF16)
        S.dma("pool", wpool_sb[:], w_pool.rearrange("g (cc p) d -> p g cc d", p=128),
              chan="wpool", writes=["wpool"])
        bpool_sb = sb("bpool_sb", [128, 8])
        pscale_sb = sb("pscale_sb", [128, 8])
        S.dma("sp", bpool_sb[:], b_pool, chan="bpool", writes=["bpool"])
        S.dma("sp", pscale_sb[:], pool_scale, chan="pscale", writes=["pscale"])
        nw_row = sb("nw_row", [1, 256])
        S.dma("sp", nw_row[:], gla_norm_w, chan="nw_row", writes=["nw_row"])
        nw_bc = sb("nw_bc", [128, 256])
        ps, pk = next_pg()
        S.op("pe", lambda e: e.matmul(ps[:, 0:256], lhsT=ones_row[0:1, :], rhs=nw_row[0:1, :], start=True, stop=True),
             reads=["nw_row", "ones_row"], writes=[pk])
        S.op("act", lambda e: e.activation(out=nw_bc[:], in_=ps[:, 0:256], func=AF.Copy), reads=[pk], writes=["nw_bc"])
        lnq_sb = sb("lnq_sb", [128, 1])
        S.op("dve", lambda e: e.memset(lnq_sb[:], float(np.log(128.0 ** -0.5))), writes=["lnq"])
        rms_eps = sb("rms_eps", [128, 1])
        S.op("dve", lambda e: e.memset(rms_eps[:], 1e-6), writes=["rms_eps"])

        if stop_after == "A1s":
            S.barrier()
            ph.close()
            return nc
        WB = [sb("wb%d" % i, [128, 16, 512], BF16) for i in range(3)]
        wb_i = [0]

        def load_w(src_ap):
            i = wb_i[0] % 3
            wb_i[0] += 1
            k = "wb%d" % i
            for h in range(2):
                S.dma("pool", WB[i][:, h * 8:(h + 1) * 8, :],
                      src_ap[h * 1024:(h + 1) * 1024, :].rearrange("(kc p) n -> p kc n", p=128),
                      chan=k, writes=[k])
            return WB[i], k

        XT = [sb("xt%d" % i, [128, D]) for i in range(2)]
        xt_i = [0]
        xn = sb("xn", [128, D], BF16)
        stats = sb("stats", [128, 4, 6])
        mv = sb("mv", [128, 2])
        rstd = sb("rstd", [128, 1])
        hT = sb("hT", [128, 16, SEG], BF16)
        uT = sb("uT", [128, 8, 16 + SEG])
        sA = sb("sA", [128, 2, 16 + SEG])
        sB = sb("sB", [128, 2, 16 + SEG])
        pooled = sb("pooled", [128, 2, SEG], BF16)
        fixs = sb("fixs", [128, 2, 16])
        qtil = sb("qtil", [128, 4, SEG], BF16)
        ktil = sb("ktil", [128, 4, SEG], BF16)
        khatT = sb("khatT", [128, 4, SEG], BF16)
        khat_tm = sb("khat_tm", [128, 4, 4, 128], BF16)
        v_tm = sb("v_tm", [128, 4, 1024], BF16)
        sg_tm = sb("sg_tm", [128, 4, 1024], BF16)
        gkT = sb("gkT", [16, SEG])
        spl = sb("spl", [128, SEG])
        cs = sb("cs", [128, 4, SEG])
        eq = sb("eq", [128, SEG])
        enb = sb("enb", [128, SEG])
        ehat = sb("ehat", [128, SEG])
        eL = sb("eL", [128, 4, 8])
        state32 = sb("state32", [128, 4, 256])
        stateb = sb("stateb", [128, 4, 256], BF16)
        scm = sb("scm", [128, 2, 128], BF16)
        yB = sb("yB", [128, 1024], BF16)
        nwsg = sb("nwsg", [128, 256])
        ssq = sb("ssq", [128, 1])
        rr = sb("rr", [128, 1])
        osq = sb("osq", [128, 256])
        yT = sb("yT", [128, 16, SEG], BF16)
        S.op("pool", lambda e: e.memset(state32[:], 0.0), writes=[("state32", h) for h in range(4)])
        S.op("pool", lambda e: e.memset(stateb[:], 0.0), writes=[("stateb", h) for h in range(4)])
        S.op("pool", lambda e: e.memset(uT[:], 0.0), writes=[("uT", ch) for ch in range(8)])
        S.op("pool", lambda e: e.memset(sA[:], 0.0), writes=["sA"])
        S.op("pool", lambda e: e.memset(sB[:], 0.0), writes=["sB"])

        def layer_norm_stats(xt, xk):
            for j in range(4):
                S.op("dve", lambda e, j=j: e.bn_stats(out=stats[:, j, :], in_=xt[:, j * 512:(j + 1) * 512]),
                     reads=[xk], writes=[("stats", j)])
            S.op("dve", lambda e: e.bn_aggr(out=mv[:], in_=stats[:].rearrange("p a b -> p (a b)")),
                 reads=[("stats", j) for j in range(4)], writes=["mv"])
            S.op("act", lambda e: e.activation(out=rstd[:], in_=mv[:, 1:2], func=AF.Sqrt, bias=eps_sb[:, 0:1], scale=1.0),
                 reads=["mv", "eps"], writes=["rstd0"])
            S.op("dve", lambda e: e.reciprocal(out=rstd[:], in_=rstd[:]), reads=["rstd0"], writes=["rstd"])

        def mixer_segment(xsrc, seg, own, need_u):
            for tt in range(4):
                i = xt_i[0] % 2
                xt_i[0] += 1
                xk = "xt%d" % i
                r0 = seg * SEG + tt * 128
                S.dma("sp", XT[i][:], xsrc[r0:r0 + 128, :], chan=xk, writes=[xk])
                layer_norm_stats(XT[i], xk)
                S.op("dve", lambda e, i=i: e.tensor_scalar(out=xn[:], in0=XT[i][:], scalar1=mv[:, 0:1], scalar2=rstd[:, 0:1],
                                                           op0=ALU.subtract, op1=ALU.mult),
                     reads=[xk, "mv", "rstd"], writes=["xn"])
                for half in range(2):
                    PT = PTS[half]
                    ptk = "pt%d" % half
                    for j in range(8):
                        kc = half * 8 + j
                        S.op("pe", lambda e, kc=kc, j=j, PT=PT: e.transpose(PT[:, j * 128:(j + 1) * 128], xn[:, kc * 128:(kc + 1) * 128], identb[:]),
                             reads=["xn", "identb"], writes=[ptk])
                    for j in range(8):
                        kc = half * 8 + j
                        eng = "act" if j % 2 == 0 else "dve"
                        if eng == "act":
                            S.op("act", lambda e, kc=kc, j=j, tt=tt, PT=PT: e.activation(
                                out=hT[:, kc, tt * 128:(tt + 1) * 128], in_=PT[:, j * 128:(j + 1) * 128], func=AF.Identity,
                                bias=sh1_fm[:, kc:kc + 1], scale=sc1p_fm[:, kc:kc + 1]),
                                reads=[ptk, "sh1_fm", "sc1p_fm"], writes=[("hT", tt)])
                        else:
                            S.op("dve", lambda e, kc=kc, j=j, tt=tt, PT=PT: e.tensor_scalar(
                                out=hT[:, kc, tt * 128:(tt + 1) * 128], in0=PT[:, j * 128:(j + 1) * 128],
                                scalar1=sc1p_fm[:, kc:kc + 1], scalar2=sh1_fm[:, kc:kc + 1], op0=ALU.mult, op1=ALU.add),
                                reads=[ptk, "sh1_fm", "sc1p_fm"], writes=[("hT", tt)])
            hkeys = [("hT", tt) for tt in range(4)]
            if stop_after == "A1a":
                raise _Stop()

            def fm_group(wt, wk, m, evac):
                ps, pk = next_pg()
                for kc in range(16):
                    S.op("pe", lambda e, kc=kc: e.matmul(ps[:, :], lhsT=wt[:, kc, m * 128:(m + 1) * 128], rhs=hT[:, kc, :],
                                                         start=(kc == 0), stop=(kc == 15)),
                         reads=[wk] + hkeys, writes=[pk])
                evac(ps, pk)

            def tm_group(wt, wk, tt, evac):
                ps, pk = next_pg()
                for kc in range(16):
                    S.op("pe", lambda e, kc=kc: e.matmul(ps[:, :], lhsT=hT[:, kc, tt * 128:(tt + 1) * 128], rhs=wt[:, kc, :],
                                                         start=(kc == 0), stop=(kc == 15)),
                         reads=[wk, ("hT", tt)], writes=[pk])
                evac(ps, pk)

            ps, pk = next_pg()
            for kc in range(16):
                S.op("pe", lambda e, kc=kc: e.matmul(ps[0:16, :], lhsT=wgkin[:, kc, :], rhs=hT[:, kc, :],
                                                     start=(kc == 0), stop=(kc == 15)),
                     reads=["wgkin"] + hkeys, writes=[pk])
            S.op("act", lambda e: e.activation(out=gkT[:], in_=ps[0:16, :], func=AF.Copy), reads=[pk], writes=["gkT"])
            for h in range(4):
                ps, pk = next_pg()
                S.op("pe", lambda e, h=h: e.matmul(ps[:, :], lhsT=wgk_sb[:, h * 128:(h + 1) * 128], rhs=gkT[:, :],
                                                   start=True, stop=True), reads=["wgk", "gkT"], writes=[pk])
                S.op("act", lambda e, h=h: e.activation(out=spl[:], in_=ps[:, :], func=AF.Exp, bias=nbgk[:, h:h + 1], scale=-1.0),
                     reads=[pk, "nbgk"], writes=["spl"])
                S.op("act", lambda e: e.activation(out=spl[:], in_=spl[:], func=AF.Ln, bias=1.0, scale=1.0),
                     reads=["spl"], writes=["spl"])
                S.op("dve", lambda e, h=h: e.tensor_tensor_scan(out=cs[:, h, :], data0=rmask_sb[:], data1=spl[:], initial=0.0,
                                                                op0=ALU.mult, op1=ALU.add),
                     reads=["spl", "rmask"], writes=[("cs", h)])
            if stop_after == "A1b":
                raise _Stop()
            wt, wk = load_w(w_in[:, 1536:2048])
            for h in range(4):
                S.op("act", lambda e, h=h: e.activation(out=enb[:], in_=cs[:, h, :], func=AF.Exp, scale=1.0 / 16.0),
                     reads=[("cs", h)], writes=["enb"])
                S.op("act", lambda e, h=h: e.activation(
                    out=eL[:, h, :], in_=cs[:, h, :].rearrange("p (c t) -> p c t", t=64)[:, :, 63], func=AF.Exp, scale=-1.0 / 16.0),
                    reads=[("cs", h)], writes=[("eL", h)])
                S.op("dve", lambda e, h=h: e.tensor_tensor(
                    out=ehat[:].rearrange("p (c t) -> p c t", t=64), in0=enb[:].rearrange("p (c t) -> p c t", t=64),
                    in1=eL[:, h, :].unsqueeze(2).broadcast_to([128, 8, 64]), op=ALU.mult),
                    reads=["enb", ("eL", h)], writes=["ehat"])

                def evac_k(ps, pk, h=h):
                    if own:
                        S.op("dve", lambda e: e.tensor_tensor(out=ktil[:, h, :], in0=ps[:, :], in1=enb[:], op=ALU.mult),
                             reads=[pk, "enb"], writes=[("ktil", h)])
                    S.op("dve", lambda e: e.tensor_tensor(out=khatT[:, h, :], in0=ps[:, :], in1=ehat[:], op=ALU.mult),
                         reads=[pk, "ehat"], writes=[("khatT", h)])
                fm_group(wt, wk, h, evac_k)
            if stop_after == "A1c":
                raise _Stop()
            for tt in range(4):
                PT = PTS[tt % 2]
                ptk = "pt%d" % (tt % 2)
                for h in range(4):
                    S.op("pe", lambda e, tt=tt, h=h, PT=PT: e.transpose(PT[:, h * 128:(h + 1) * 128], khatT[:, h, tt * 128:(tt + 1) * 128], identb[:]),
                         reads=[("khatT", h), "identb"], writes=[ptk])
                S.op("act", lambda e, tt=tt, PT=PT: e.activation(out=khat_tm[:, tt, :, :], in_=PT[:, 0:512].rearrange("p (h d) -> p h d", h=4),
                                                          func=AF.Copy),
                     reads=[ptk], writes=[("khat_tm", tt)])
            for half in range(2):
                wt, wk = load_w(w_in[:, 2048 + half * 512: 2048 + (half + 1) * 512])
                for tt in range(4):
                    def evac_v(ps, pk, tt=tt, half=half):
                        S.op("act", lambda e: e.activation(out=v_tm[:, tt, half * 512:(half + 1) * 512], in_=ps[:, :], func=AF.Copy),
                             reads=[pk], writes=[("v_tm", tt)])
                    tm_group(wt, wk, tt, evac_v)
            if own:
                wt, wk = load_w(w_in[:, 1024:1536])
                for h in range(4):
                    S.op("act", lambda e, h=h: e.activation(out=eq[:], in_=cs[:, h, :], func=AF.Exp, scale=-1.0 / 16.0, bias=lnq_sb[:, 0:1]),
                         reads=[("cs", h), "lnq"], writes=["eq"])

                    def evac_q(ps, pk, h=h):
                        S.op("dve", lambda e: e.tensor_tensor(out=qtil[:, h, :], in0=ps[:, :], in1=eq[:], op=ALU.mult),
                             reads=[pk, "eq"], writes=[("qtil", h)])
                    fm_group(wt, wk, h, evac_q)
                for half in range(2):
                    wt, wk = load_w(w_in[:, 3088 + half * 512: 3088 + (half + 1) * 512])
                    for tt in range(4):
                        def evac_g(ps, pk, tt=tt, half=half):
                            S.op("act", lambda e: e.activation(out=sg_tm[:, tt, half * 512:(half + 1) * 512], in_=ps[:, :], func=AF.Silu),
                                 reads=[pk], writes=[("sg_tm", tt)])
                        tm_group(wt, wk, tt, evac_g)
            if need_u:
                for half in range(2):
                    wt, wk = load_w(w_in[:, half * 512:(half + 1) * 512])
                    for m in range(4):
                        def evac_u(ps, pk, ch=half * 4 + m):
                            S.op("act", lambda e: e.activation(out=uT[:, ch, 16:16 + SEG], in_=ps[:, :], func=AF.Copy),
                                 reads=[pk], writes=[("uT", ch)])
                        fm_group(wt, wk, m, evac_u)
            if own and seg == 0:
                S.op("dve", lambda e: e.tensor_scalar(out=state32[:].rearrange("p a b -> p (a b)"), in0=state32[:].rearrange("p a b -> p (a b)"),
                                                      scalar1=flag_sb[:, 0:1], scalar2=None, op0=ALU.mult),
                     reads=[("state32", h) for h in range(4)] + ["flag"], writes=[("state32", h) for h in range(4)])
                S.op("dve", lambda e: e.tensor_scalar(out=stateb[:].rearrange("p a b -> p (a b)"), in0=state32[:].rearrange("p a b -> p (a b)"),
                                                      scalar1=1.0, scalar2=None, op0=ALU.mult),
                     reads=[("state32", h) for h in range(4)], writes=[("stateb", h) for h in range(4)])
                for ch in range(8):
                    S.op("pool", lambda e, ch=ch: e.tensor_scalar(out=uT[:, ch, 0:16], in0=uT[:, ch, 0:16], scalar1=flag_sb[:, 0:1], scalar2=None,
                                                                  op0=ALU.mult),
                         reads=[("uT", ch), "flag"], writes=[("uT", ch)])
            if own:
                for g in range(4):
                    w = 2 ** (g + 1)
                    chs = [("uT", 2 * g), ("uT", 2 * g + 1)]
                    src = uT[:, 2 * g:2 * g + 2, :]
                    srck = chs
                    bufs = [(sA, "sA"), (sB, "sB")]
                    bi = 0
                    step = 1
                    L = 16 + SEG
                    while step < w:
                        dst, dk = bufs[bi]
                        S.op("pool", lambda e, src=src, dst=dst, step=step: e.tensor_tensor(
                            out=dst[:, :, step:L], in0=src[:, :, step:L], in1=src[:, :, 0:L - step], op=ALU.add),
                            reads=srck, writes=[dk])
                        src, srck = dst[:, :, :], [dk]
                        bi ^= 1
                        step *= 2
                    S.op("dve", lambda e, src=src, g=g, w=w: e.scalar_tensor_tensor(
                        out=pooled[:, :, :], in0=src[:, :, 16:16 + SEG], scalar=1.0 / w, in1=uT[:, 2 * g:2 * g + 2, 16:16 + SEG],
                        op0=ALU.mult, op1=ALU.subtract), reads=srck + chs, writes=["pooled"])
                    if seg == 0:
                        for cc in range(2):
                            S.op("dve", lambda e, src=src, g=g, cc=cc: e.tensor_tensor(
                                out=fixs[:, cc, :], in0=src[:, cc, 16:32], in1=invc_sb[:, g, :], op=ALU.mult),
                                reads=srck + ["invc"], writes=["fixs"])
                            S.op("dve", lambda e, g=g, cc=cc: e.tensor_tensor(
                                out=pooled[:, cc, 0:16], in0=fixs[:, cc, :], in1=uT[:, 2 * g + cc, 16:32], op=ALU.subtract),
                                reads=["fixs"] + chs, writes=["pooled"])
                    for dh in range(2):
                        ps, pk = next_pg()
                        for cc in range(2):
                            S.op("pe", lambda e, g=g, cc=cc, dh=dh: e.matmul(ps[:, :], lhsT=wpool_sb[:, g, cc, dh * 128:(dh + 1) * 128],
                                                                           rhs=pooled[:, cc, :], start=(cc == 0), stop=(cc == 1)),
                                 reads=["wpool", "pooled"], writes=[pk])
                        ch = 2 * g + dh
                        S.op("dve", lambda e, ch=ch, ps=ps: e.tensor_scalar(out=yT[:, ch, :], in0=ps[:, :], scalar1=bpool_sb[:, ch:ch + 1],
                                                                            scalar2=pscale_sb[:, ch:ch + 1], op0=ALU.add, op1=ALU.mult),
                             reads=[pk, "bpool", "pscale"], writes=[("yT", ch)])
            if need_u:
                for ch in range(8):
                    S.op("pool", lambda e, ch=ch: e.tensor_copy(out=uT[:, ch, 0:16], in_=uT[:, ch, SEG:SEG + 16]),
                         reads=[("uT", ch)], writes=[("uT", ch)])
            if stop_after == "A1d":
                raise _Stop()
            for tt in range(4):
                if own:
                    for h in range(4):
                        sl = slice(tt * 128, (tt + 1) * 128)
                        S.op("pe", lambda e, h=h, sl=sl: e.matmul(PSC[:, (h % 4) * 128:(h % 4 + 1) * 128], lhsT=ktil[:, h, sl], rhs=qtil[:, h, sl],
                                                                  start=True, stop=True),
                             reads=[("ktil", h), ("qtil", h)], writes=["psc"])
                for h in range(4):
                    if own:
                        S.op("dve", lambda e, h=h: e.tensor_tensor(out=scm[:, h % 2, :], in0=PSC[:, h * 128:(h + 1) * 128], in1=triu_sb[:], op=ALU.mult),
                             reads=["psc", "triu"], writes=[("scm", h % 2)])
                        po = PO[h // 2]
                        pok = ("po", h // 2)
                    for c in range(2):
                        rows = slice(c * 64, (c + 1) * 64)
                        cg = tt * 2 + c
                        if own:
                            S.op("pe", lambda e, h=h, c=c, rows=rows, po=po: e.matmul(
                                po[rows, (h % 2) * 256:(h % 2 + 1) * 256], lhsT=scm[rows, h % 2, c * 64:(c + 1) * 64],
                                rhs=v_tm[rows, tt, h * 256:(h + 1) * 256], start=True, stop=False),
                                reads=[("scm", h % 2), ("v_tm", tt)], writes=[pok])
                            S.op("pe", lambda e, h=h, c=c, rows=rows, po=po: e.matmul(
                                po[rows, (h % 2) * 256:(h % 2 + 1) * 256], lhsT=qtil[:, h, tt * 128 + c * 64: tt * 128 + (c + 1) * 64],
                                rhs=stateb[:, h, :], start=False, stop=True),
                                reads=[("qtil", h), ("stateb", h)], writes=[pok])
                        slot = (h * 2 + c) % 2
                        S.op("pe", lambda e, h=h, rows=rows, slot=slot: e.matmul(
                            PS_[:, slot * 256:(slot + 1) * 256], lhsT=khat_tm[rows, tt, h, :], rhs=v_tm[rows, tt, h * 256:(h + 1) * 256],
                            start=True, stop=True),
                            reads=[("khat_tm", tt), ("v_tm", tt)], writes=["pss"])
                        S.op("dve", lambda e, h=h, cg=cg, slot=slot: e.scalar_tensor_tensor(
                            out=state32[:, h, :], in0=state32[:, h, :], scalar=eL[:, h, cg:cg + 1], in1=PS_[:, slot * 256:(slot + 1) * 256],
                            op0=ALU.mult, op1=ALU.add),
                            reads=["pss", ("eL", h), ("state32", h)], writes=[("state32", h)])
                        S.op("act", lambda e, h=h: e.activation(out=stateb[:, h, :], in_=state32[:, h, :], func=AF.Copy),
                             reads=[("state32", h)], writes=[("stateb", h)])
                    if own:
                        S.op("act", lambda e, h=h, po=po: e.activation(out=osq[:], in_=po[:, (h % 2) * 256:(h % 2 + 1) * 256], func=AF.Square,
                                                                       accum_out=ssq[:, 0:1]),
                             reads=[pok], writes=["osq", "ssq"])
                        S.op("act", lambda e: e.activation(out=rr[:], in_=ssq[:], func=AF.Sqrt, bias=rms_eps[:, 0:1], scale=1.0 / 256.0),
                             reads=["ssq", "rms_eps"], writes=["rr0"])
                        S.op("dve", lambda e: e.reciprocal(out=rr[:], in_=rr[:]), reads=["rr0"], writes=["rr"])
                        S.op("dve", lambda e, h=h, tt=tt: e.tensor_tensor(out=nwsg[:], in0=nw_bc[:], in1=sg_tm[:, tt, h * 256:(h + 1) * 256], op=ALU.mult),
                             reads=["nw_bc", ("sg_tm", tt)], writes=["nwsg"])
                        S.op("dve", lambda e, h=h, po=po: e.scalar_tensor_tensor(
                            out=yB[:, h * 256:(h + 1) * 256], in0=po[:, (h % 2) * 256:(h % 2 + 1) * 256], scalar=rr[:, 0:1], in1=nwsg[:],
                            op0=ALU.mult, op1=ALU.mult), reads=[pok, "rr", "nwsg"], writes=[("yB", h)])
                if own:
                    PT = PTS[tt % 2]
                    ptk = "pt%d" % (tt % 2)
                    for j in range(8):
                        S.op("pe", lambda e, j=j, PT=PT: e.transpose(PT[:, j * 128:(j + 1) * 128], yB[:, j * 128:(j + 1) * 128], identb[:]),
                             reads=[("yB", j // 2), "identb"], writes=[ptk])
                    S.op("act", lambda e, tt=tt, PT=PT: e.activation(out=yT[:, 8:16, tt * 128:(tt + 1) * 128],
                                                              in_=PT[:, :].rearrange("p (j t) -> p j t", j=8), func=AF.Copy),
                         reads=[ptk], writes=[("yTb", tt)])
            if own:
                ykeys = [("yT", ch) for ch in range(8)] + [("yTb", tt) for tt in range(4)]
                S.dma("sp", ybufT[:, :, seg * SEG:(seg + 1) * SEG].rearrange("c p t -> p c t"), yT[:], chan="yT",
                      reads=ykeys, writes=[("ybufT", seg)])

        try:
            for seg in range(NSEG):
                mixer_segment(x_pre, seg, own=False, need_u=(seg == NSEG - 1))
            for seg in range(NSEG):
                mixer_segment(x_own, seg, own=True, need_u=True)
        except _Stop:
            S.barrier()
            ph.close()
            print("sems", len(S.dsem), "inst", S.n_inst, "waits", S.n_wait)
            return nc

        if debug:
            for seg in range(NSEG):
                S.dma("sp", yT[:], ybufT[:, :, seg * SEG:(seg + 1) * SEG].rearrange("c p t -> p c t"), chan="yT",
                      reads=[("ybufT", seg)], writes=["yT_dbg"] + [("yT", ch) for ch in range(8)] + [("yTb", tt) for tt in range(4)])
                S.dma("sp", dbg["yT"][:, :, seg * SEG:(seg + 1) * SEG].rearrange("c p t -> p c t"), yT[:], chan="yT",
                      reads=["yT_dbg"], writes=["dbg_yT"])
        S.barrier()
        ph.close()
        if stop_after == "A1":
            print("sems", len(S.dsem), "inst", S.n_inst, "waits", S.n_wait)
            return nc
        ph = ExitStack()
        cur[0] = ph
        PB = [PG[0], PG[1], PSC, PO[0], PO[1], PS_]
        PBK = ["pg0", "pg1", "psc", ("po", 0), ("po", 1), "pss"]
        pb_i = [0]

        def next_pb():
            i = pb_i[0] % len(PB)
            pb_i[0] += 1
            return PB[i], PBK[i]

        wout = sb("wout", [128, 16, D], BF16)
        for j in range(4):
            for h in range(2):
                S.dma("pool", wout[:, h * 8:(h + 1) * 8, j * 512:(j + 1) * 512],
                      w_out[h * 1024:(h + 1) * 1024, j * 512:(j + 1) * 512].rearrange("(kc p) n -> p kc n", p=128),
                      chan="wout", writes=["wout"])

        def bc_tile(name, src_row):
            t = sb(name, [128, D])
            S.dma("sp", t[:], src_row.partition_broadcast(128), chan=name, reads=["moddram"], writes=[name])
            return t
        g1_bc = bc_tile("g1_bc", moddram[0, 2 * D:3 * D])
        sh2_bc = bc_tile("sh2_bc", moddram[0, 3 * D:4 * D])
        sc2p_bc = bc_tile("sc2p_bc", moddram[0, 4 * D:5 * D])
        S.op("pool", lambda e: e.tensor_scalar(out=sc2p_bc[:], in0=sc2p_bc[:], scalar1=1.0, scalar2=None, op0=ALU.add),
             reads=["sc2p_bc"], writes=["sc2p_bc"])
        l1g_bc = bc_tile("l1g_bc", ln1_g[0, :])
        l1b_bc = bc_tile("l1b_bc", ln1_b[0, :])
        wr_sb = sb("wr_sb", [128, 16, NE])
        S.dma("sp", wr_sb[:], w_router.rearrange("(kc p) n -> p kc n", p=128), chan="wr_sb", writes=["wr_sb"])
        br_bc = sb("br_bc", [128, NE])
        S.dma("sp", br_bc[:], b_router[0, :].partition_broadcast(128), chan="br_bc", writes=["br_bc"])
        ltri_sb = sb("ltri_sb", [128, 128])
        S.dma("sp", ltri_sb[:], ltri, chan="ltri", writes=["ltri"])
        ones_sq = sb("ones_sq", [128, 128])
        S.op("pool", lambda e: e.memset(ones_sq[:], 1.0), writes=["ones_sq"])
        iotae_sb = sb("iotae_sb", [128, NE])
        S.dma("sp", iotae_sb[:], iota_e, chan="iotae", writes=["iotae"])
        iota32 = sb("iota32", [128, NE])
        S.dma("sp", iota32[:], iota_n, chan="iota32", writes=["iota32"])
        tokid_sb = sb("tokid_sb", [128, 16], I32)
        S.dma("sp", tokid_sb[:], tokid, chan="tokid", writes=["tokid"])
        carry = sb("carry", [128, NE])
        S.op("pool", lambda e: e.memset(carry[:], 0.0), writes=["carry"])
        zt = sb("zt", [128, D])
        S.op("pool", lambda e: e.memset(zt[:], 0.0), writes=["zt"])
        for r in range(17):
            S.dma("sp", yacc[r * 128:(r + 1) * 128, :], zt[:], chan="zt", reads=["zt"], writes=["yacc"])
        ztb = sb("ztb", [128, D], BF16)
        S.op("pool", lambda e: e.memset(ztb[:], 0.0), writes=["ztb"])
        S.dma("sp", h2buf[T:T + 128, :], ztb[:], chan="ztb", reads=["ztb"], writes=["h2dummy"])
        li_sb = sb("li_sb", [128, 1024], I32)
        S.dma("sp", li_sb[:], list_init.rearrange("(p r) two -> p (r two)", p=128), chan="li_sb", writes=["li_sb"])
        S.dma("sp", lists.rearrange("(p r) two -> p (r two)", p=128), li_sb[:], chan="li_sb", reads=["li_sb"], writes=["lists"])

        YTT = [sb("ytt%d" % i, [128, 16, 128], BF16) for i in range(2)]
        XA = [sb("xa%d" % i, [128, D]) for i in range(2)]
        x1pre = sb("x1pre", [128, D])
        x1t = sb("x1t", [128, D])
        h2t = sb("h2t", [128, D])
        h2b = sb("h2b", [128, D], BF16)
        h2T = sb("h2T", [128, 16, 128])
        stats2 = sb("stats2", [128, 4, 6])
        mv2 = sb("mv2", [128, 2])
        rstd2 = sb("rstd2", [128, 1])
        lg = sb("lg", [128, NE])
        max8 = sb("max8", [128, 8])
        idx8 = sb("idx8", [128, 8], U32)
        idxf = sb("idxf", [128, 8])
        negm = sb("negm", [128, 1])
        ew = sb("ew", [128, 4])
        den = sb("den", [128, 1])
        w4 = sb("w4", [128, 4])
        maskt = sb("maskt", [128, NE])
        slotf = sb("slotf", [128, NE])
        oh = sb("oh", [128, NE])
        sl4 = sb("sl4", [128, 4])
        sl4i = sb("sl4i", [128, 4], I32)
        pairs = sb("pairs", [128, 16, 4, 2], I32)

        def ln_stats(src, skey, st_t, mv_t, rs_t, tag):
            for j in range(4):
                S.op("dve", lambda e, j=j: e.bn_stats(out=st_t[:, j, :], in_=src[:, j * 512:(j + 1) * 512]),
                     reads=[skey], writes=[(tag + "st", j)])
            S.op("dve", lambda e: e.bn_aggr(out=mv_t[:], in_=st_t[:].rearrange("p a b -> p (a b)")),
                 reads=[(tag + "st", j) for j in range(4)], writes=[tag + "mv"])
            S.op("act", lambda e: e.activation(out=rs_t[:], in_=mv_t[:, 1:2], func=AF.Sqrt, bias=eps_sb[:, 0:1], scale=1.0),
                 reads=[tag + "mv", "eps"], writes=[tag + "rs0"])
            S.op("dve", lambda e: e.reciprocal(out=rs_t[:], in_=rs_t[:]), reads=[tag + "rs0"], writes=[tag + "rs"])

        for ti in range(16):
            b2 = ti % 2
            yk, xk = "ytt%d" % b2, "xa%d" % b2
            S.dma("sp", YTT[b2][:], ybufT[:, :, ti * 128:(ti + 1) * 128].rearrange("c p t -> p c t"), chan=yk,
                  reads=[("ybufT", ti // 4)], writes=[yk])
            S.dma("sp", XA[b2][:], x_own[ti * 128:(ti + 1) * 128, :], chan=xk, writes=[xk])
            for j in range(4):
                ps, pk = next_pb()
                for kc in range(16):
                    S.op("pe", lambda e, kc=kc, j=j, ps=ps, b2=b2: e.matmul(ps[:, :], lhsT=YTT[b2][:, kc, :], rhs=wout[:, kc, j * 512:(j + 1) * 512],
                                                                          start=(kc == 0), stop=(kc == 15)),
                         reads=[yk, "wout"], writes=[pk])
                S.op("dve", lambda e, j=j, ps=ps: e.tensor_tensor(out=x1pre[:, j * 512:(j + 1) * 512], in0=ps[:, :], in1=g1_bc[:, j * 512:(j + 1) * 512], op=ALU.mult),
                     reads=[pk, "g1_bc"], writes=[("x1pre", j)])
                S.op("dve", lambda e, j=j, b2=b2: e.scalar_tensor_tensor(out=x1pre[:, j * 512:(j + 1) * 512], in0=XA[b2][:, j * 512:(j + 1) * 512], scalar=ALPHA,
                                                                       in1=x1pre[:, j * 512:(j + 1) * 512], op0=ALU.mult, op1=ALU.add),
                     reads=[xk, ("x1pre", j)], writes=[("x1pre", j)])
            xpk = [("x1pre", j) for j in range(4)]
            for j in range(4):
                S.op("dve", lambda e, j=j: e.bn_stats(out=stats2[:, j, :], in_=x1pre[:, j * 512:(j + 1) * 512]),
                     reads=[("x1pre", j)], writes=[("ast", j)])
            S.op("dve", lambda e: e.bn_aggr(out=mv2[:], in_=stats2[:].rearrange("p a b -> p (a b)")),
                 reads=[("ast", j) for j in range(4)], writes=["amv"])
            S.op("act", lambda e: e.activation(out=rstd2[:], in_=mv2[:, 1:2], func=AF.Sqrt, bias=eps_sb[:, 0:1], scale=1.0),
                 reads=["amv", "eps"], writes=["ars0"])
            S.op("dve", lambda e: e.reciprocal(out=rstd2[:], in_=rstd2[:]), reads=["ars0"], writes=["ars"])
            S.op("dve", lambda e: e.tensor_scalar(out=x1t[:], in0=x1pre[:], scalar1=mv2[:, 0:1], scalar2=rstd2[:, 0:1], op0=ALU.subtract, op1=ALU.mult),
                 reads=xpk + ["amv", "ars"], writes=["x1t"])
            S.op("pool", lambda e: e.tensor_tensor(out=x1t[:], in0=x1t[:], in1=l1g_bc[:], op=ALU.mult), reads=["x1t", "l1g_bc"], writes=["x1t"])
            S.op("pool", lambda e: e.tensor_tensor(out=x1t[:], in0=x1t[:], in1=l1b_bc[:], op=ALU.add), reads=["x1t", "l1b_bc"], writes=["x1t"])
            S.dma("sp", x1buf[ti * 128:(ti + 1) * 128, :], x1t[:], chan="x1t", reads=["x1t"], writes=[("x1buf", ti)])
            ln_stats(x1t, "x1t", stats2, mv2, rstd2, "b")
            S.op("dve", lambda e: e.tensor_scalar(out=h2t[:], in0=x1t[:], scalar1=mv2[:, 0:1], scalar2=rstd2[:, 0:1], op0=ALU.subtract, op1=ALU.mult),
                 reads=["x1t", "bmv", "brs"], writes=["h2t"])
            S.op("pool", lambda e: e.tensor_tensor(out=h2t[:], in0=h2t[:], in1=sc2p_bc[:], op=ALU.mult), reads=["h2t", "sc2p_bc"], writes=["h2t"])
            S.op("pool", lambda e: e.tensor_tensor(out=h2t[:], in0=h2t[:], in1=sh2_bc[:], op=ALU.add), reads=["h2t", "sh2_bc"], writes=["h2t"])
            S.op("act", lambda e: e.activation(out=h2b[:], in_=h2t[:], func=AF.Copy), reads=["h2t"], writes=["h2b"])
            S.dma("sp", h2buf[ti * 128:(ti + 1) * 128, :], h2b[:], chan="h2b", reads=["h2b"], writes=[("h2buf", ti)])
            for q4 in range(4):
                ps, pk = next_pb()
                for j in range(4):
                    kc = q4 * 4 + j
                    S.op("pe", lambda e, kc=kc, j=j, ps=ps: e.transpose(ps[:, j * 128:(j + 1) * 128], h2t[:, kc * 128:(kc + 1) * 128], ident32[:]),
                         reads=["h2t", "ident32"], writes=[pk])
                S.op("act", lambda e, q4=q4, ps=ps: e.activation(out=h2T[:, q4 * 4:(q4 + 1) * 4, :], in_=ps[:, :].rearrange("p (j t) -> p j t", j=4), func=AF.Copy),
                     reads=[pk], writes=[("h2T", q4)])
            ps, pk = next_pb()
            for kc in range(16):
                S.op("pe", lambda e, kc=kc, ps=ps: e.matmul(ps[:, 0:NE], lhsT=h2T[:, kc, :], rhs=wr_sb[:, kc, :], start=(kc == 0), stop=(kc == 15)),
                     reads=[("h2T", kc // 4), "wr_sb"], writes=[pk])
            S.op("dve", lambda e, ps=ps: e.tensor_tensor(out=lg[:], in0=ps[:, 0:NE], in1=br_bc[:], op=ALU.add), reads=[pk, "br_bc"], writes=["lg"])
            if debug:
                S.dma("sp", dbg["lg"][ti * 128:(ti + 1) * 128, :], lg[:], chan="lg", reads=["lg"], writes=["dbg_lg"])
            S.op("dve", lambda e: e.max(out=max8[:], in_=lg[:]), reads=["lg"], writes=["max8"])
            S.op("dve", lambda e: e.max_index(out=idx8[:], in_max=max8[:], in_values=lg[:]), reads=["lg", "max8"], writes=["idx8"])
            S.op("dve", lambda e: e.tensor_copy(out=idxf[:], in_=idx8[:]), reads=["idx8"], writes=["idxf"])
            S.op("dve", lambda e: e.tensor_scalar(out=negm[:], in0=max8[:, 0:1], scalar1=-1.0, scalar2=None, op0=ALU.mult), reads=["max8"], writes=["negm"])
            S.op("act", lambda e: e.activation(out=ew[:], in_=max8[:, 0:4], func=AF.Exp, bias=negm[:, 0:1], scale=1.0, accum_out=den[:, 0:1]),
                 reads=["max8", "negm"], writes=["ew", "den"])
            S.op("dve", lambda e: e.reciprocal(out=den[:], in_=den[:]), reads=["den"], writes=["den"])
            S.op("dve", lambda e: e.tensor_scalar(out=w4[:], in0=ew[:], scalar1=den[:, 0:1], scalar2=None, op0=ALU.mult), reads=["ew", "den"], writes=["w4"])
            S.op("dve", lambda e: e.tensor_scalar(out=maskt[:], in0=lg[:], scalar1=max8[:, 3:4], scalar2=None, op0=ALU.is_ge), reads=["lg", "max8"], writes=["maskt"])
            ps, pk = next_pb()
            S.op("pe", lambda e, ps=ps: e.matmul(ps[:, 0:NE], lhsT=ltri_sb[:], rhs=maskt[:], start=True, stop=True), reads=["ltri", "maskt"], writes=[pk])
            S.op("dve", lambda e, ps=ps: e.tensor_tensor(out=slotf[:], in0=ps[:, 0:NE], in1=carry[:], op=ALU.add), reads=[pk, "carry"], writes=["slotf"])
            S.op("dve", lambda e: e.tensor_tensor(out=slotf[:], in0=slotf[:], in1=iotae_sb[:], op=ALU.add), reads=["slotf", "iotae"], writes=["slotf"])
            ps2, pk2 = next_pb()
            S.op("pe", lambda e, ps2=ps2: e.matmul(ps2[:, 0:NE], lhsT=ones_sq[:], rhs=maskt[:], start=True, stop=True), reads=["ones_sq", "maskt"], writes=[pk2])
            S.op("dve", lambda e, ps2=ps2: e.tensor_tensor(out=carry[:], in0=ps2[:, 0:NE], in1=carry[:], op=ALU.add), reads=[pk2, "carry"], writes=["carry"])
            for j in range(4):
                S.op("dve", lambda e, j=j: e.tensor_scalar(out=oh[:], in0=iota32[:], scalar1=idxf[:, j:j + 1], scalar2=None, op0=ALU.is_equal),
                     reads=["iota32", "idxf"], writes=["oh"])
                S.op("dve", lambda e: e.tensor_tensor(out=oh[:], in0=oh[:], in1=slotf[:], op=ALU.mult), reads=["oh", "slotf"], writes=["oh"])
                S.op("dve", lambda e, j=j: e.reduce_sum(out=sl4[:, j:j + 1], in_=oh[:], axis=AX.X), reads=["oh"], writes=["sl4"])
            S.op("dve", lambda e: e.tensor_copy(out=sl4i[:], in_=sl4[:]), reads=["sl4"], writes=["sl4i"])
            S.op("dve", lambda e, ti=ti: e.tensor_copy(out=pairs[:, ti, :, 0], in_=tokid_sb[:, ti:ti + 1].broadcast_to([128, 4])), reads=["tokid"], writes=[("pairs", ti)])
            S.op("dve", lambda e, ti=ti: e.tensor_copy(out=pairs[:, ti, :, 1].bitcast(F32), in_=w4[:]), reads=["w4", ("pairs", ti)], writes=[("pairs", ti)])
            for j in range(4):
                S.dma("pool", lists, pairs[:, ti, j, :], chan=("pairs", ti), reads=[("pairs", ti), "sl4i"], writes=["lists"],
                      indirect=dict(out_offset=bass.IndirectOffsetOnAxis(ap=sl4i[:, j:j + 1], axis=0), in_offset=None))
        flg_f = sb("flg_f", [1, 4 * NE])
        for g_ in range(4):
            S.op("dve", lambda e, g_=g_: e.tensor_scalar(out=flg_f[0:1, g_ * NE:(g_ + 1) * NE], in0=carry[0:1, :], scalar1=float(g_ * GRP),
                                                         scalar2=None, op0=ALU.is_gt), reads=["carry"], writes=["flg_f"])
        S.op("dve", lambda e: e.tensor_copy(out=flg_i[:], in_=flg_f[:]), reads=["flg_f"], writes=["flg_i"])
        S.barrier()
        ph.close()
        if stop_after == "A2":
            print("sems", len(S.dsem), "inst", S.n_inst, "waits", S.n_wait)
            return nc

        ph = ExitStack()
        cur[0] = ph
        WB = [sb("wbm%d" % i, [128, 16, 512], BF16) for i in range(4)]
        wb_i[0] = 0
        NWB = 4

        def load_w2(src_ap):
            i = wb_i[0] % NWB
            wb_i[0] += 1
            k = "wbm%d" % i
            for h in range(2):
                S.dma("pool", WB[i][:, h * 8:(h + 1) * 8, :],
                      src_ap[h * 1024:(h + 1) * 1024, :].rearrange("(kc p) n -> p kc n", p=128), chan=k, writes=[k])
            return WB[i], k

        stg = sb("stg", [128, 16, 512])

        def load_w_hw(src_ap):
            i = wb_i[0] % NWB
            wb_i[0] += 1
            k = "wbm%d" % i
            S.dma("sp", stg[:], src_ap.rearrange("(kc p) n -> p kc n", p=128), chan="stg", writes=["stg"])
            for h in range(2):
                S.op("act", lambda e, h=h, i=i: e.activation(out=WB[i][:, h * 8:(h + 1) * 8, :], in_=stg[:, h * 8:(h + 1) * 8, :], func=AF.Copy),
                     reads=["stg"], writes=[k])
            return WB[i], k

        lst = sb("lst", [128, 4, 2], I32)
        dum = sb("dum", [1, 32])
        xg = sb("xg", [128, 4, D], BF16)
        xT = sb("xT", [128, 16, GRP], BF16)
        actT = sb("actT", [128, 16, GRP], BF16)
        Yt = sb("Yt", [128, 4, D])
        bd_bc = sb("bd_bc", [128, D])
        bg_sb = sb("bg_sb", [128, 16])
        bu_sb = sb("bu_sb", [128, 16])
        gt = [sb("gt%d" % i, [128, GRP]) for i in range(2)]
        sgm = [sb("sgm%d" % i, [128, GRP]) for i in range(2)]
        ut = [sb("ut%d" % i, [128, GRP]) for i in range(2)]
        PGA = [(PG[0], "pg0"), (PG[1], "pg1")]
        PUP = [(PO[0], ("po", 0)), (PO[1], ("po", 1))]
        PDN = [(PSC, "psc"), (PS_, "pss")]
        it = [0]
        flag_regs = nc.alloc_registers("flg", engines=mybir.ALL_ENGINES)
        for e_ in range(NE):
            S.dma("sp", bd_bc[:], b_down[e_, :].partition_broadcast(128), chan="bd_bc", writes=["bd_bc"])
            S.dma("sp", bg_sb[:], b_gate[e_], chan="bg_sb", writes=["bg_sb"])
            S.dma("sp", bu_sb[:], b_up[e_], chan="bu_sb", writes=["bu_sb"])
            for g_ in range(n_groups):
              base = e_ * CAP + g_ * GRP
              nc.regs_load(flag_regs, flg_i[0:1, g_ * NE + e_: g_ * NE + e_ + 1])
              snap_cnt = dict(S.cnt)
              snap_d = dict(S.dcnt)
              snap_seen = {k_: dict(v_) for k_, v_ in S.seen.items()}
              with nc.If_cmp(flag_regs, 0, "IS_NE"):
                S.dma("sp", lst[:], lists[base:base + GRP, :].rearrange("(b p) two -> p b two", p=128), chan="lst",
                      reads=["lists"], writes=["lst"])
                for blk in range(4):
                    S.dma("pool", xg[:, blk, :], h2buf, chan="xg", reads=["lst", "h2dummy"] + [("h2buf", ti) for ti in range(16)],
                          writes=[("xg", blk)],
                          indirect=dict(out_offset=None, in_offset=bass.IndirectOffsetOnAxis(ap=lst[:, blk, 0:1], axis=0)))
                for blk in range(4):
                    S.lastw[("xg", blk)] = ("dma", "xg", S.dcnt["xg"])
                for blk in range(4):
                    for half in range(2):
                        PT = PTS[half]
                        ptk = "pt%d" % half
                        for j in range(8):
                            kc = half * 8 + j
                            S.op("pe", lambda e, kc=kc, j=j, PT=PT, blk=blk: e.transpose(PT[:, j * 128:(j + 1) * 128], xg[:, blk, kc * 128:(kc + 1) * 128], identb[:]),
                                 reads=[("xg", blk), "identb"], writes=[ptk])
                        eng = "act" if half == 0 else "dve"
                        if eng == "act":
                            S.op("act", lambda e, PT=PT, blk=blk, half=half: e.activation(
                                out=xT[:, half * 8:(half + 1) * 8, blk * 128:(blk + 1) * 128], in_=PT[:, :].rearrange("p (j t) -> p j t", j=8), func=AF.Copy),
                                reads=[ptk], writes=[("xT", blk)])
                        else:
                            S.op("dve", lambda e, PT=PT, blk=blk, half=half: e.tensor_copy(
                                out=xT[:, half * 8:(half + 1) * 8, blk * 128:(blk + 1) * 128], in_=PT[:, :].rearrange("p (j t) -> p j t", j=8)),
                                reads=[ptk], writes=[("xT", blk)])
                xkeys = [("xT", blk) for blk in range(4)]
                for fq in range(4):
                    wg, wgk = load_w2(w_gate[e_, :, fq * 512:(fq + 1) * 512])
                    wu, wuk = load_w2(w_up[e_, :, fq * 512:(fq + 1) * 512])
                    for m in range(4):
                        fc = fq * 4 + m
                        i2 = it[0] % 2
                        it[0] += 1
                        pg_, pgk = PGA[i2]
                        pu_, puk = PUP[i2]
                        for kc in range(16):
                            S.op("pe", lambda e, kc=kc, m=m, wg=wg, pg_=pg_: e.matmul(pg_[:, :], lhsT=wg[:, kc, m * 128:(m + 1) * 128], rhs=xT[:, kc, :],
                                                                                    start=(kc == 0), stop=(kc == 15)), reads=[wgk] + xkeys, writes=[pgk])
                        for kc in range(16):
                            S.op("pe", lambda e, kc=kc, m=m, wu=wu, pu_=pu_: e.matmul(pu_[:, :], lhsT=wu[:, kc, m * 128:(m + 1) * 128], rhs=xT[:, kc, :],
                                                                                    start=(kc == 0), stop=(kc == 15)), reads=[wuk] + xkeys, writes=[puk])
                        gk_, sk_, uk_ = "gt%d" % i2, "sgm%d" % i2, "ut%d" % i2
                        S.op("dve", lambda e, fc=fc, i2=i2, pg_=pg_: e.tensor_scalar(out=gt[i2][:], in0=pg_[:, :], scalar1=bg_sb[:, fc:fc + 1], scalar2=7.0,
                                                                                   op0=ALU.add, op1=ALU.min), reads=[pgk, "bg_sb"], writes=[gk_])
                        S.op("act", lambda e, i2=i2: e.activation(out=sgm[i2][:], in_=gt[i2][:], func=AF.Sigmoid, scale=1.702), reads=[gk_], writes=[sk_])
                        S.op("dve", lambda e, fc=fc, i2=i2, pu_=pu_: e.tensor_scalar(out=ut[i2][:], in0=pu_[:, :], scalar1=bu_sb[:, fc:fc + 1], scalar2=7.0,
                                                                                   op0=ALU.add, op1=ALU.min), reads=[puk, "bu_sb"], writes=[uk_])
                        S.op("dve", lambda e, i2=i2: e.tensor_scalar(out=ut[i2][:], in0=ut[i2][:], scalar1=-7.0, scalar2=1.0, op0=ALU.max, op1=ALU.add),
                             reads=[uk_], writes=[uk_])
                        S.op("dve", lambda e, i2=i2: e.tensor_tensor(out=gt[i2][:], in0=gt[i2][:], in1=sgm[i2][:], op=ALU.mult), reads=[gk_, sk_], writes=[gk_])
                        S.op("dve", lambda e, i2=i2, fc=fc: e.tensor_tensor(out=actT[:, fc, :], in0=gt[i2][:], in1=ut[i2][:], op=ALU.mult),
                             reads=[gk_, uk_], writes=[("actT", fc)])
                akeys = [("actT", fc) for fc in range(16)]
                for dq in range(4):
                    wd, wdk = load_w_hw(w_down[e_, :, dq * 512:(dq + 1) * 512])
                    for blk in range(4):
                        i2 = it[0] % 2
                        it[0] += 1
                        pd_, pdk = PDN[i2]
                        for fc in range(16):
                            S.op("pe", lambda e, fc=fc, blk=blk, wd=wd, pd_=pd_: e.matmul(pd_[:, :], lhsT=actT[:, fc, blk * 128:(blk + 1) * 128], rhs=wd[:, fc, :],
                                                                                        start=(fc == 0), stop=(fc == 15)), reads=[wdk] + akeys, writes=[pdk])
                        S.op("dve", lambda e, blk=blk, dq=dq, pd_=pd_: e.tensor_tensor(out=Yt[:, blk, dq * 512:(dq + 1) * 512], in0=pd_[:, :],
                                                                                     in1=bd_bc[:, dq * 512:(dq + 1) * 512], op=ALU.add),
                             reads=[pdk, "bd_bc"], writes=[("Yt", blk)])
                        S.op("dve", lambda e, blk=blk, dq=dq: e.tensor_scalar(out=Yt[:, blk, dq * 512:(dq + 1) * 512], in0=Yt[:, blk, dq * 512:(dq + 1) * 512],
                                                                             scalar1=lst[:, blk, 1:2].bitcast(F32), scalar2=None, op0=ALU.mult),
                             reads=[("Yt", blk), "lst"], writes=[("Yt", blk)])
                for blk in range(4):
                    S.dma("pool", yacc, Yt[:, blk, :], chan="Ysc", reads=[("Yt", blk), "lst"], writes=["yacc"],
                          indirect=dict(out_offset=bass.IndirectOffsetOnAxis(ap=lst[:, blk, 0:1], axis=0), in_offset=None, compute_op=ALU.add))
                fin_ = ("dma", "Ysc", S.dcnt["Ysc"])
                for blk in range(4):
                    S.readers[("Yt", blk)] = [fin_]
                S.readers["lst"] = [fin_]
                S.lastw["yacc"] = fin_
              with nc.Else():
                for en_ in S.eng:
                    dn = S.cnt[en_] - snap_cnt[en_]
                    if dn > 0:
                        if snap_cnt[en_] > 0:
                            S.eng[en_].wait_ge(S.sem[en_], snap_cnt[en_])
                        S.eng[en_].sem_inc(S.sem[en_], dn)
                ci_ = 0
                for k_ in S.dcnt:
                    dd = S.dcnt[k_] - snap_d.get(k_, 0)
                    if dd > 0:
                        if S.dq[k_] == "pool":
                            if snap_d.get(k_, 0) > 0:
                                nc.gpsimd.wait_ge(S.dsem[k_], snap_d[k_])
                            nc.gpsimd.dma_start(out=dum[0:1, ci_:ci_ + 1], in_=flag[0:1, 0:1]).then_inc(S.dsem[k_], dd)
                            ci_ += 1
                        else:
                            if snap_d.get(k_, 0) > 0:
                                nc.sync.wait_ge(S.dsem[k_], snap_d[k_])
                            nc.sync.sem_inc(S.dsem[k_], dd)
              S.seen = snap_seen
        S.barrier()
        ph.close()

        ph = ExitStack()
        cur[0] = ph
        g2_bc = bc_tile("g2_bc", moddram[0, 5 * D:6 * D])
        l2g_bc = bc_tile("l2g_bc", ln2_g[0, :])
        l2b_bc = bc_tile("l2b_bc", ln2_b[0, :])
        YA = [sb("ya%d" % i, [128, D]) for i in range(2)]
        X1 = [sb("x1_%d" % i, [128, D]) for i in range(2)]
        OT = [sb("ot%d" % i, [128, D]) for i in range(2)]
        stats3 = sb("stats3", [128, 4, 6])
        mv3 = sb("mv3", [128, 2])
        rstd3 = sb("rstd3", [128, 1])
        for ti in range(16):
            b2 = ti % 2
            yk, xk, ok = "ya%d" % b2, "x1_%d" % b2, "ot%d" % b2
            S.dma("sp", YA[b2][:], yacc[ti * 128:(ti + 1) * 128, :], chan=yk, reads=["yacc"], writes=[yk])
            S.dma("sp", X1[b2][:], x1buf[ti * 128:(ti + 1) * 128, :], chan=xk, reads=[("x1buf", ti)], writes=[xk])
            S.op("pool", lambda e, b2=b2: e.tensor_tensor(out=YA[b2][:], in0=YA[b2][:], in1=g2_bc[:], op=ALU.mult), reads=[yk, "g2_bc"], writes=[yk])
            S.op("dve", lambda e, b2=b2: e.scalar_tensor_tensor(out=YA[b2][:], in0=X1[b2][:], scalar=ALPHA, in1=YA[b2][:], op0=ALU.mult, op1=ALU.add),
                 reads=[xk, yk], writes=[yk])
            ln_stats(YA[b2], yk, stats3, mv3, rstd3, "c")
            S.op("dve", lambda e, b2=b2: e.tensor_scalar(out=OT[b2][:], in0=YA[b2][:], scalar1=mv3[:, 0:1], scalar2=rstd3[:, 0:1], op0=ALU.subtract, op1=ALU.mult),
                 reads=[yk, "cmv", "crs"], writes=[ok])
            S.op("pool", lambda e, b2=b2: e.tensor_tensor(out=OT[b2][:], in0=OT[b2][:], in1=l2g_bc[:], op=ALU.mult), reads=[ok, "l2g_bc"], writes=[ok])
            S.op("pool", lambda e, b2=b2: e.tensor_tensor(out=OT[b2][:], in0=OT[b2][:], in1=l2b_bc[:], op=ALU.add), reads=[ok, "l2b_bc"], writes=[ok])
            S.dma("sp", out[ti * 128:(ti + 1) * 128, :], OT[b2][:], chan=ok, reads=[ok], writes=["out"])
        S.barrier()
        ph.close()
        print("sems", len(S.dsem), "inst", S.n_inst, "waits", S.n_wait)
    return nc


def host_consts():
    ident = np.eye(128, dtype=np.float32)
    triu2 = np.zeros((128, 128), np.float32)
    for b in range(2):
        triu2[b * 64:(b + 1) * 64, b * 64:(b + 1) * 64] = np.triu(np.ones((64, 64), np.float32))
    rmask = np.ones((128, SEG), np.float32)
    rmask[:, ::64] = 0.0
    ltri = np.triu(np.ones((128, 128), np.float32), 1)
    iota_e = np.tile((np.arange(NE, dtype=np.float32) * CAP)[None, :], (128, 1))
    tokid = (np.arange(16, dtype=np.int32)[None, :] * 128 + np.arange(128, dtype=np.int32)[:, None]).astype(np.int32)
    iota_n = np.tile(np.arange(NE, dtype=np.float32)[None, :], (128, 1))
    list_init = np.zeros((NE * CAP, 2), np.int32)
    list_init[:, 0] = T + (np.arange(NE * CAP) % 128)
    return dict(ident_f=ident, triu2=triu2, rmask=rmask, ltri=ltri, iota_e=iota_e, tokid=tokid, iota_n=iota_n, list_init=list_init)


def make_in_maps(inp):
    f = lambda a: np.ascontiguousarray(a, dtype=np.float32)
    x = inp["x"]
    consts = host_consts()
    shared = dict(
        w_ada=f(inp["w_ada"][0]), b_ada=f(inp["b_ada"][0][None, :]), w_in=f(inp["w_in"][0]), w_gk=f(inp["w_gk"][0]),
        b_gk=f(inp["b_gk"][0].reshape(4, 128).T), w_pool=f(inp["w_pool"][0]),
        b_pool=f(inp["b_pool"][0].reshape(8, 128).T), pool_scale=f(inp["pool_scale"][0].reshape(8, 128).T),
        gla_norm_w=f(inp["gla_norm_w"][0][None, :]), w_out=f(inp["w_out"][0]),
        ln1_g=f(inp["ln1_g"][0][None, :]), ln1_b=f(inp["ln1_b"][0][None, :]),
        w_router=f(inp["w_router"][0]), b_router=f(inp["b_router"][0][None, :]),
        w_gate=f(inp["w_gate"][0]), b_gate=f(inp["b_gate"][0].reshape(NE, 16, 128).transpose(0, 2, 1)),
        w_up=f(inp["w_up"][0]), b_up=f(inp["b_up"][0].reshape(NE, 16, 128).transpose(0, 2, 1)),
        w_down=f(inp["w_down"][0]), b_down=f(inp["b_down"][0]),
        ln2_g=f(inp["ln2_g"][0][None, :]), ln2_b=f(inp["ln2_b"][0][None, :]),
    )
    shared.update(consts)
    maps = []
    for core in range(8):
        b, half = core // 2, core % 2
        m = dict(shared)
        m["x_own"] = f(x[b, half * T:(half + 1) * T, :])
        m["x_pre"] = f(x[b, 0:T, :])
        m["c_l"] = f(inp["c"][b].reshape(16, 128).T)
        m["flag"] = np.full((128, 1), float(half), np.float32)
        ic = np.zeros((128, 4, 16), np.float32)
        for g in range(4):
            w = 2 ** (g + 1)
            pos = np.arange(1, 17, dtype=np.float32)
            ic[:, g, :] = (1.0 / np.minimum(pos, w) if half == 0 else np.full(16, 1.0 / w, np.float32))[None, :]
        m["invcnt"] = ic
        maps.append(m)
    return maps


N_GROUPS = 4


def kernel(**inputs):
    nc = build_program(n_groups=N_GROUPS)
    in_maps = make_in_maps(inputs)
    res = run_bass_kernel_spmd(nc, in_maps, core_ids=list(range(8)))
    outs = [res.results[i]["out"] for i in range(8)]
    full = np.stack([np.concatenate([outs[2 * b], outs[2 * b + 1]], axis=0) for b in range(4)], axis=0)
    return full.astype(np.float32)
```

```python
from contextlib import ExitStack
import numpy as np
import concourse.bass as bass
import concourse.mybir as mybir
from concourse.bass_utils import run_bass_kernel_spmd

F32 = mybir.dt.float32
BF16 = mybir.dt.bfloat16
I32 = mybir.dt.int32
U32 = mybir.dt.uint32
AF = mybir.ActivationFunctionType
ALU = mybir.AluOpType
AX = mybir.AxisListType

D = 2048
T = 2048
SEG = 512
NSEG = T // SEG
NE = 32
CAP = 2048
GRP = 512
ALPHA = 2.0 ** 0.25
LN_EPS = 1e-5
IN_W = 4112


class _Stop(Exception):
    pass


class Sched:
    def __init__(self, nc, stack):
        self.nc = nc
        self.eng = {"pe": nc.tensor, "act": nc.scalar, "dve": nc.vector,
                    "pool": nc.gpsimd, "sp": nc.sync}
        self.sem = {}
        self.cnt = {}
        self.stack = stack
        for e in self.eng:
            self.sem[e] = stack.enter_context(nc.semaphore("prog_" + e))
            self.cnt[e] = 0
        self.dsem = {}
        self.dcnt = {}
        self.dq = {}
        self.seen = {e: {} for e in self.eng}
        self.lastw = {}
        self.readers = {}
        self.n_wait = 0
        self.n_inst = 0

    def _chan(self, key):
        if key not in self.dsem:
            self.dsem[key] = self.stack.enter_context(
                self.nc.semaphore("d_" + str(len(self.dsem))))
            self.dcnt[key] = 0
        return self.dsem[key]

    def _wait(self, e, tok):
        kind, k, c = tok
        semkey = (kind, k)
        if self.seen[e].get(semkey, 0) >= c:
            return
        sem = self.sem[k] if kind == "eng" else self.dsem[k]
        self.eng[e].wait_ge(sem, c)
        self.seen[e][semkey] = c
        self.n_wait += 1

    def _deps(self, e, reads, writes, skip_same=False):
        toks = []
        for r in reads:
            w = self.lastw.get(r)
            if w is not None:
                toks.append(w)
        for w_ in writes:
            w = self.lastw.get(w_)
            if w is not None:
                toks.append(w)
            toks.extend(self.readers.get(w_, []))
        for t in toks:
            if skip_same and t[0] == "eng" and t[1] == e:
                continue
            self._wait(e, t)

    def _record(self, tok, reads, writes):
        for r in reads:
            self.readers.setdefault(r, []).append(tok)
        for w in writes:
            self.lastw[w] = tok
            self.readers[w] = []

    def op(self, e, fn, reads=(), writes=()):
        self._deps(e, reads, writes, skip_same=(e == "pe"))
        ins = fn(self.eng[e])
        self.cnt[e] += 1
        ins.then_inc(self.sem[e], 1)
        self._record(("eng", e, self.cnt[e]), reads, writes)
        self.n_inst += 1
        return ins

    def dma(self, q, out, in_, chan, reads=(), writes=(), indirect=None, **kw):
        self._deps(q, reads, writes)
        sem = self._chan(chan)
        if indirect is None:
            ins = self.eng[q].dma_start(out=out, in_=in_, **kw)
        else:
            ins = self.eng[q].indirect_dma_start(out=out, in_=in_, **indirect)
        self.dcnt[chan] += 16
        self.dq[chan] = q
        ins.then_inc(sem, 16)
        self._record(("dma", chan, self.dcnt[chan]), reads, writes)
        self.n_inst += 1
        return ins

    def barrier(self):
        for e in self.eng:
            for o in self.eng:
                if self.cnt[o] > 0:
                    self._wait(e, ("eng", o, self.cnt[o]))
            for k, c in self.dcnt.items():
                if c > 0:
                    self._wait(e, ("dma", k, c))

    def finish(self, e="sp"):
        for k, w in list(self.lastw.items()):
            if w is not None:
                self._wait(e, w)
            for r in self.readers.get(k, []):
                self._wait(e, r)


def build_program(debug=False, n_groups=4, stop_after=None):
    nc = bass.Bass("TRN2", target_bir_lowering=False)

    in_names = []
    lean = stop_after in ("A0", "A1", "A1s", "A1a", "A1b", "A1c", "A1d", "A2")

    def din(name, shape, dt=F32):
        if lean and name in ("w_gate", "w_up", "w_down"):
            return None
        in_names.append(name)
        return nc.dram_tensor(name, list(shape), dt, kind="ExternalInput").ap()

    x_own = din("x_own", [T, D])
    x_pre = din("x_pre", [T, D])
    c_l = din("c_l", [128, 16])
    flag = din("flag", [128, 1])
    invcnt = din("invcnt", [128, 4, 16])
    ident_f = din("ident_f", [128, 128])
    triu2 = din("triu2", [128, 128])
    rmask = din("rmask", [128, SEG])
    ltri = din("ltri", [128, 128])
    iota_e = din("iota_e", [128, NE])
    tokid = din("tokid", [128, 16], I32)
    iota_n = din("iota_n", [128, NE])
    list_init = din("list_init", [NE * CAP, 2], I32)
    w_ada = din("w_ada", [D, 6 * D])
    b_ada = din("b_ada", [1, 6 * D])
    w_in = din("w_in", [D, IN_W])
    w_gk = din("w_gk", [16, 512])
    b_gk = din("b_gk", [128, 4])
    w_pool = din("w_pool", [4, 256, 256])
    b_pool = din("b_pool", [128, 8])
    pool_scale = din("pool_scale", [128, 8])
    gla_norm_w = din("gla_norm_w", [1, 256])
    w_out = din("w_out", [D, D])
    ln1_g = din("ln1_g", [1, D])
    ln1_b = din("ln1_b", [1, D])
    w_router = din("w_router", [D, NE])
    b_router = din("b_router", [1, NE])
    w_gate = din("w_gate", [NE, D, D])
    b_gate = din("b_gate", [NE, 128, 16])
    w_up = din("w_up", [NE, D, D])
    b_up = din("b_up", [NE, 128, 16])
    w_down = din("w_down", [NE, D, D])
    b_down = din("b_down", [NE, D])
    ln2_g = din("ln2_g", [1, D])
    ln2_b = din("ln2_b", [1, D])

    out = nc.dram_tensor("out", [T, D], F32, kind="ExternalOutput").ap()

    def dscratch(name, shape, dt):
        return nc.dram_tensor(name, list(shape), dt, kind="Internal").ap()

    ybufT = dscratch("ybufT", [16, 128, T], BF16)
    x1buf = dscratch("x1buf", [T, D], F32)
    h2buf = dscratch("h2buf", [T + 128, D], BF16)
    lists = dscratch("lists", [NE * CAP, 2], I32)
    yacc = dscratch("yacc", [T + 128, D], F32)
    moddram = dscratch("moddram", [1, 6 * D], F32)
    dbg = None
    if debug:
        dbg = {
            "x1": nc.dram_tensor("dbg_x1", [T, D], F32, kind="ExternalOutput").ap(),
            "yT": nc.dram_tensor("dbg_yT", [16, 128, T], BF16, kind="ExternalOutput").ap(),
            "lg": nc.dram_tensor("dbg_lg", [T, NE], F32, kind="ExternalOutput").ap(),
        }

    nc.in_names = in_names
    with ExitStack() as st:
        S = Sched(nc, st)

        def sb(name, shape, dt=F32):
            return st.enter_context(nc.sbuf_tensor(name, list(shape), dt))

        def pst(name, shape, dt=F32):
            return st.enter_context(nc.psum_tensor(name, list(shape), dt))

        ident32 = sb("ident32", [128, 128])
        identb = sb("identb", [128, 128], BF16)
        triu_sb = sb("triu_sb", [128, 128])
        rmask_sb = sb("rmask_sb", [128, SEG])
        flag_sb = sb("flag_sb", [128, 1])
        invc_sb = sb("invc_sb", [128, 4, 16])
        ones_row = sb("ones_row", [1, 128])
        one11 = sb("one11", [1, 1])
        eps_sb = sb("eps_sb", [128, 1])
        S.dma("sp", ident32[:], ident_f, chan="ident32", writes=["ident32"])
        S.dma("pool", identb[:], ident_f, chan="identb", writes=["identb"])
        S.dma("sp", triu_sb[:], triu2, chan="triu", writes=["triu"])
        S.dma("sp", rmask_sb[:], rmask, chan="rmask", writes=["rmask"])
        S.dma("sp", flag_sb[:], flag, chan="flag", writes=["flag"])
        S.dma("sp", invc_sb[:], invcnt, chan="invc", writes=["invc"])
        S.op("dve", lambda e: e.memset(ones_row[:], 1.0), writes=["ones_row"])
        S.op("dve", lambda e: e.memset(one11[:], 1.0), writes=["one11"])
        S.op("dve", lambda e: e.memset(eps_sb[:], LN_EPS), writes=["eps"])

        PG = [pst("pg%d" % i, [128, 512]) for i in range(2)]
        PTS = [pst("ptr%d" % i, [128, 1024], BF16) for i in range(2)]
        PSC = pst("psc", [128, 512])
        PO = [pst("po%d" % i, [128, 512]) for i in range(2)]
        PS_ = pst("pss", [128, 512])
        pg_i = [0]

        def next_pg():
            i = pg_i[0] % 2
            pg_i[0] += 1
            return PG[i], "pg%d" % i

        flg_i = sb("flg_i", [1, 4 * NE], I32)
        cur = [st]

        def sb(name, shape, dt=F32):
            return cur[0].enter_context(nc.sbuf_tensor(name, list(shape), dt))

        ph = ExitStack()
        cur[0] = ph
        c_sb = sb("c_sb", [128, 16])
        sc_sb = sb("sc_sb", [128, 16])
        S.dma("sp", c_sb[:], c_l, chan="c_sb", writes=["c_sb"])
        S.op("act", lambda e: e.activation(out=sc_sb[:], in_=c_sb[:], func=AF.Silu),
             reads=["c_sb"], writes=["sc_sb"])
        WF = [sb("wf%d" % i, [128, 16, 512]) for i in range(2)]
        brow = [sb("brow%d" % i, [1, 512]) for i in range(2)]
        mrow = [sb("mrow%d" % i, [1, 512]) for i in range(2)]
        for j in range(24):
            i = j % 2
            wk = "wf%d" % i
            S.dma("sp", WF[i][:], w_ada[:, j * 512:(j + 1) * 512].rearrange("(kc p) n -> p kc n", p=128),
                  chan=wk, writes=[wk])
            S.dma("sp", brow[i][:], b_ada[0:1, j * 512:(j + 1) * 512], chan="brow%d" % i, writes=["brow%d" % i])
            ps, pk = next_pg()
            for kc in range(16):
                S.op("pe", lambda e, kc=kc, i=i, ps=ps: e.matmul(ps[0:1, :], lhsT=sc_sb[:, kc:kc + 1], rhs=WF[i][:, kc, :],
                                                                 start=(kc == 0), stop=(kc == 15)),
                     reads=[wk, "sc_sb"], writes=[pk])
            S.op("dve", lambda e, i=i, ps=ps: e.tensor_tensor(out=mrow[i][:], in0=ps[0:1, :], in1=brow[i][:], op=ALU.add),
                 reads=[pk, "brow%d" % i], writes=["mrow%d" % i])
            S.dma("sp", moddram[0:1, j * 512:(j + 1) * 512], mrow[i][:], chan="mrow%d" % i, reads=["mrow%d" % i],
                  writes=["moddram"])
        S.barrier()
        ph.close()
        if stop_after == "A0":
            print("sems", len(S.dsem), "inst", S.n_inst, "waits", S.n_wait)
            return nc

        ph = ExitStack()
        cur[0] = ph
        sh1_fm = sb("sh1_fm", [128, 16])
        sc1p_fm = sb("sc1p_fm", [128, 16])
        with nc.allow_non_contiguous_dma(reason="tiny strided vector load"):
            S.dma("sp", sh1_fm[:], moddram[0, 0:D].rearrange("(kc p) -> p kc", p=128), chan="sh1_fm",
                  reads=["moddram"], writes=["sh1_fm"])
            S.dma("sp", sc1p_fm[:], moddram[0, D:2 * D].rearrange("(kc p) -> p kc", p=128), chan="sc1p_fm",
                  reads=["moddram"], writes=["sc1p_0"])
        S.op("dve", lambda e: e.tensor_scalar(out=sc1p_fm[:], in0=sc1p_fm[:], scalar1=1.0, scalar2=None, op0=ALU.add),
             reads=["sc1p_0"], writes=["sc1p_fm"])
        wgkin = sb("wgkin", [128, 16, 16], BF16)
        S.dma("pool", wgkin[:], w_in[:, 3072:3088].rearrange("(kc p) n -> p kc n", p=128),
              chan="wgkin", writes=["wgkin"])
        wgk_sb = sb("wgk_sb", [16, 512])
        S.dma("sp", wgk_sb[:], w_gk, chan="wgk", writes=["wgk"])
        nbgk = sb("nbgk", [128, 4])
        S.dma("sp", nbgk[:], b_gk, chan="nbgk", writes=["nbgk0"])
        S.op("dve", lambda e: e.tensor_scalar(out=nbgk[:], in0=nbgk[:], scalar1=-1.0, scalar2=None, op0=ALU.mult),
             reads=["nbgk0"], writes=["nbgk"])
        wpool_sb = sb("wpool_sb", [128, 4, 2, 256], BF16)
        S.dma("pool", wpool_sb[:], w_pool.rearrange("g (cc p) d -> p g cc d", p=128),
              chan="wpool", writes=["wpool"])
        bpool_sb = sb("bpool_sb", [128, 8])
        pscale_sb = sb("pscale_sb", [128, 8])
        S.dma("sp", bpool_sb[:], b_pool, chan="bpool", writes=["bpool"])
        S.dma("sp", pscale_sb[:], pool_scale, chan="pscale", writes=["pscale"])
        nw_row = sb("nw_row", [1, 256])
        S.dma("sp", nw_row[:], gla_norm_w, chan="nw_row", writes=["nw_row"])
        nw_bc = sb("nw_bc", [128, 256])
        ps, pk = next_pg()
        S.op("pe", lambda e: e.matmul(ps[:, 0:256], lhsT=ones_row[0:1, :], rhs=nw_row[0:1, :], start=True, stop=True),
             reads=["nw_row", "ones_row"], writes=[pk])
        S.op("act", lambda e: e.activation(out=nw_bc[:], in_=ps[:, 0:256], func=AF.Copy), reads=[pk], writes=["nw_bc"])
        lnq_sb = sb("lnq_sb", [128, 1])
        S.op("dve", lambda e: e.memset(lnq_sb[:], float(np.log(128.0 ** -0.5))), writes=["lnq"])
        rms_eps = sb("rms_eps", [128, 1])
        S.op("dve", lambda e: e.memset(rms_eps[:], 1e-6), writes=["rms_eps"])

        if stop_after == "A1s":
            S.barrier()
            ph.close()
            return nc
        WB = [sb("wb%d" % i, [128, 16, 512], BF16) for i in range(3)]
        wb_i = [0]

        def load_w(src_ap):
            i = wb_i[0] % 3
            wb_i[0] += 1
            k = "wb%d" % i
            for h in range(2):
                S.dma("pool", WB[i][:, h * 8:(h + 1) * 8, :],
                      src_ap[h * 1024:(h + 1) * 1024, :].rearrange("(kc p) n -> p kc n", p=128),
                      chan=k, writes=[k])
            return WB[i], k

        XT = [sb("xt%d" % i, [128, D]) for i in range(2)]
        xt_i = [0]
        xn = sb("xn", [128, D], BF16)
        stats = sb("stats", [128, 4, 6])
        mv = sb("mv", [128, 2])
        rstd = sb("rstd", [128, 1])
        hT = sb("hT", [128, 16, SEG], BF16)
        uT = sb("uT", [128, 8, 16 + SEG])
        sA = sb("sA", [128, 2, 16 + SEG])
        sB = sb("sB", [128, 2, 16 + SEG])
        pooled = sb("pooled", [128, 2, SEG], BF16)
        fixs = sb("fixs", [128, 2, 16])
        qtil = sb("qtil", [128, 4, SEG], BF16)
        ktil = sb("ktil", [128, 4, SEG], BF16)
        khatT = sb("khatT", [128, 4, SEG], BF16)
        khat_tm = sb("khat_tm", [128, 4, 4, 128], BF16)
        v_tm = sb("v_tm", [128, 4, 1024], BF16)
        sg_tm = sb("sg_tm", [128, 4, 1024], BF16)
        gkT = sb("gkT", [16, SEG])
        spl = sb("spl", [128, SEG])
        cs = sb("cs", [128, 4, SEG])
        eq = sb("eq", [128, SEG])
        enb = sb("enb", [128, SEG])
        ehat = sb("ehat", [128, SEG])
        eL = sb("eL", [128, 4, 8])
        state32 = sb("state32", [128, 4, 256])
        stateb = sb("stateb", [128, 4, 256], BF16)
        scm = sb("scm", [128, 2, 128], BF16)
        yB = sb("yB", [128, 1024], BF16)
        nwsg = sb("nwsg", [128, 256])
        ssq = sb("ssq", [128, 1])
        rr = sb("rr", [128, 1])
        osq = sb("osq", [128, 256])
        yT = sb("yT", [128, 16, SEG], BF16)
        S.op("pool", lambda e: e.memset(state32[:], 0.0), writes=[("state32", h) for h in range(4)])
        S.op("pool", lambda e: e.memset(stateb[:], 0.0), writes=[("stateb", h) for h in range(4)])
        S.op("pool", lambda e: e.memset(uT[:], 0.0), writes=[("uT", ch) for ch in range(8)])
        S.op("pool", lambda e: e.memset(sA[:], 0.0), writes=["sA"])
        S.op("pool", lambda e: e.memset(sB[:], 0.0), writes=["sB"])

        def layer_norm_stats(xt, xk):
            for j in range(4):
                S.op("dve", lambda e, j=j: e.bn_stats(out=stats[:, j, :], in_=xt[:, j * 512:(j + 1) * 512]),
                     reads=[xk], writes=[("stats", j)])
            S.op("dve", lambda e: e.bn_aggr(out=mv[:], in_=stats[:].rearrange("p a b -> p (a b)")),
                 reads=[("stats", j) for j in range(4)], writes=["mv"])
            S.op("act", lambda e: e.activation(out=rstd[:], in_=mv[:, 1:2], func=AF.Sqrt, bias=eps_sb[:, 0:1], scale=1.0),
                 reads=["mv", "eps"], writes=["rstd0"])
            S.op("dve", lambda e: e.reciprocal(out=rstd[:], in_=rstd[:]), reads=["rstd0"], writes=["rstd"])

        def mixer_segment(xsrc, seg, own, need_u):
            for tt in range(4):
                i = xt_i[0] % 2
                xt_i[0] += 1
                xk = "xt%d" % i
                r0 = seg * SEG + tt * 128
                S.dma("sp", XT[i][:], xsrc[r0:r0 + 128, :], chan=xk, writes=[xk])
                layer_norm_stats(XT[i], xk)
                S.op("dve", lambda e, i=i: e.tensor_scalar(out=xn[:], in0=XT[i][:], scalar1=mv[:, 0:1], scalar2=rstd[:, 0:1],
                                                           op0=ALU.subtract, op1=ALU.mult),
                     reads=[xk, "mv", "rstd"], writes=["xn"])
                for half in range(2):
                    PT = PTS[half]
                    ptk = "pt%d" % half
                    for j in range(8):
                        kc = half * 8 + j
                        S.op("pe", lambda e, kc=kc, j=j, PT=PT: e.transpose(PT[:, j * 128:(j + 1) * 128], xn[:, kc * 128:(kc + 1) * 128], identb[:]),
                             reads=["xn", "identb"], writes=[ptk])
                    for j in range(8):
                        kc = half * 8 + j
                        eng = "act" if j % 2 == 0 else "dve"
                        if eng == "act":
                            S.op("act", lambda e, kc=kc, j=j, tt=tt, PT=PT: e.activation(
                                out=hT[:, kc, tt * 128:(tt + 1) * 128], in_=PT[:, j * 128:(j + 1) * 128], func=AF.Identity,
                                bias=sh1_fm[:, kc:kc + 1], scale=sc1p_fm[:, kc:kc + 1]),
                                reads=[ptk, "sh1_fm", "sc1p_fm"], writes=[("hT", tt)])
                        else:
                            S.op("dve", lambda e, kc=kc, j=j, tt=tt, PT=PT: e.tensor_scalar(
                                out=hT[:, kc, tt * 128:(tt + 1) * 128], in0=PT[:, j * 128:(j + 1) * 128],
                                scalar1=sc1p_fm[:, kc:kc + 1], scalar2=sh1_fm[:, kc:kc + 1], op0=ALU.mult, op1=ALU.add),
                                reads=[ptk, "sh1_fm", "sc1p_fm"], writes=[("hT", tt)])
            hkeys = [("hT", tt) for tt in range(4)]
            if stop_after == "A1a":
                raise _Stop()

            def fm_group(wt, wk, m, evac):
                ps, pk = next_pg()
                for kc in range(16):
                    S.op("pe", lambda e, kc=kc: e.matmul(ps[:, :], lhsT=wt[:, kc, m * 128:(m + 1) * 128], rhs=hT[:, kc, :],
                                                         start=(kc == 0), stop=(kc == 15)),
                         reads=[wk] + hkeys, writes=[pk])
                evac(ps, pk)

            def tm_group(wt, wk, tt, evac):
                ps, pk = next_pg()
                for kc in range(16):
                    S.op("pe", lambda e, kc=kc: e.matmul(ps[:, :], lhsT=hT[:, kc, tt * 128:(tt + 1) * 128], rhs=wt[:, kc, :],
                                                         start=(kc == 0), stop=(kc == 15)),
                         reads=[wk, ("hT", tt)], writes=[pk])
                evac(ps, pk)

            ps, pk = next_pg()
            for kc in range(16):
                S.op("pe", lambda e, kc=kc: e.matmul(ps[0:16, :], lhsT=wgkin[:, kc, :], rhs=hT[:, kc, :],
                                                     start=(kc == 0), stop=(kc == 15)),
                     reads=["wgkin"] + hkeys, writes=[pk])
            S.op("act", lambda e: e.activation(out=gkT[:], in_=ps[0:16, :], func=AF.Copy), reads=[pk], writes=["gkT"])
            for h in range(4):
                ps, pk = next_pg()
                S.op("pe", lambda e, h=h: e.matmul(ps[:, :], lhsT=wgk_sb[:, h * 128:(h + 1) * 128], rhs=gkT[:, :],
                                                   start=True, stop=True), reads=["wgk", "gkT"], writes=[pk])
                S.op("act", lambda e, h=h: e.activation(out=spl[:], in_=ps[:, :], func=AF.Exp, bias=nbgk[:, h:h + 1], scale=-1.0),
                     reads=[pk, "nbgk"], writes=["spl"])
                S.op("act", lambda e: e.activation(out=spl[:], in_=spl[:], func=AF.Ln, bias=1.0, scale=1.0),
                     reads=["spl"], writes=["spl"])
                S.op("dve", lambda e, h=h: e.tensor_tensor_scan(out=cs[:, h, :], data0=rmask_sb[:], data1=spl[:], initial=0.0,
                                                                op0=ALU.mult, op1=ALU.add),
                     reads=["spl", "rmask"], writes=[("cs", h)])
            if stop_after == "A1b":
                raise _Stop()
            wt, wk = load_w(w_in[:, 1536:2048])
            for h in range(4):
                S.op("act", lambda e, h=h: e.activation(out=enb[:], in_=cs[:, h, :], func=AF.Exp, scale=1.0 / 16.0),
                     reads=[("cs", h)], writes=["enb"])
                S.op("act", lambda e, h=h: e.activation(
                    out=eL[:, h, :], in_=cs[:, h, :].rearrange("p (c t) -> p c t", t=64)[:, :, 63], func=AF.Exp, scale=-1.0 / 16.0),
                    reads=[("cs", h)], writes=[("eL", h)])
                S.op("dve", lambda e, h=h: e.tensor_tensor(
                    out=ehat[:].rearrange("p (c t) -> p c t", t=64), in0=enb[:].rearrange("p (c t) -> p c t", t=64),
                    in1=eL[:, h, :].unsqueeze(2).broadcast_to([128, 8, 64]), op=ALU.mult),
                    reads=["enb", ("eL", h)], writes=["ehat"])

                def evac_k(ps, pk, h=h):
                    if own:
                        S.op("dve", lambda e: e.tensor_tensor(out=ktil[:, h, :], in0=ps[:, :], in1=enb[:], op=ALU.mult),
                             reads=[pk, "enb"], writes=[("ktil", h)])
                    S.op("dve", lambda e: e.tensor_tensor(out=khatT[:, h, :], in0=ps[:, :], in1=ehat[:], op=ALU.mult),
                         reads=[pk, "ehat"], writes=[("khatT", h)])
                fm_group(wt, wk, h, evac_k)
            if stop_after == "A1c":
                raise _Stop()
            for tt in range(4):
                PT = PTS[tt % 2]
                ptk = "pt%d" % (tt % 2)
                for h in range(4):
                    S.op("pe", lambda e, tt=tt, h=h, PT=PT: e.transpose(PT[:, h * 128:(h + 1) * 128], khatT[:, h, tt * 128:(tt + 1) * 128], identb[:]),
                         reads=[("khatT", h), "identb"], writes=[ptk])
                S.op("act", lambda e, tt=tt, PT=PT: e.activation(out=khat_tm[:, tt, :, :], in_=PT[:, 0:512].rearrange("p (h d) -> p h d", h=4),
                                                          func=AF.Copy),
                     reads=[ptk], writes=[("khat_tm", tt)])
            for half in range(2):
                wt, wk = load_w(w_in[:, 2048 + half * 512: 2048 + (half + 1) * 512])
                for tt in range(4):
                    def evac_v(ps, pk, tt=tt, half=half):
                        S.op("act", lambda e: e.activation(out=v_tm[:, tt, half * 512:(half + 1) * 512], in_=ps[:, :], func=AF.Copy),
                             reads=[pk], writes=[("v_tm", tt)])
                    tm_group(wt, wk, tt, evac_v)
            if own:
                wt, wk = load_w(w_in[:, 1024:1536])
                for h in range(4):
                    S.op("act", lambda e, h=h: e.activation(out=eq[:], in_=cs[:, h, :], func=AF.Exp, scale=-1.0 / 16.0, bias=lnq_sb[:, 0:1]),
                         reads=[("cs", h), "lnq"], writes=["eq"])

                    def evac_q(ps, pk, h=h):
                        S.op("dve", lambda e: e.tensor_tensor(out=qtil[:, h, :], in0=ps[:, :], in1=eq[:], op=ALU.mult),
                             reads=[pk, "eq"], writes=[("qtil", h)])
                    fm_group(wt, wk, h, evac_q)
                for half in range(2):
                    wt, wk = load_w(w_in[:, 3088 + half * 512: 3088 + (half + 1) * 512])
                    for tt in range(4):
                        def evac_g(ps, pk, tt=tt, half=half):
                            S.op("act", lambda e: e.activation(out=sg_tm[:, tt, half * 512:(half + 1) * 512], in_=ps[:, :], func=AF.Silu),
                                 reads=[pk], writes=[("sg_tm", tt)])
                        tm_group(wt, wk, tt, evac_g)
            if need_u:
                for half in range(2):
                    wt, wk = load_w(w_in[:, half * 512:(half + 1) * 512])
                    for m in range(4):
                        def evac_u(ps, pk, ch=half * 4 + m):
                            S.op("act", lambda e: e.activation(out=uT[:, ch, 16:16 + SEG], in_=ps[:, :], func=AF.Copy),
                                 reads=[pk], writes=[("uT", ch)])
                        fm_group(wt, wk, m, evac_u)
            if own and seg == 0:
                S.op("dve", lambda e: e.tensor_scalar(out=state32[:].rearrange("p a b -> p (a b)"), in0=state32[:].rearrange("p a b -> p (a b)"),
                                                      scalar1=flag_sb[:, 0:1], scalar2=None, op0=ALU.mult),
                     reads=[("state32", h) for h in range(4)] + ["flag"], writes=[("state32", h) for h in range(4)])
                S.op("dve", lambda e: e.tensor_scalar(out=stateb[:].rearrange("p a b -> p (a b)"), in0=state32[:].rearrange("p a b -> p (a b)"),
                                                      scalar1=1.0, scalar2=None, op0=ALU.mult),
                     reads=[("state32", h) for h in range(4)], writes=[("stateb", h) for h in range(4)])
                for ch in range(8):
                    S.op("pool", lambda e, ch=ch: e.tensor_scalar(out=uT[:, ch, 0:16], in0=uT[:, ch, 0:16], scalar1=flag_sb[:, 0:1], scalar2=None,
                                                                  op0=ALU.mult),
                         reads=[("uT", ch), "flag"], writes=[("uT", ch)])
            if own:
                for g in range(4):
                    w = 2 ** (g + 1)
                    chs = [("uT", 2 * g), ("uT", 2 * g + 1)]
                    src = uT[:, 2 * g:2 * g + 2, :]
                    srck = chs
                    bufs = [(sA, "sA"), (sB, "sB")]
                    bi = 0
                    step = 1
                    L = 16 + SEG
                    while step < w:
                        dst, dk = bufs[bi]
                        S.op("pool", lambda e, src=src, dst=dst, step=step: e.tensor_tensor(
                            out=dst[:, :, step:L], in0=src[:, :, step:L], in1=src[:, :, 0:L - step], op=ALU.add),
                            reads=srck, writes=[dk])
                        src, srck = dst[:, :, :], [dk]
                        bi ^= 1
                        step *= 2
                    S.op("dve", lambda e, src=src, g=g, w=w: e.scalar_tensor_tensor(
                        out=pooled[:, :, :], in0=src[:, :, 16:16 + SEG], scalar=1.0 / w, in1=uT[:, 2 * g:2 * g + 2, 16:16 + SEG],
                        op0=ALU.mult, op1=ALU.subtract), reads=srck + chs, writes=["pooled"])
                    if seg == 0:
                        for cc in range(2):
                            S.op("dve", lambda e, src=src, g=g, cc=cc: e.tensor_tensor(
                                out=fixs[:, cc, :], in0=src[:, cc, 16:32], in1=invc_sb[:, g, :], op=ALU.mult),
                                reads=srck + ["invc"], writes=["fixs"])
                            S.op("dve", lambda e, g=g, cc=cc: e.tensor_tensor(
                                out=pooled[:, cc, 0:16], in0=fixs[:, cc, :], in1=uT[:, 2 * g + cc, 16:32], op=ALU.subtract),
                                reads=["fixs"] + chs, writes=["pooled"])
                    for dh in range(2):
                        ps, pk = next_pg()
                        for cc in range(2):
                            S.op("pe", lambda e, g=g, cc=cc, dh=dh: e.matmul(ps[:, :], lhsT=wpool_sb[:, g, cc, dh * 128:(dh + 1) * 128],
                                                                           rhs=pooled[:, cc, :], start=(cc == 0), stop=(cc == 1)),
                                 reads=["wpool", "pooled"], writes=[pk])
                        ch = 2 * g + dh
                        S.op("dve", lambda e, ch=ch, ps=ps: e.tensor_scalar(out=yT[:, ch, :], in0=ps[:, :], scalar1=bpool_sb[:, ch:ch + 1],
                                                                            scalar2=pscale_sb[:, ch:ch + 1], op0=ALU.add, op1=ALU.mult),
                             reads=[pk, "bpool", "pscale"], writes=[("yT", ch)])
            if need_u:
                for ch in range(8):
                    S.op("pool", lambda e, ch=ch: e.tensor_copy(out=uT[:, ch, 0:16], in_=uT[:, ch, SEG:SEG + 16]),
                         reads=[("uT", ch)], writes=[("uT", ch)])
            if stop_after == "A1d":
                raise _Stop()
            for tt in range(4):
                if own:
                    for h in range(4):
                        sl = slice(tt * 128, (tt + 1) * 128)
                        S.op("pe", lambda e, h=h, sl=sl: e.matmul(PSC[:, (h % 4) * 128:(h % 4 + 1) * 128], lhsT=ktil[:, h, sl], rhs=qtil[:, h, sl],
                                                                  start=True, stop=True),
                             reads=[("ktil", h), ("qtil", h)], writes=["psc"])
                for h in range(4):
                    if own:
                        S.op("dve", lambda e, h=h: e.tensor_tensor(out=scm[:, h % 2, :], in0=PSC[:, h * 128:(h + 1) * 128], in1=triu_sb[:], op=ALU.mult),
                             reads=["psc", "triu"], writes=[("scm", h % 2)])
                        po = PO[h // 2]
                        pok = ("po", h // 2)
                    for c in range(2):
                        rows = slice(c * 64, (c + 1) * 64)
                        cg = tt * 2 + c
                        if own:
                            S.op("pe", lambda e, h=h, c=c, rows=rows, po=po: e.matmul(
                                po[rows, (h % 2) * 256:(h % 2 + 1) * 256], lhsT=scm[rows, h % 2, c * 64:(c + 1) * 64],
                                rhs=v_tm[rows, tt, h * 256:(h + 1) * 256], start=True, stop=False),
                                reads=[("scm", h % 2), ("v_tm", tt)], writes=[pok])
                            S.op("pe", lambda e, h=h, c=c, rows=rows, po=po: e.matmul(
                                po[rows, (h % 2) * 256:(h % 2 + 1) * 256], lhsT=qtil[:, h, tt * 128 + c * 64: tt * 128 + (c + 1) * 64],
                                rhs=stateb[:, h, :], start=False, stop=True),
                                reads=[("qtil", h), ("stateb", h)], writes=[pok])
                        slot = (h * 2 + c) % 2
                        S.op("pe", lambda e, h=h, rows=rows, slot=slot: e.matmul(
                            PS_[:, slot * 256:(slot + 1) * 256], lhsT=khat_tm[rows, tt, h, :], rhs=v_tm[rows, tt, h * 256:(h + 1) * 256],
                            start=True, stop=True),
                            reads=[("khat_tm", tt), ("v_tm", tt)], writes=["pss"])
                        S.op("dve", lambda e, h=h, cg=cg, slot=slot: e.scalar_tensor_tensor(
                            out=state32[:, h, :], in0=state32[:, h, :], scalar=eL[:, h, cg:cg + 1], in1=PS_[:, slot * 256:(slot + 1) * 256],
                            op0=ALU.mult, op1=ALU.add),
                            reads=["pss", ("eL", h), ("state32", h)], writes=[("state32", h)])
                        S.op("act", lambda e, h=h: e.activation(out=stateb[:, h, :], in_=state32[:, h, :], func=AF.Copy),
                             reads=[("state32", h)], writes=[("stateb", h)])
                    if own:
                        S.op("act", lambda e, h=h, po=po: e.activation(out=osq[:], in_=po[:, (h % 2) * 256:(h % 2 + 1) * 256], func=AF.Square,
                                                                       accum_out=ssq[:, 0:1]),
                             reads=[pok], writes=["osq", "ssq"])
                        S.op("act", lambda e: e.activation(out=rr[:], in_=ssq[:], func=AF.Sqrt, bias=rms_eps[:, 0:1], scale=1.0 / 256.0),
                             reads=["ssq", "rms_eps"], writes=["rr0"])
                        S.op("dve", lambda e: e.reciprocal(out=rr[:], in_=rr[:]), reads=["rr0"], writes=["rr"])
                        S.op("dve", lambda e, h=h, tt=tt: e.tensor_tensor(out=nwsg[:], in0=nw_bc[:], in1=sg_tm[:, tt, h * 256:(h + 1) * 256], op=ALU.mult),
                             reads=["nw_bc", ("sg_tm", tt)], writes=["nwsg"])
                        S.op("dve", lambda e, h=h, po=po: e.scalar_tensor_tensor(
                            out=yB[:, h * 256:(h + 1) * 256], in0=po[:, (h % 2) * 256:(h % 2 + 1) * 256], scalar=rr[:, 0:1], in1=nwsg[:],
                            op0=ALU.mult, op1=ALU.mult), reads=[pok, "rr", "nwsg"], writes=[("yB", h)])
                if own:
                    PT = PTS[tt % 2]
                    ptk = "pt%d" % (tt % 2)
                    for j in range(8):
                        S.op("pe", lambda e, j=j, PT=PT: e.transpose(PT[:, j * 128:(j + 1) * 128], yB[:, j * 128:(j + 1) * 128], identb[:]),
                             reads=[("yB", j // 2), "identb"], writes=[ptk])
                    S.op("act", lambda e, tt=tt, PT=PT: e.activation(out=yT[:, 8:16, tt * 128:(tt + 1) * 128],
                                                              in_=PT[:, :].rearrange("p (j t) -> p j t", j=8), func=AF.Copy),
                         reads=[ptk], writes=[("yTb", tt)])
            if own:
                ykeys = [("yT", ch) for ch in range(8)] + [("yTb", tt) for tt in range(4)]
                S.dma("sp", ybufT[:, :, seg * SEG:(seg + 1) * SEG].rearrange("c p t -> p c t"), yT[:], chan="yT",
                      reads=ykeys, writes=[("ybufT", seg)])

        try:
            for seg in range(NSEG):
                mixer_segment(x_pre, seg, own=False, need_u=(seg == NSEG - 1))
            for seg in range(NSEG):
                mixer_segment(x_own, seg, own=True, need_u=True)
        except _Stop:
            S.barrier()
            ph.close()
            print("sems", len(S.dsem), "inst", S.n_inst, "waits", S.n_wait)
            return nc

        if debug:
            for seg in range(NSEG):
                S.dma("sp", yT[:], ybufT[:, :, seg * SEG:(seg + 1) * SEG].rearrange("c p t -> p c t"), chan="yT",
                      reads=[("ybufT", seg)], writes=["yT_dbg"] + [("yT", ch) for ch in range(8)] + [("yTb", tt) for tt in range(4)])
                S.dma("sp", dbg["yT"][:, :, seg * SEG:(seg + 1) * SEG].rearrange("c p t -> p c t"), yT[:], chan="yT",
                      reads=["yT_dbg"], writes=["dbg_yT"])
        S.barrier()
        ph.close()
        if stop_after == "A1":
            print("sems", len(S.dsem), "inst", S.n_inst, "waits", S.n_wait)
            return nc
        ph = ExitStack()
        cur[0] = ph
        PB = [PG[0], PG[1], PSC, PO[0], PO[1], PS_]
        PBK = ["pg0", "pg1", "psc", ("po", 0), ("po", 1), "pss"]
        pb_i = [0]

        def next_pb():
            i = pb_i[0] % len(PB)
            pb_i[0] += 1
            return PB[i], PBK[i]

        wout = sb("wout", [128, 16, D], BF16)
        for j in range(4):
            for h in range(2):
                S.dma("pool", wout[:, h * 8:(h + 1) * 8, j * 512:(j + 1) * 512],
                      w_out[h * 1024:(h + 1) * 1024, j * 512:(j + 1) * 512].rearrange("(kc p) n -> p kc n", p=128),
                      chan="wout", writes=["wout"])

        def bc_tile(name, src_row):
            t = sb(name, [128, D])
            S.dma("sp", t[:], src_row.partition_broadcast(128), chan=name, reads=["moddram"], writes=[name])
            return t
        g1_bc = bc_tile("g1_bc", moddram[0, 2 * D:3 * D])
        sh2_bc = bc_tile("sh2_bc", moddram[0, 3 * D:4 * D])
        sc2p_bc = bc_tile("sc2p_bc", moddram[0, 4 * D:5 * D])
        S.op("pool", lambda e: e.tensor_scalar(out=sc2p_bc[:], in0=sc2p_bc[:], scalar1=1.0, scalar2=None, op0=ALU.add),
             reads=["sc2p_bc"], writes=["sc2p_bc"])
        l1g_bc = bc_tile("l1g_bc", ln1_g[0, :])
        l1b_bc = bc_tile("l1b_bc", ln1_b[0, :])
        wr_sb = sb("wr_sb", [128, 16, NE])
        S.dma("sp", wr_sb[:], w_router.rearrange("(kc p) n -> p kc n", p=128), chan="wr_sb", writes=["wr_sb"])
        br_bc = sb("br_bc", [128, NE])
        S.dma("sp", br_bc[:], b_router[0, :].partition_broadcast(128), chan="br_bc", writes=["br_bc"])
        ltri_sb = sb("ltri_sb", [128, 128])
        S.dma("sp", ltri_sb[:], ltri, chan="ltri", writes=["ltri"])
        ones_sq = sb("ones_sq", [128, 128])
        S.op("pool", lambda e: e.memset(ones_sq[:], 1.0), writes=["ones_sq"])
        iotae_sb = sb("iotae_sb", [128, NE])
        S.dma("sp", iotae_sb[:], iota_e, chan="iotae", writes=["iotae"])
        iota32 = sb("iota32", [128, NE])
        S.dma("sp", iota32[:], iota_n, chan="iota32", writes=["iota32"])
        tokid_sb = sb("tokid_sb", [128, 16], I32)
        S.dma("sp", tokid_sb[:], tokid, chan="tokid", writes=["tokid"])
        carry = sb("carry", [128, NE])
        S.op("pool", lambda e: e.memset(carry[:], 0.0), writes=["carry"])
        zt = sb("zt", [128, D])
        S.op("pool", lambda e: e.memset(zt[:], 0.0), writes=["zt"])
        for r in range(17):
            S.dma("sp", yacc[r * 128:(r + 1) * 128, :], zt[:], chan="zt", reads=["zt"], writes=["yacc"])
        ztb = sb("ztb", [128, D], BF16)
        S.op("pool", lambda e: e.memset(ztb[:], 0.0), writes=["ztb"])
        S.dma("sp", h2buf[T:T + 128, :], ztb[:], chan="ztb", reads=["ztb"], writes=["h2dummy"])
        li_sb = sb("li_sb", [128, 1024], I32)
        S.dma("sp", li_sb[:], list_init.rearrange("(p r) two -> p (r two)", p=128), chan="li_sb", writes=["li_sb"])
        S.dma("sp", lists.rearrange("(p r) two -> p (r two)", p=128), li_sb[:], chan="li_sb", reads=["li_sb"], writes=["lists"])

        YTT = [sb("ytt%d" % i, [128, 16, 128], BF16) for i in range(2)]
        XA = [sb("xa%d" % i, [128, D]) for i in range(2)]
        x1pre = sb("x1pre", [128, D])
        x1t = sb("x1t", [128, D])
        h2t = sb("h2t", [128, D])
        h2b = sb("h2b", [128, D], BF16)
        h2T = sb("h2T", [128, 16, 128])
        stats2 = sb("stats2", [128, 4, 6])
        mv2 = sb("mv2", [128, 2])
        rstd2 = sb("rstd2", [128, 1])
        lg = sb("lg", [128, NE])
        max8 = sb("max8", [128, 8])
        idx8 = sb("idx8", [128, 8], U32)
        idxf = sb("idxf", [128, 8])
        negm = sb("negm", [128, 1])
        ew = sb("ew", [128, 4])
        den = sb("den", [128, 1])
        w4 = sb("w4", [128, 4])
        maskt = sb("maskt", [128, NE])
        slotf = sb("slotf", [128, NE])
        oh = sb("oh", [128, NE])
        sl4 = sb("sl4", [128, 4])
        sl4i = sb("sl4i", [128, 4], I32)
        pairs = sb("pairs", [128, 16, 4, 2], I32)

        def ln_stats(src, skey, st_t, mv_t, rs_t, tag):
            for j in range(4):
                S.op("dve", lambda e, j=j: e.bn_stats(out=st_t[:, j, :], in_=src[:, j * 512:(j + 1) * 512]),
                     reads=[skey], writes=[(tag + "st", j)])
            S.op("dve", lambda e: e.bn_aggr(out=mv_t[:], in_=st_t[:].rearrange("p a b -> p (a b)")),
                 reads=[(tag + "st", j) for j in range(4)], writes=[tag + "mv"])
            S.op("act", lambda e: e.activation(out=rs_t[:], in_=mv_t[:, 1:2], func=AF.Sqrt, bias=eps_sb[:, 0:1], scale=1.0),
                 reads=[tag + "mv", "eps"], writes=[tag + "rs0"])
            S.op("dve", lambda e: e.reciprocal(out=rs_t[:], in_=rs_t[:]), reads=[tag + "rs0"], writes=[tag + "rs"])

        for ti in range(16):
            b2 = ti % 2
            yk, xk = "ytt%d" % b2, "xa%d" % b2
            S.dma("sp", YTT[b2][:], ybufT[:, :, ti * 128:(ti + 1) * 128].rearrange("c p t -> p c t"), chan=yk,
                  reads=[("ybufT", ti // 4)], writes=[yk])
            S.dma("sp", XA[b2][:], x_own[ti * 128:(ti + 1) * 128, :], chan=xk, writes=[xk])
            for j in range(4):
                ps, pk = next_pb()
                for kc in range(16):
                    S.op("pe", lambda e, kc=kc, j=j, ps=ps, b2=b2: e.matmul(ps[:, :], lhsT=YTT[b2][:, kc, :], rhs=wout[:, kc, j * 512:(j + 1) * 512],
                                                                          start=(kc == 0), stop=(kc == 15)),
                         reads=[yk, "wout"], writes=[pk])
                S.op("dve", lambda e, j=j, ps=ps: e.tensor_tensor(out=x1pre[:, j * 512:(j + 1) * 512], in0=ps[:, :], in1=g1_bc[:, j * 512:(j + 1) * 512], op=ALU.mult),
                     reads=[pk, "g1_bc"], writes=[("x1pre", j)])
                S.op("dve", lambda e, j=j, b2=b2: e.scalar_tensor_tensor(out=x1pre[:, j * 512:(j + 1) * 512], in0=XA[b2][:, j * 512:(j + 1) * 512], scalar=ALPHA,
                                                                       in1=x1pre[:, j * 512:(j + 1) * 512], op0=ALU.mult, op1=ALU.add),
                     reads=[xk, ("x1pre", j)], writes=[("x1pre", j)])
            xpk = [("x1pre", j) for j in range(4)]
            for j in range(4):
                S.op("dve", lambda e, j=j: e.bn_stats(out=stats2[:, j, :], in_=x1pre[:, j * 512:(j + 1) * 512]),
                     reads=[("x1pre", j)], writes=[("ast", j)])
            S.op("dve", lambda e: e.bn_aggr(out=mv2[:], in_=stats2[:].rearrange("p a b -> p (a b)")),
                 reads=[("ast", j) for j in range(4)], writes=["amv"])
            S.op("act", lambda e: e.activation(out=rstd2[:], in_=mv2[:, 1:2], func=AF.Sqrt, bias=eps_sb[:, 0:1], scale=1.0),
                 reads=["amv", "eps"], writes=["ars0"])
            S.op("dve", lambda e: e.reciprocal(out=rstd2[:], in_=rstd2[:]), reads=["ars0"], writes=["ars"])
            S.op("dve", lambda e: e.tensor_scalar(out=x1t[:], in0=x1pre[:], scalar1=mv2[:, 0:1], scalar2=rstd2[:, 0:1], op0=ALU.subtract, op1=ALU.mult),
                 reads=xpk + ["amv", "ars"], writes=["x1t"])
            S.op("pool", lambda e: e.tensor_tensor(out=x1t[:], in0=x1t[:], in1=l1g_bc[:], op=ALU.mult), reads=["x1t", "l1g_bc"], writes=["x1t"])
            S.op("pool", lambda e: e.tensor_tensor(out=x1t[:], in0=x1t[:], in1=l1b_bc[:], op=ALU.add), reads=["x1t", "l1b_bc"], writes=["x1t"])
            S.dma("sp", x1buf[ti * 128:(ti + 1) * 128, :], x1t[:], chan="x1t", reads=["x1t"], writes=[("x1buf", ti)])
            ln_stats(x1t, "x1t", stats2, mv2, rstd2, "b")
            S.op("dve", lambda e: e.tensor_scalar(out=h2t[:], in0=x1t[:], scalar1=mv2[:, 0:1], scalar2=rstd2[:, 0:1], op0=ALU.subtract, op1=ALU.mult),
                 reads=["x1t", "bmv", "brs"], writes=["h2t"])
            S.op("pool", lambda e: e.tensor_tensor(out=h2t[:], in0=h2t[:], in1=sc2p_bc[:], op=ALU.mult), reads=["h2t", "sc2p_bc"], writes=["h2t"])
            S.op("pool", lambda e: e.tensor_tensor(out=h2t[:], in0=h2t[:], in1=sh2_bc[:], op=ALU.add), reads=["h2t", "sh2_bc"], writes=["h2t"])
            S.op("act", lambda e: e.activation(out=h2b[:], in_=h2t[:], func=AF.Copy), reads=["h2t"], writes=["h2b"])
            S.dma("sp", h2buf[ti * 128:(ti + 1) * 128, :], h2b[:], chan="h2b", reads=["h2b"], writes=[("h2buf", ti)])
            for q4 in range(4):
                ps, pk = next_pb()
                for j in range(4):
                    kc = q4 * 4 + j
                    S.op("pe", lambda e, kc=kc, j=j, ps=ps: e.transpose(ps[:, j * 128:(j + 1) * 128], h2t[:, kc * 128:(kc + 1) * 128], ident32[:]),
                         reads=["h2t", "ident32"], writes=[pk])
                S.op("act", lambda e, q4=q4, ps=ps: e.activation(out=h2T[:, q4 * 4:(q4 + 1) * 4, :], in_=ps[:, :].rearrange("p (j t) -> p j t", j=4), func=AF.Copy),
                     reads=[pk], writes=[("h2T", q4)])
            ps, pk = next_pb()
            for kc in range(16):
                S.op("pe", lambda e, kc=kc, ps=ps: e.matmul(ps[:, 0:NE], lhsT=h2T[:, kc, :], rhs=wr_sb[:, kc, :], start=(kc == 0), stop=(kc == 15)),
                     reads=[("h2T", kc // 4), "wr_sb"], writes=[pk])
            S.op("dve", lambda e, ps=ps: e.tensor_tensor(out=lg[:], in0=ps[:, 0:NE], in1=br_bc[:], op=ALU.add), reads=[pk, "br_bc"], writes=["lg"])
            if debug:
                S.dma("sp", dbg["lg"][ti * 128:(ti + 1) * 128, :], lg[:], chan="lg", reads=["lg"], writes=["dbg_lg"])
            S.op("dve", lambda e: e.max(out=max8[:], in_=lg[:]), reads=["lg"], writes=["max8"])
            S.op("dve", lambda e: e.max_index(out=idx8[:], in_max=max8[:], in_values=lg[:]), reads=["lg", "max8"], writes=["idx8"])
            S.op("dve", lambda e: e.tensor_copy(out=idxf[:], in_=idx8[:]), reads=["idx8"], writes=["idxf"])
            S.op("dve", lambda e: e.tensor_scalar(out=negm[:], in0=max8[:, 0:1], scalar1=-1.0, scalar2=None, op0=ALU.mult), reads=["max8"], writes=["negm"])
            S.op("act", lambda e: e.activation(out=ew[:], in_=max8[:, 0:4], func=AF.Exp, bias=negm[:, 0:1], scale=1.0, accum_out=den[:, 0:1]),
                 reads=["max8", "negm"], writes=["ew", "den"])
            S.op("dve", lambda e: e.reciprocal(out=den[:], in_=den[:]), reads=["den"], writes=["den"])
            S.op("dve", lambda e: e.tensor_scalar(out=w4[:], in0=ew[:], scalar1=den[:, 0:1], scalar2=None, op0=ALU.mult), reads=["ew", "den"], writes=["w4"])
            S.op("dve", lambda e: e.tensor_scalar(out=maskt[:], in0=lg[:], scalar1=max8[:, 3:4], scalar2=None, op0=ALU.is_ge), reads=["lg", "max8"], writes=["maskt"])
            ps, pk = next_pb()
            S.op("pe", lambda e, ps=ps: e.matmul(ps[:, 0:NE], lhsT=ltri_sb[:], rhs=maskt[:], start=True, stop=True), reads=["ltri", "maskt"], writes=[pk])
            S.op("dve", lambda e, ps=ps: e.tensor_tensor(out=slotf[:], in0=ps[:, 0:NE], in1=carry[:], op=ALU.add), reads=[pk, "carry"], writes=["slotf"])
            S.op("dve", lambda e: e.tensor_tensor(out=slotf[:], in0=slotf[:], in1=iotae_sb[:], op=ALU.add), reads=["slotf", "iotae"], writes=["slotf"])
            ps2, pk2 = next_pb()
            S.op("pe", lambda e, ps2=ps2: e.matmul(ps2[:, 0:NE], lhsT=ones_sq[:], rhs=maskt[:], start=True, stop=True), reads=["ones_sq", "maskt"], writes=[pk2])
            S.op("dve", lambda e, ps2=ps2: e.tensor_tensor(out=carry[:], in0=ps2[:, 0:NE], in1=carry[:], op=ALU.add), reads=[pk2, "carry"], writes=["carry"])
            for j in range(4):
                S.op("dve", lambda e, j=j: e.tensor_scalar(out=oh[:], in0=iota32[:], scalar1=idxf[:, j:j + 1], scalar2=None, op0=ALU.is_equal),
                     reads=["iota32", "idxf"], writes=["oh"])
                S.op("dve", lambda e: e.tensor_tensor(out=oh[:], in0=oh[:], in1=slotf[:], op=ALU.mult), reads=["oh", "slotf"], writes=["oh"])
                S.op("dve", lambda e, j=j: e.reduce_sum(out=sl4[:, j:j + 1], in_=oh[:], axis=AX.X), reads=["oh"], writes=["sl4"])
            S.op("dve", lambda e: e.tensor_copy(out=sl4i[:], in_=sl4[:]), reads=["sl4"], writes=["sl4i"])
            S.op("dve", lambda e, ti=ti: e.tensor_copy(out=pairs[:, ti, :, 0], in_=tokid_sb[:, ti:ti + 1].broadcast_to([128, 4])), reads=["tokid"], writes=[("pairs", ti)])
            S.op("dve", lambda e, ti=ti: e.tensor_copy(out=pairs[:, ti, :, 1].bitcast(F32), in_=w4[:]), reads=["w4", ("pairs", ti)], writes=[("pairs", ti)])
            for j in range(4):
                S.dma("pool", lists, pairs[:, ti, j, :], chan=("pairs", ti), reads=[("pairs", ti), "sl4i"], writes=["lists"],
                      indirect=dict(out_offset=bass.IndirectOffsetOnAxis(ap=sl4i[:, j:j + 1], axis=0), in_offset=None))
        flg_f = sb("flg_f", [1, 4 * NE])
        for g_ in range(4):
            S.op("dve", lambda e, g_=g_: e.tensor_scalar(out=flg_f[0:1, g_ * NE:(g_ + 1) * NE], in0=carry[0:1, :], scalar1=float(g_ * GRP),
                                                         scalar2=None, op0=ALU.is_gt), reads=["carry"], writes=["flg_f"])
        S.op("dve", lambda e: e.tensor_copy(out=flg_i[:], in_=flg_f[:]), reads=["flg_f"], writes=["flg_i"])
        S.barrier()
        ph.close()
        if stop_after == "A2":
            print("sems", len(S.dsem), "inst", S.n_inst, "waits", S.n_wait)
            return nc

        ph = ExitStack()
        cur[0] = ph
        WB = [sb("wbm%d" % i, [128, 16, 512], BF16) for i in range(4)]
        wb_i[0] = 0
        NWB = 4

        def load_w2(src_ap):
            i = wb_i[0] % NWB
            wb_i[0] += 1
            k = "wbm%d" % i
            for h in range(2):
                S.dma("pool", WB[i][:, h * 8:(h + 1) * 8, :],
                      src_ap[h * 1024:(h + 1) * 1024, :].rearrange("(kc p) n -> p kc n", p=128), chan=k, writes=[k])
            return WB[i], k

        lst = sb("lst", [128, 4, 2], I32)
        dum = sb("dum", [1, 64])
        dum_idx = {}
        xg = sb("xg", [128, 4, D], BF16)
        xT = sb("xT", [128, 16, GRP], BF16)
        actT = sb("actT", [128, 16, GRP], BF16)
        Yt = sb("Yt", [128, 4, D])
        bd_bc = sb("bd_bc", [128, D])
        bg_sb = sb("bg_sb", [128, 16])
        bu_sb = sb("bu_sb", [128, 16])
        gt = [sb("gt%d" % i, [128, GRP]) for i in range(2)]
        sgm = [sb("sgm%d" % i, [128, GRP]) for i in range(2)]
        ut = [sb("ut%d" % i, [128, GRP]) for i in range(2)]
        PGA = [(PG[0], "pg0"), (PG[1], "pg1")]
        PUP = [(PO[0], ("po", 0)), (PO[1], ("po", 1))]
        PDN = [(PSC, "psc"), (PS_, "pss")]
        it = [0]
        flag_regs = nc.alloc_registers("flg", engines=mybir.ALL_ENGINES)

        def pass_body(e_, g_):
            base = e_ * CAP + g_ * GRP
            S.dma("sp", lst[:], lists[base:base + GRP, :].rearrange("(b p) two -> p b two", p=128), chan="lst",
                  reads=["lists"], writes=["lst"])
            for blk in range(4):
                S.dma("pool", xg[:, blk, :], h2buf, chan="xg", reads=["lst", "h2dummy"] + [("h2buf", ti) for ti in range(16)],
                      writes=[("xg", blk)],
                      indirect=dict(out_offset=None, in_offset=bass.IndirectOffsetOnAxis(ap=lst[:, blk, 0:1], axis=0)))
            for blk in range(4):
                S.lastw[("xg", blk)] = ("dma", "xg", S.dcnt["xg"])
            for blk in range(4):
                for half in range(2):
                    PT = PTS[half]
                    ptk = "pt%d" % half
                    for j in range(8):
                        kc = half * 8 + j
                        S.op("pe", lambda e, kc=kc, j=j, PT=PT, blk=blk: e.transpose(PT[:, j * 128:(j + 1) * 128], xg[:, blk, kc * 128:(kc + 1) * 128], identb[:]),
                             reads=[("xg", blk), "identb"], writes=[ptk])
                    eng = "act" if half == 0 else "dve"
                    if eng == "act":
                        S.op("act", lambda e, PT=PT, blk=blk, half=half: e.activation(
                            out=xT[:, half * 8:(half + 1) * 8, blk * 128:(blk + 1) * 128], in_=PT[:, :].rearrange("p (j t) -> p j t", j=8), func=AF.Copy),
                            reads=[ptk], writes=[("xT", blk)])
                    else:
                        S.op("dve", lambda e, PT=PT, blk=blk, half=half: e.tensor_copy(
                            out=xT[:, half * 8:(half + 1) * 8, blk * 128:(blk + 1) * 128], in_=PT[:, :].rearrange("p (j t) -> p j t", j=8)),
                            reads=[ptk], writes=[("xT", blk)])
            xkeys = [("xT", blk) for blk in range(4)]
            for fq in range(4):
                wg, wgk = load_w2(w_gate[e_, :, fq * 512:(fq + 1) * 512])
                wu, wuk = load_w2(w_up[e_, :, fq * 512:(fq + 1) * 512])
                for m in range(4):
                    fc = fq * 4 + m
                    i2 = it[0] % 2
                    it[0] += 1
                    pg_, pgk = PGA[i2]
                    pu_, puk = PUP[i2]
                    for kc in range(16):
                        S.op("pe", lambda e, kc=kc, m=m, wg=wg, pg_=pg_: e.matmul(pg_[:, :], lhsT=wg[:, kc, m * 128:(m + 1) * 128], rhs=xT[:, kc, :],
                                                                                start=(kc == 0), stop=(kc == 15)), reads=[wgk] + xkeys, writes=[pgk])
                    for kc in range(16):
                        S.op("pe", lambda e, kc=kc, m=m, wu=wu, pu_=pu_: e.matmul(pu_[:, :], lhsT=wu[:, kc, m * 128:(m + 1) * 128], rhs=xT[:, kc, :],
                                                                                start=(kc == 0), stop=(kc == 15)), reads=[wuk] + xkeys, writes=[puk])
                    gk_, sk_, uk_ = "gt%d" % i2, "sgm%d" % i2, "ut%d" % i2
                    S.op("dve", lambda e, fc=fc, i2=i2, pg_=pg_: e.tensor_scalar(out=gt[i2][:], in0=pg_[:, :], scalar1=bg_sb[:, fc:fc + 1], scalar2=7.0,
                                                                               op0=ALU.add, op1=ALU.min), reads=[pgk, "bg_sb"], writes=[gk_])
                    S.op("act", lambda e, i2=i2: e.activation(out=sgm[i2][:], in_=gt[i2][:], func=AF.Sigmoid, scale=1.702), reads=[gk_], writes=[sk_])
                    S.op("dve", lambda e, fc=fc, i2=i2, pu_=pu_: e.tensor_scalar(out=ut[i2][:], in0=pu_[:, :], scalar1=bu_sb[:, fc:fc + 1], scalar2=7.0,
                                                                               op0=ALU.add, op1=ALU.min), reads=[puk, "bu_sb"], writes=[uk_])
                    S.op("dve", lambda e, i2=i2: e.tensor_scalar(out=ut[i2][:], in0=ut[i2][:], scalar1=-7.0, scalar2=1.0, op0=ALU.max, op1=ALU.add),
                         reads=[uk_], writes=[uk_])
                    S.op("dve", lambda e, i2=i2: e.tensor_tensor(out=gt[i2][:], in0=gt[i2][:], in1=sgm[i2][:], op=ALU.mult), reads=[gk_, sk_], writes=[gk_])
                    S.op("dve", lambda e, i2=i2, fc=fc: e.tensor_tensor(out=actT[:, fc, :], in0=gt[i2][:], in1=ut[i2][:], op=ALU.mult),
                         reads=[gk_, uk_], writes=[("actT", fc)])
            akeys = [("actT", fc) for fc in range(16)]
            for dq in range(4):
                wd, wdk = load_w2(w_down[e_, :, dq * 512:(dq + 1) * 512])
                for blk in range(4):
                    i2 = it[0] % 2
                    it[0] += 1
                    pd_, pdk = PDN[i2]
                    for fc in range(16):
                        S.op("pe", lambda e, fc=fc, blk=blk, wd=wd, pd_=pd_: e.matmul(pd_[:, :], lhsT=actT[:, fc, blk * 128:(blk + 1) * 128], rhs=wd[:, fc, :],
                                                                                    start=(fc == 0), stop=(fc == 15)), reads=[wdk] + akeys, writes=[pdk])
                    S.op("dve", lambda e, blk=blk, dq=dq, pd_=pd_: e.tensor_tensor(out=Yt[:, blk, dq * 512:(dq + 1) * 512], in0=pd_[:, :],
                                                                                 in1=bd_bc[:, dq * 512:(dq + 1) * 512], op=ALU.add),
                         reads=[pdk, "bd_bc"], writes=[("Yt", blk)])
                    S.op("dve", lambda e, blk=blk, dq=dq: e.tensor_scalar(out=Yt[:, blk, dq * 512:(dq + 1) * 512], in0=Yt[:, blk, dq * 512:(dq + 1) * 512],
                                                                         scalar1=lst[:, blk, 1:2].bitcast(F32), scalar2=None, op0=ALU.mult),
                         reads=[("Yt", blk), "lst"], writes=[("Yt", blk)])
            for blk in range(4):
                S.dma("pool", yacc, Yt[:, blk, :], chan="Ysc", reads=[("Yt", blk), "lst"], writes=["yacc"],
                      indirect=dict(out_offset=bass.IndirectOffsetOnAxis(ap=lst[:, blk, 0:1], axis=0), in_offset=None, compute_op=ALU.add))
            fin_ = ("dma", "Ysc", S.dcnt["Ysc"])
            for blk in range(4):
                S.readers[("Yt", blk)] = [fin_]
            S.readers["lst"] = [fin_]
            S.lastw["yacc"] = fin_

        def bump_skipped(snap_cnt, snap_d):
            for en_ in S.eng:
                dn = S.cnt[en_] - snap_cnt[en_]
                if dn > 0:
                    if snap_cnt[en_] > 0:
                        S.eng[en_].wait_ge(S.sem[en_], snap_cnt[en_])
                    S.eng[en_].sem_inc(S.sem[en_], dn)
            ci_ = 0
            for k_ in S.dcnt:
                dd = S.dcnt[k_] - snap_d.get(k_, 0)
                if dd > 0:
                    if S.dq[k_] == "pool":
                        if snap_d.get(k_, 0) > 0:
                            nc.gpsimd.wait_ge(S.dsem[k_], snap_d[k_])
                        ci_ = dum_idx.setdefault(k_, len(dum_idx))
                        j_ = 0
                        while dd > 0:
                            d1 = min(dd, 96)
                            nc.gpsimd.dma_start(out=dum[0:1, ci_ * 4 + j_:ci_ * 4 + j_ + 1], in_=flag[0:1, 0:1]).then_inc(S.dsem[k_], d1)
                            dd -= d1
                            j_ += 1
                    else:
                        if snap_d.get(k_, 0) > 0:
                            nc.sync.wait_ge(S.dsem[k_], snap_d[k_])
                        nc.sync.sem_inc(S.dsem[k_], dd)

        def guarded(e_, g_):
            nc.regs_load(flag_regs, flg_i[0:1, g_ * NE + e_: g_ * NE + e_ + 1])
            snap_cnt = dict(S.cnt)
            snap_d = dict(S.dcnt)
            snap_seen = {k_: dict(v_) for k_, v_ in S.seen.items()}
            with nc.If_cmp(flag_regs, 0, "IS_NE"):
                pass_body(e_, g_)
                if g_ + 1 < n_groups:
                    guarded(e_, g_ + 1)
            with nc.Else():
                bump_skipped(snap_cnt, snap_d)
            S.seen = snap_seen

        for e_ in range(NE):
            S.dma("sp", bd_bc[:], b_down[e_, :].partition_broadcast(128), chan="bd_bc", writes=["bd_bc"])
            S.dma("sp", bg_sb[:], b_gate[e_], chan="bg_sb", writes=["bg_sb"])
            S.dma("sp", bu_sb[:], b_up[e_], chan="bu_sb", writes=["bu_sb"])
            guarded(e_, 0)
        S.barrier()
        ph.close()

        ph = ExitStack()
        cur[0] = ph
        g2_bc = bc_tile("g2_bc", moddram[0, 5 * D:6 * D])
        l2g_bc = bc_tile("l2g_bc", ln2_g[0, :])
        l2b_bc = bc_tile("l2b_bc", ln2_b[0, :])
        YA = [sb("ya%d" % i, [128, D]) for i in range(2)]
        X1 = [sb("x1_%d" % i, [128, D]) for i in range(2)]
        OT = [sb("ot%d" % i, [128, D]) for i in range(2)]
        stats3 = sb("stats3", [128, 4, 6])
        mv3 = sb("mv3", [128, 2])
        rstd3 = sb("rstd3", [128, 1])
        for ti in range(16):
            b2 = ti % 2
            yk, xk, ok = "ya%d" % b2, "x1_%d" % b2, "ot%d" % b2
            S.dma("sp", YA[b2][:], yacc[ti * 128:(ti + 1) * 128, :], chan=yk, reads=["yacc"], writes=[yk])
            S.dma("sp", X1[b2][:], x1buf[ti * 128:(ti + 1) * 128, :], chan=xk, reads=[("x1buf", ti)], writes=[xk])
            S.op("pool", lambda e, b2=b2: e.tensor_tensor(out=YA[b2][:], in0=YA[b2][:], in1=g2_bc[:], op=ALU.mult), reads=[yk, "g2_bc"], writes=[yk])
            S.op("dve", lambda e, b2=b2: e.scalar_tensor_tensor(out=YA[b2][:], in0=X1[b2][:], scalar=ALPHA, in1=YA[b2][:], op0=ALU.mult, op1=ALU.add),
                 reads=[xk, yk], writes=[yk])
            ln_stats(YA[b2], yk, stats3, mv3, rstd3, "c")
            S.op("dve", lambda e, b2=b2: e.tensor_scalar(out=OT[b2][:], in0=YA[b2][:], scalar1=mv3[:, 0:1], scalar2=rstd3[:, 0:1], op0=ALU.subtract, op1=ALU.mult),
                 reads=[yk, "cmv", "crs"], writes=[ok])
            S.op("pool", lambda e, b2=b2: e.tensor_tensor(out=OT[b2][:], in0=OT[b2][:], in1=l2g_bc[:], op=ALU.mult), reads=[ok, "l2g_bc"], writes=[ok])
            S.op("pool", lambda e, b2=b2: e.tensor_tensor(out=OT[b2][:], in0=OT[b2][:], in1=l2b_bc[:], op=ALU.add), reads=[ok, "l2b_bc"], writes=[ok])
            S.dma("sp", out[ti * 128:(ti + 1) * 128, :], OT[b2][:], chan=ok, reads=[ok], writes=["out"])
        S.barrier()
        ph.close()
        print("sems", len(S.dsem), "inst", S.n_inst, "waits", S.n_wait)
    return nc


def host_consts():
    ident = np.eye(128, dtype=np.float32)
    triu2 = np.zeros((128, 128), np.float32)
    for b in range(2):
        triu2[b * 64:(b + 1) * 64, b * 64:(b + 1) * 64] = np.triu(np.ones((64, 64), np.float32))
    rmask = np.ones((128, SEG), np.float32)
    rmask[:, ::64] = 0.0
    ltri = np.triu(np.ones((128, 128), np.float32), 1)
    iota_e = np.tile((np.arange(NE, dtype=np.float32) * CAP)[None, :], (128, 1))
    tokid = (np.arange(16, dtype=np.int32)[None, :] * 128 + np.arange(128, dtype=np.int32)[:, None]).astype(np.int32)
    iota_n = np.tile(np.arange(NE, dtype=np.float32)[None, :], (128, 1))
    list_init = np.zeros((NE * CAP, 2), np.int32)
    list_init[:, 0] = T + (np.arange(NE * CAP) % 128)
    return dict(ident_f=ident, triu2=triu2, rmask=rmask, ltri=ltri, iota_e=iota_e, tokid=tokid, iota_n=iota_n, list_init=list_init)


def make_in_maps(inp):
    f = lambda a: np.ascontiguousarray(a, dtype=np.float32)
    x = inp["x"]
    consts = host_consts()
    shared = dict(
        w_ada=f(inp["w_ada"][0]), b_ada=f(inp["b_ada"][0][None, :]), w_in=f(inp["w_in"][0]), w_gk=f(inp["w_gk"][0]),
        b_gk=f(inp["b_gk"][0].reshape(4, 128).T), w_pool=f(inp["w_pool"][0]),
        b_pool=f(inp["b_pool"][0].reshape(8, 128).T), pool_scale=f(inp["pool_scale"][0].reshape(8, 128).T),
        gla_norm_w=f(inp["gla_norm_w"][0][None, :]), w_out=f(inp["w_out"][0]),
        ln1_g=f(inp["ln1_g"][0][None, :]), ln1_b=f(inp["ln1_b"][0][None, :]),
        w_router=f(inp["w_router"][0]), b_router=f(inp["b_router"][0][None, :]),
        w_gate=f(inp["w_gate"][0]), b_gate=f(inp["b_gate"][0].reshape(NE, 16, 128).transpose(0, 2, 1)),
        w_up=f(inp["w_up"][0]), b_up=f(inp["b_up"][0].reshape(NE, 16, 128).transpose(0, 2, 1)),
        w_down=f(inp["w_down"][0]), b_down=f(inp["b_down"][0]),
        ln2_g=f(inp["ln2_g"][0][None, :]), ln2_b=f(inp["ln2_b"][0][None, :]),
    )
    shared.update(consts)
    maps = []
    for core in range(8):
        b, half = core // 2, core % 2
        m = dict(shared)
        m["x_own"] = f(x[b, half * T:(half + 1) * T, :])
        m["x_pre"] = f(x[b, 0:T, :])
        m["c_l"] = f(inp["c"][b].reshape(16, 128).T)
        m["flag"] = np.full((128, 1), float(half), np.float32)
        ic = np.zeros((128, 4, 16), np.float32)
        for g in range(4):
            w = 2 ** (g + 1)
            pos = np.arange(1, 17, dtype=np.float32)
            ic[:, g, :] = (1.0 / np.minimum(pos, w) if half == 0 else np.full(16, 1.0 / w, np.float32))[None, :]
        m["invcnt"] = ic
        maps.append(m)
    return maps


N_GROUPS = 4


def kernel(**inputs):
    nc = build_program(n_groups=N_GROUPS)
    in_maps = make_in_maps(inputs)
    res = run_bass_kernel_spmd(nc, in_maps, core_ids=list(range(8)))
    outs = [res.results[i]["out"] for i in range(8)]
    full = np.stack([np.concatenate([outs[2 * b], outs[2 * b + 1]], axis=0) for b in range(4)], axis=0)
    return full.astype(np.float32)
```

```python
from contextlib import ExitStack
import numpy as np
import concourse.bass as bass
import concourse.mybir as mybir
from concourse.bass_utils import run_bass_kernel_spmd

F32 = mybir.dt.float32
BF16 = mybir.dt.bfloat16
I32 = mybir.dt.int32
U32 = mybir.dt.uint32
AF = mybir.ActivationFunctionType
ALU = mybir.AluOpType
AX = mybir.AxisListType

D = 2048
T = 2048
SEG = 512
NSEG = T // SEG
NE = 32
CAP = 2048
GRP = 512
ALPHA = 2.0 ** 0.25
LN_EPS = 1e-5
IN_W = 4112


class _Stop(Exception):
    pass


class Sched:
    def __init__(self, nc, stack):
        self.nc = nc
        self.eng = {"pe": nc.tensor, "act": nc.scalar, "dve": nc.vector,
                    "pool": nc.gpsimd, "sp": nc.sync}
        self.sem = {}
        self.cnt = {}
        self.stack = stack
        for e in self.eng:
            self.sem[e] = stack.enter_context(nc.semaphore("prog_" + e))
            self.cnt[e] = 0
        self.dsem = {}
        self.dcnt = {}
        self.dq = {}
        self.seen = {e: {} for e in self.eng}
        self.lastw = {}
        self.readers = {}
        self.n_wait = 0
        self.n_inst = 0

    def _chan(self, key):
        if key not in self.dsem:
            self.dsem[key] = self.stack.enter_context(
                self.nc.semaphore("d_" + str(len(self.dsem))))
            self.dcnt[key] = 0
        return self.dsem[key]

    def _wait(self, e, tok):
        kind, k, c = tok
        semkey = (kind, k)
        if self.seen[e].get(semkey, 0) >= c:
            return
        sem = self.sem[k] if kind == "eng" else self.dsem[k]
        self.eng[e].wait_ge(sem, c)
        self.seen[e][semkey] = c
        self.n_wait += 1

    def _deps(self, e, reads, writes, skip_same=False):
        toks = []
        for r in reads:
            w = self.lastw.get(r)
            if w is not None:
                toks.append(w)
        for w_ in writes:
            w = self.lastw.get(w_)
            if w is not None:
                toks.append(w)
            toks.extend(self.readers.get(w_, []))
        for t in toks:
            if skip_same and t[0] == "eng" and t[1] == e:
                continue
            self._wait(e, t)

    def _record(self, tok, reads, writes):
        for r in reads:
            self.readers.setdefault(r, []).append(tok)
        for w in writes:
            self.lastw[w] = tok
            self.readers[w] = []

    def op(self, e, fn, reads=(), writes=()):
        self._deps(e, reads, writes, skip_same=(e == "pe"))
        ins = fn(self.eng[e])
        self.cnt[e] += 1
        ins.then_inc(self.sem[e], 1)
        self._record(("eng", e, self.cnt[e]), reads, writes)
        self.n_inst += 1
        return ins

    def dma(self, q, out, in_, chan, reads=(), writes=(), indirect=None, **kw):
        self._deps(q, reads, writes)
        sem = self._chan(chan)
        if indirect is None:
            ins = self.eng[q].dma_start(out=out, in_=in_, **kw)
        else:
            ins = self.eng[q].indirect_dma_start(out=out, in_=in_, **indirect)
        self.dcnt[chan] += 16
        self.dq[chan] = q
        ins.then_inc(sem, 16)
        self._record(("dma", chan, self.dcnt[chan]), reads, writes)
        self.n_inst += 1
        return ins

    def barrier(self):
        for e in self.eng:
            for o in self.eng:
                if self.cnt[o] > 0:
                    self._wait(e, ("eng", o, self.cnt[o]))
            for k, c in self.dcnt.items():
                if c > 0:
                    self._wait(e, ("dma", k, c))

    def finish(self, e="sp"):
        for k, w in list(self.lastw.items()):
            if w is not None:
                self._wait(e, w)
            for r in self.readers.get(k, []):
                self._wait(e, r)


def build_program(debug=False, n_groups=4, stop_after=None):
    nc = bass.Bass("TRN2", target_bir_lowering=False)

    in_names = []
    lean = stop_after in ("A0", "A1", "A1s", "A1a", "A1b", "A1c", "A1d", "A2")

    def din(name, shape, dt=F32):
        if lean and name in ("w_gate", "w_up", "w_down"):
            return None
        in_names.append(name)
        return nc.dram_tensor(name, list(shape), dt, kind="ExternalInput").ap()

    x_own = din("x_own", [T, D])
    x_pre = din("x_pre", [T, D])
    c_l = din("c_l", [128, 16])
    flag = din("flag", [128, 1])
    invcnt = din("invcnt", [128, 4, 16])
    ident_f = din("ident_f", [128, 128])
    triu2 = din("triu2", [128, 128])
    rmask = din("rmask", [128, SEG])
    ltri = din("ltri", [128, 128])
    iota_e = din("iota_e", [128, NE])
    tokid = din("tokid", [128, 16], I32)
    iota_n = din("iota_n", [128, NE])
    list_init = din("list_init", [NE * CAP, 2], I32)
    w_ada = din("w_ada", [D, 6 * D])
    b_ada = din("b_ada", [1, 6 * D])
    w_in = din("w_in", [D, IN_W])
    w_gk = din("w_gk", [16, 512])
    b_gk = din("b_gk", [128, 4])
    w_pool = din("w_pool", [4, 256, 256])
    b_pool = din("b_pool", [128, 8])
    pool_scale = din("pool_scale", [128, 8])
    gla_norm_w = din("gla_norm_w", [1, 256])
    w_out = din("w_out", [D, D])
    ln1_g = din("ln1_g", [1, D])
    ln1_b = din("ln1_b", [1, D])
    w_router = din("w_router", [D, NE])
    b_router = din("b_router", [1, NE])
    w_gate = din("w_gate", [NE, D, D])
    b_gate = din("b_gate", [NE, 128, 16])
    w_up = din("w_up", [NE, D, D])
    b_up = din("b_up", [NE, 128, 16])
    w_down = din("w_down", [NE, D, D])
    b_down = din("b_down", [NE, D])
    ln2_g = din("ln2_g", [1, D])
    ln2_b = din("ln2_b", [1, D])

    out = nc.dram_tensor("out", [T, D], F32, kind="ExternalOutput").ap()

    def dscratch(name, shape, dt):
        return nc.dram_tensor(name, list(shape), dt, kind="Internal").ap()

    ybufT = dscratch("ybufT", [16, 128, T], BF16)
    x1buf = dscratch("x1buf", [T, D], F32)
    h2buf = dscratch("h2buf", [T + 128, D], BF16)
    lists = dscratch("lists", [NE * CAP, 2], I32)
    yacc = dscratch("yacc", [T + 128, D], F32)
    moddram = dscratch("moddram", [1, 6 * D], F32)
    dbg = None
    if debug:
        dbg = {
            "x1": nc.dram_tensor("dbg_x1", [T, D], F32, kind="ExternalOutput").ap(),
            "yT": nc.dram_tensor("dbg_yT", [16, 128, T], BF16, kind="ExternalOutput").ap(),
            "lg": nc.dram_tensor("dbg_lg", [T, NE], F32, kind="ExternalOutput").ap(),
        }

    nc.in_names = in_names
    with ExitStack() as st:
        S = Sched(nc, st)

        def sb(name, shape, dt=F32):
            return st.enter_context(nc.sbuf_tensor(name, list(shape), dt))

        def pst(name, shape, dt=F32):
            return st.enter_context(nc.psum_tensor(name, list(shape), dt))

        ident32 = sb("ident32", [128, 128])
        identb = sb("identb", [128, 128], BF16)
        triu_sb = sb("triu_sb", [128, 128])
        rmask_sb = sb("rmask_sb", [128, SEG])
        flag_sb = sb("flag_sb", [128, 1])
        invc_sb = sb("invc_sb", [128, 4, 16])
        ones_row = sb("ones_row", [1, 128])
        one11 = sb("one11", [1, 1])
        eps_sb = sb("eps_sb", [128, 1])
        S.dma("sp", ident32[:], ident_f, chan="ident32", writes=["ident32"])
        S.dma("pool", identb[:], ident_f, chan="identb", writes=["identb"])
        S.dma("sp", triu_sb[:], triu2, chan="triu", writes=["triu"])
        S.dma("sp", rmask_sb[:], rmask, chan="rmask", writes=["rmask"])
        S.dma("sp", flag_sb[:], flag, chan="flag", writes=["flag"])
        S.dma("sp", invc_sb[:], invcnt, chan="invc", writes=["invc"])
        S.op("dve", lambda e: e.memset(ones_row[:], 1.0), writes=["ones_row"])
        S.op("dve", lambda e: e.memset(one11[:], 1.0), writes=["one11"])
        S.op("dve", lambda e: e.memset(eps_sb[:], LN_EPS), writes=["eps"])

        PG = [pst("pg%d" % i, [128, 512]) for i in range(2)]
        PTS = [pst("ptr%d" % i, [128, 1024], BF16) for i in range(2)]
        PSC = pst("psc", [128, 512])
        PO = [pst("po%d" % i, [128, 512]) for i in range(2)]
        PS_ = pst("pss", [128, 512])
        pg_i = [0]

        def next_pg():
            i = pg_i[0] % 2
            pg_i[0] += 1
            return PG[i], "pg%d" % i

        flg_i = sb("flg_i", [1, 4 * NE], I32)
        cur = [st]

        def sb(name, shape, dt=F32):
            return cur[0].enter_context(nc.sbuf_tensor(name, list(shape), dt))

        ph = ExitStack()
        cur[0] = ph
        c_sb = sb("c_sb", [128, 16])
        sc_sb = sb("sc_sb", [128, 16])
        S.dma("sp", c_sb[:], c_l, chan="c_sb", writes=["c_sb"])
        S.op("act", lambda e: e.activation(out=sc_sb[:], in_=c_sb[:], func=AF.Silu),
             reads=["c_sb"], writes=["sc_sb"])
        WF = [sb("wf%d" % i, [128, 16, 512]) for i in range(2)]
        brow = [sb("brow%d" % i, [1, 512]) for i in range(2)]
        mrow = [sb("mrow%d" % i, [1, 512]) for i in range(2)]
        for j in range(24):
            i = j % 2
            wk = "wf%d" % i
            S.dma("sp", WF[i][:], w_ada[:, j * 512:(j + 1) * 512].rearrange("(kc p) n -> p kc n", p=128),
                  chan=wk, writes=[wk])
            S.dma("sp", brow[i][:], b_ada[0:1, j * 512:(j + 1) * 512], chan="brow%d" % i, writes=["brow%d" % i])
            ps, pk = next_pg()
            for kc in range(16):
                S.op("pe", lambda e, kc=kc, i=i, ps=ps: e.matmul(ps[0:1, :], lhsT=sc_sb[:, kc:kc + 1], rhs=WF[i][:, kc, :],
                                                                 start=(kc == 0), stop=(kc == 15)),
                     reads=[wk, "sc_sb"], writes=[pk])
            S.op("dve", lambda e, i=i, ps=ps: e.tensor_tensor(out=mrow[i][:], in0=ps[0:1, :], in1=brow[i][:], op=ALU.add),
                 reads=[pk, "brow%d" % i], writes=["mrow%d" % i])
            S.dma("sp", moddram[0:1, j * 512:(j + 1) * 512], mrow[i][:], chan="mrow%d" % i, reads=["mrow%d" % i],
                  writes=["moddram"])
        S.barrier()
        ph.close()
        if stop_after == "A0":
            print("sems", len(S.dsem), "inst", S.n_inst, "waits", S.n_wait)
            return nc

        ph = ExitStack()
        cur[0] = ph
        sh1_fm = sb("sh1_fm", [128, 16])
        sc1p_fm = sb("sc1p_fm", [128, 16])
        with nc.allow_non_contiguous_dma(reason="tiny strided vector load"):
            S.dma("sp", sh1_fm[:], moddram[0, 0:D].rearrange("(kc p) -> p kc", p=128), chan="sh1_fm",
                  reads=["moddram"], writes=["sh1_fm"])
            S.dma("sp", sc1p_fm[:], moddram[0, D:2 * D].rearrange("(kc p) -> p kc", p=128), chan="sc1p_fm",
                  reads=["moddram"], writes=["sc1p_0"])
        S.op("dve", lambda e: e.tensor_scalar(out=sc1p_fm[:], in0=sc1p_fm[:], scalar1=1.0, scalar2=None, op0=ALU.add),
             reads=["sc1p_0"], writes=["sc1p_fm"])
        wgkin = sb("wgkin", [128, 16, 16], BF16)
        S.dma("pool", wgkin[:], w_in[:, 3072:3088].rearrange("(kc p) n -> p kc n", p=128),
              chan="wgkin", writes=["wgkin"])
        wgk_sb = sb("wgk_sb", [16, 512])
        S.dma("sp", wgk_sb[:], w_gk, chan="wgk", writes=["wgk"])
        nbgk = sb("nbgk", [128, 4])
        S.dma("sp", nbgk[:], b_gk, chan="nbgk", writes=["nbgk0"])
        S.op("dve", lambda e: e.tensor_scalar(out=nbgk[:], in0=nbgk[:], scalar1=-1.0, scalar2=None, op0=ALU.mult),
             reads=["nbgk0"], writes=["nbgk"])
        wpool_sb = sb("wpool_sb", [128, 4, 2, 256], BF16)
        S.dma("pool", wpool_sb[:], w_pool.rearrange("g (cc p) d -> p g cc d", p=128),
              chan="wpool", writes=["wpool"])
        bpool_sb = sb("bpool_sb", [128, 8])
        pscale_sb = sb("pscale_sb", [128, 8])
        S.dma("sp", bpool_sb[:], b_pool, chan="bpool", writes=["bpool"])
        S.dma("sp", pscale_sb[:], pool_scale, chan="pscale", writes=["pscale"])
        nw_row = sb("nw_row", [1, 256])
        S.dma("sp", nw_row[:], gla_norm_w, chan="nw_row", writes=["nw_row"])
        nw_bc = sb("nw_bc", [128, 256])
        ps, pk = next_pg()
        S.op("pe", lambda e: e.matmul(ps[:, 0:256], lhsT=ones_row[0:1, :], rhs=nw_row[0:1, :], start=True, stop=True),
             reads=["nw_row", "ones_row"], writes=[pk])
        S.op("act", lambda e: e.activation(out=nw_bc[:], in_=ps[:, 0:256], func=AF.Copy), reads=[pk], writes=["nw_bc"])
        lnq_sb = sb("lnq_sb", [128, 1])
        S.op("dve", lambda e: e.memset(lnq_sb[:], float(np.log(128.0 ** -0.5))), writes=["lnq"])
        rms_eps = sb("rms_eps", [128, 1])
        S.op("dve", lambda e: e.memset(rms_eps[:], 1e-6), writes=["rms_eps"])

        if stop_after == "A1s":
            S.barrier()
            ph.close()
            return nc
        WB = [sb("wb%d" % i, [128, 16, 512], BF16) for i in range(3)]
        wb_i = [0]

        def load_w(src_ap):
            i = wb_i[0] % 3
            wb_i[0] += 1
            k = "wb%d" % i
            for h in range(2):
                S.dma("pool", WB[i][:, h * 8:(h + 1) * 8, :],
                      src_ap[h * 1024:(h + 1) * 1024, :].rearrange("(kc p) n -> p kc n", p=128),
                      chan=k, writes=[k])
            return WB[i], k

        XT = [sb("xt%d" % i, [128, D]) for i in range(2)]
        xt_i = [0]
        xn = sb("xn", [128, D], BF16)
        stats = sb("stats", [128, 4, 6])
        mv = sb("mv", [128, 2])
        rstd = sb("rstd", [128, 1])
        hT = sb("hT", [128, 16, SEG], BF16)
        uT = sb("uT", [128, 8, 16 + SEG])
        sA = sb("sA", [128, 2, 16 + SEG])
        sB = sb("sB", [128, 2, 16 + SEG])
        pooled = sb("pooled", [128, 2, SEG], BF16)
        fixs = sb("fixs", [128, 2, 16])
        qtil = sb("qtil", [128, 4, SEG], BF16)
        ktil = sb("ktil", [128, 4, SEG], BF16)
        khatT = sb("khatT", [128, 4, SEG], BF16)
        khat_tm = sb("khat_tm", [128, 4, 4, 128], BF16)
        v_tm = sb("v_tm", [128, 4, 1024], BF16)
        sg_tm = sb("sg_tm", [128, 4, 1024], BF16)
        gkT = sb("gkT", [16, SEG])
        spl = sb("spl", [128, SEG])
        cs = sb("cs", [128, 4, SEG])
        eq = sb("eq", [128, SEG])
        enb = sb("enb", [128, SEG])
        ehat = sb("ehat", [128, SEG])
        eL = sb("eL", [128, 4, 8])
        state32 = sb("state32", [128, 4, 256])
        stateb = sb("stateb", [128, 4, 256], BF16)
        scm = sb("scm", [128, 2, 128], BF16)
        yB = sb("yB", [128, 1024], BF16)
        nwsg = sb("nwsg", [128, 256])
        ssq = sb("ssq", [128, 1])
        rr = sb("rr", [128, 1])
        osq = sb("osq", [128, 256])
        yT = sb("yT", [128, 16, SEG], BF16)
        S.op("pool", lambda e: e.memset(state32[:], 0.0), writes=[("state32", h) for h in range(4)])
        S.op("pool", lambda e: e.memset(stateb[:], 0.0), writes=[("stateb", h) for h in range(4)])
        S.op("pool", lambda e: e.memset(uT[:], 0.0), writes=[("uT", ch) for ch in range(8)])
        S.op("pool", lambda e: e.memset(sA[:], 0.0), writes=["sA"])
        S.op("pool", lambda e: e.memset(sB[:], 0.0), writes=["sB"])

        def layer_norm_stats(xt, xk):
            for j in range(4):
                S.op("dve", lambda e, j=j: e.bn_stats(out=stats[:, j, :], in_=xt[:, j * 512:(j + 1) * 512]),
                     reads=[xk], writes=[("stats", j)])
            S.op("dve", lambda e: e.bn_aggr(out=mv[:], in_=stats[:].rearrange("p a b -> p (a b)")),
                 reads=[("stats", j) for j in range(4)], writes=["mv"])
            S.op("act", lambda e: e.activation(out=rstd[:], in_=mv[:, 1:2], func=AF.Sqrt, bias=eps_sb[:, 0:1], scale=1.0),
                 reads=["mv", "eps"], writes=["rstd0"])
            S.op("dve", lambda e: e.reciprocal(out=rstd[:], in_=rstd[:]), reads=["rstd0"], writes=["rstd"])

        def mixer_segment(xsrc, seg, own, need_u):
            for tt in range(4):
                i = xt_i[0] % 2
                xt_i[0] += 1
                xk = "xt%d" % i
                r0 = seg * SEG + tt * 128
                S.dma("sp", XT[i][:], xsrc[r0:r0 + 128, :], chan=xk, writes=[xk])
                layer_norm_stats(XT[i], xk)
                S.op("dve", lambda e, i=i: e.tensor_scalar(out=xn[:], in0=XT[i][:], scalar1=mv[:, 0:1], scalar2=rstd[:, 0:1],
                                                           op0=ALU.subtract, op1=ALU.mult),
                     reads=[xk, "mv", "rstd"], writes=["xn"])
                for half in range(2):
                    PT = PTS[half]
                    ptk = "pt%d" % half
                    for j in range(8):
                        kc = half * 8 + j
                        S.op("pe", lambda e, kc=kc, j=j, PT=PT: e.transpose(PT[:, j * 128:(j + 1) * 128], xn[:, kc * 128:(kc + 1) * 128], identb[:]),
                             reads=["xn", "identb"], writes=[ptk])
                    for j in range(8):
                        kc = half * 8 + j
                        eng = "act" if j % 2 == 0 else "dve"
                        if eng == "act":
                            S.op("act", lambda e, kc=kc, j=j, tt=tt, PT=PT: e.activation(
                                out=hT[:, kc, tt * 128:(tt + 1) * 128], in_=PT[:, j * 128:(j + 1) * 128], func=AF.Identity,
                                bias=sh1_fm[:, kc:kc + 1], scale=sc1p_fm[:, kc:kc + 1]),
                                reads=[ptk, "sh1_fm", "sc1p_fm"], writes=[("hT", tt)])
                        else:
                            S.op("dve", lambda e, kc=kc, j=j, tt=tt, PT=PT: e.tensor_scalar(
                                out=hT[:, kc, tt * 128:(tt + 1) * 128], in0=PT[:, j * 128:(j + 1) * 128],
                                scalar1=sc1p_fm[:, kc:kc + 1], scalar2=sh1_fm[:, kc:kc + 1], op0=ALU.mult, op1=ALU.add),
                                reads=[ptk, "sh1_fm", "sc1p_fm"], writes=[("hT", tt)])
            hkeys = [("hT", tt) for tt in range(4)]
            if stop_after == "A1a":
                raise _Stop()

            def fm_group(wt, wk, m, evac):
                ps, pk = next_pg()
                for kc in range(16):
                    S.op("pe", lambda e, kc=kc: e.matmul(ps[:, :], lhsT=wt[:, kc, m * 128:(m + 1) * 128], rhs=hT[:, kc, :],
                                                         start=(kc == 0), stop=(kc == 15)),
                         reads=[wk] + hkeys, writes=[pk])
                evac(ps, pk)

            def tm_group(wt, wk, tt, evac):
                ps, pk = next_pg()
                for kc in range(16):
                    S.op("pe", lambda e, kc=kc: e.matmul(ps[:, :], lhsT=hT[:, kc, tt * 128:(tt + 1) * 128], rhs=wt[:, kc, :],
                                                         start=(kc == 0), stop=(kc == 15)),
                         reads=[wk, ("hT", tt)], writes=[pk])
                evac(ps, pk)

            ps, pk = next_pg()
            for kc in range(16):
                S.op("pe", lambda e, kc=kc: e.matmul(ps[0:16, :], lhsT=wgkin[:, kc, :], rhs=hT[:, kc, :],
                                                     start=(kc == 0), stop=(kc == 15)),
                     reads=["wgkin"] + hkeys, writes=[pk])
            S.op("act", lambda e: e.activation(out=gkT[:], in_=ps[0:16, :], func=AF.Copy), reads=[pk], writes=["gkT"])
            for h in range(4):
                ps, pk = next_pg()
                S.op("pe", lambda e, h=h: e.matmul(ps[:, :], lhsT=wgk_sb[:, h * 128:(h + 1) * 128], rhs=gkT[:, :],
                                                   start=True, stop=True), reads=["wgk", "gkT"], writes=[pk])
                S.op("act", lambda e, h=h: e.activation(out=spl[:], in_=ps[:, :], func=AF.Exp, bias=nbgk[:, h:h + 1], scale=-1.0),
                     reads=[pk, "nbgk"], writes=["spl"])
                S.op("act", lambda e: e.activation(out=spl[:], in_=spl[:], func=AF.Ln, bias=1.0, scale=1.0),
                     reads=["spl"], writes=["spl"])
                S.op("dve", lambda e, h=h: e.tensor_tensor_scan(out=cs[:, h, :], data0=rmask_sb[:], data1=spl[:], initial=0.0,
                                                                op0=ALU.mult, op1=ALU.add),
                     reads=["spl", "rmask"], writes=[("cs", h)])
            if stop_after == "A1b":
                raise _Stop()
            wt, wk = load_w(w_in[:, 1536:2048])
            for h in range(4):
                S.op("act", lambda e, h=h: e.activation(out=enb[:], in_=cs[:, h, :], func=AF.Exp, scale=1.0 / 16.0),
                     reads=[("cs", h)], writes=["enb"])
                S.op("act", lambda e, h=h: e.activation(
                    out=eL[:, h, :], in_=cs[:, h, :].rearrange("p (c t) -> p c t", t=64)[:, :, 63], func=AF.Exp, scale=-1.0 / 16.0),
                    reads=[("cs", h)], writes=[("eL", h)])
                S.op("dve", lambda e, h=h: e.tensor_tensor(
                    out=ehat[:].rearrange("p (c t) -> p c t", t=64), in0=enb[:].rearrange("p (c t) -> p c t", t=64),
                    in1=eL[:, h, :].unsqueeze(2).broadcast_to([128, 8, 64]), op=ALU.mult),
                    reads=["enb", ("eL", h)], writes=["ehat"])

                def evac_k(ps, pk, h=h):
                    if own:
                        S.op("dve", lambda e: e.tensor_tensor(out=ktil[:, h, :], in0=ps[:, :], in1=enb[:], op=ALU.mult),
                             reads=[pk, "enb"], writes=[("ktil", h)])
                    S.op("dve", lambda e: e.tensor_tensor(out=khatT[:, h, :], in0=ps[:, :], in1=ehat[:], op=ALU.mult),
                         reads=[pk, "ehat"], writes=[("khatT", h)])
                fm_group(wt, wk, h, evac_k)
            if stop_after == "A1c":
                raise _Stop()
            for tt in range(4):
                PT = PTS[tt % 2]
                ptk = "pt%d" % (tt % 2)
                for h in range(4):
                    S.op("pe", lambda e, tt=tt, h=h, PT=PT: e.transpose(PT[:, h * 128:(h + 1) * 128], khatT[:, h, tt * 128:(tt + 1) * 128], identb[:]),
                         reads=[("khatT", h), "identb"], writes=[ptk])
                S.op("act", lambda e, tt=tt, PT=PT: e.activation(out=khat_tm[:, tt, :, :], in_=PT[:, 0:512].rearrange("p (h d) -> p h d", h=4),
                                                          func=AF.Copy),
                     reads=[ptk], writes=[("khat_tm", tt)])
            for half in range(2):
                wt, wk = load_w(w_in[:, 2048 + half * 512: 2048 + (half + 1) * 512])
                for tt in range(4):
                    def evac_v(ps, pk, tt=tt, half=half):
                        S.op("act", lambda e: e.activation(out=v_tm[:, tt, half * 512:(half + 1) * 512], in_=ps[:, :], func=AF.Copy),
                             reads=[pk], writes=[("v_tm", tt)])
                    tm_group(wt, wk, tt, evac_v)
            if own:
                wt, wk = load_w(w_in[:, 1024:1536])
                for h in range(4):
                    S.op("act", lambda e, h=h: e.activation(out=eq[:], in_=cs[:, h, :], func=AF.Exp, scale=-1.0 / 16.0, bias=lnq_sb[:, 0:1]),
                         reads=[("cs", h), "lnq"], writes=["eq"])

                    def evac_q(ps, pk, h=h):
                        S.op("dve", lambda e: e.tensor_tensor(out=qtil[:, h, :], in0=ps[:, :], in1=eq[:], op=ALU.mult),
                             reads=[pk, "eq"], writes=[("qtil", h)])
                    fm_group(wt, wk, h, evac_q)
                for half in range(2):
                    wt, wk = load_w(w_in[:, 3088 + half * 512: 3088 + (half + 1) * 512])
                    for tt in range(4):
                        def evac_g(ps, pk, tt=tt, half=half):
                            S.op("act", lambda e: e.activation(out=sg_tm[:, tt, half * 512:(half + 1) * 512], in_=ps[:, :], func=AF.Silu),
                                 reads=[pk], writes=[("sg_tm", tt)])
                        tm_group(wt, wk, tt, evac_g)
            if need_u:
                for half in range(2):
                    wt, wk = load_w(w_in[:, half * 512:(half + 1) * 512])
                    for m in range(4):
                        def evac_u(ps, pk, ch=half * 4 + m):
                            S.op("act", lambda e: e.activation(out=uT[:, ch, 16:16 + SEG], in_=ps[:, :], func=AF.Copy),
                                 reads=[pk], writes=[("uT", ch)])
                        fm_group(wt, wk, m, evac_u)
            if own and seg == 0:
                S.op("dve", lambda e: e.tensor_scalar(out=state32[:].rearrange("p a b -> p (a b)"), in0=state32[:].rearrange("p a b -> p (a b)"),
                                                      scalar1=flag_sb[:, 0:1], scalar2=None, op0=ALU.mult),
                     reads=[("state32", h) for h in range(4)] + ["flag"], writes=[("state32", h) for h in range(4)])
                S.op("dve", lambda e: e.tensor_scalar(out=stateb[:].rearrange("p a b -> p (a b)"), in0=state32[:].rearrange("p a b -> p (a b)"),
                                                      scalar1=1.0, scalar2=None, op0=ALU.mult),
                     reads=[("state32", h) for h in range(4)], writes=[("stateb", h) for h in range(4)])
                for ch in range(8):
                    S.op("pool", lambda e, ch=ch: e.tensor_scalar(out=uT[:, ch, 0:16], in0=uT[:, ch, 0:16], scalar1=flag_sb[:, 0:1], scalar2=None,
                                                                  op0=ALU.mult),
                         reads=[("uT", ch), "flag"], writes=[("uT", ch)])
            if own:
                for g in range(4):
                    w = 2 ** (g + 1)
                    chs = [("uT", 2 * g), ("uT", 2 * g + 1)]
                    src = uT[:, 2 * g:2 * g + 2, :]
                    srck = chs
                    bufs = [(sA, "sA"), (sB, "sB")]
                    bi = 0
                    step = 1
                    L = 16 + SEG
                    while step < w:
                        dst, dk = bufs[bi]
                        S.op("pool", lambda e, src=src, dst=dst, step=step: e.tensor_tensor(
                            out=dst[:, :, step:L], in0=src[:, :, step:L], in1=src[:, :, 0:L - step], op=ALU.add),
                            reads=srck, writes=[dk])
                        src, srck = dst[:, :, :], [dk]
                        bi ^= 1
                        step *= 2
                    S.op("dve", lambda e, src=src, g=g, w=w: e.scalar_tensor_tensor(
                        out=pooled[:, :, :], in0=src[:, :, 16:16 + SEG], scalar=1.0 / w, in1=uT[:, 2 * g:2 * g + 2, 16:16 + SEG],
                        op0=ALU.mult, op1=ALU.subtract), reads=srck + chs, writes=["pooled"])
                    if seg == 0:
                        for cc in range(2):
                            S.op("dve", lambda e, src=src, g=g, cc=cc: e.tensor_tensor(
                                out=fixs[:, cc, :], in0=src[:, cc, 16:32], in1=invc_sb[:, g, :], op=ALU.mult),
                                reads=srck + ["invc"], writes=["fixs"])
                            S.op("dve", lambda e, g=g, cc=cc: e.tensor_tensor(
                                out=pooled[:, cc, 0:16], in0=fixs[:, cc, :], in1=uT[:, 2 * g + cc, 16:32], op=ALU.subtract),
                                reads=["fixs"] + chs, writes=["pooled"])
                    for dh in range(2):
                        ps, pk = next_pg()
                        for cc in range(2):
                            S.op("pe", lambda e, g=g, cc=cc, dh=dh: e.matmul(ps[:, :], lhsT=wpool_sb[:, g, cc, dh * 128:(dh + 1) * 128],
                                                                           rhs=pooled[:, cc, :], start=(cc == 0), stop=(cc == 1)),
                                 reads=["wpool", "pooled"], writes=[pk])
                        ch = 2 * g + dh
                        S.op("dve", lambda e, ch=ch, ps=ps: e.tensor_scalar(out=yT[:, ch, :], in0=ps[:, :], scalar1=bpool_sb[:, ch:ch + 1],
                                                                            scalar2=pscale_sb[:, ch:ch + 1], op0=ALU.add, op1=ALU.mult),
                             reads=[pk, "bpool", "pscale"], writes=[("yT", ch)])
            if need_u:
                for ch in range(8):
                    S.op("pool", lambda e, ch=ch: e.tensor_copy(out=uT[:, ch, 0:16], in_=uT[:, ch, SEG:SEG + 16]),
                         reads=[("uT", ch)], writes=[("uT", ch)])
            if stop_after == "A1d":
                raise _Stop()
            for tt in range(4):
                if own:
                    for h in range(4):
                        sl = slice(tt * 128, (tt + 1) * 128)
                        S.op("pe", lambda e, h=h, sl=sl: e.matmul(PSC[:, (h % 4) * 128:(h % 4 + 1) * 128], lhsT=ktil[:, h, sl], rhs=qtil[:, h, sl],
                                                                  start=True, stop=True),
                             reads=[("ktil", h), ("qtil", h)], writes=["psc"])
                for h in range(4):
                    if own:
                        S.op("dve", lambda e, h=h: e.tensor_tensor(out=scm[:, h % 2, :], in0=PSC[:, h * 128:(h + 1) * 128], in1=triu_sb[:], op=ALU.mult),
                             reads=["psc", "triu"], writes=[("scm", h % 2)])
                        po = PO[h // 2]
                        pok = ("po", h // 2)
                    for c in range(2):
                        rows = slice(c * 64, (c + 1) * 64)
                        cg = tt * 2 + c
                        if own:
                            S.op("pe", lambda e, h=h, c=c, rows=rows, po=po: e.matmul(
                                po[rows, (h % 2) * 256:(h % 2 + 1) * 256], lhsT=scm[rows, h % 2, c * 64:(c + 1) * 64],
                                rhs=v_tm[rows, tt, h * 256:(h + 1) * 256], start=True, stop=False),
                                reads=[("scm", h % 2), ("v_tm", tt)], writes=[pok])
                            S.op("pe", lambda e, h=h, c=c, rows=rows, po=po: e.matmul(
                                po[rows, (h % 2) * 256:(h % 2 + 1) * 256], lhsT=qtil[:, h, tt * 128 + c * 64: tt * 128 + (c + 1) * 64],
                                rhs=stateb[:, h, :], start=False, stop=True),
                                reads=[("qtil", h), ("stateb", h)], writes=[pok])
                        slot = (h * 2 + c) % 2
                        S.op("pe", lambda e, h=h, rows=rows, slot=slot: e.matmul(
                            PS_[:, slot * 256:(slot + 1) * 256], lhsT=khat_tm[rows, tt, h, :], rhs=v_tm[rows, tt, h * 256:(h + 1) * 256],
                            start=True, stop=True),
                            reads=[("khat_tm", tt), ("v_tm", tt)], writes=["pss"])
                        S.op("dve", lambda e, h=h, cg=cg, slot=slot: e.scalar_tensor_tensor(
                            out=state32[:, h, :], in0=state32[:, h, :], scalar=eL[:, h, cg:cg + 1], in1=PS_[:, slot * 256:(slot + 1) * 256],
                            op0=ALU.mult, op1=ALU.add),
                            reads=["pss", ("eL", h), ("state32", h)], writes=[("state32", h)])
                        S.op("act", lambda e, h=h: e.activation(out=stateb[:, h, :], in_=state32[:, h, :], func=AF.Copy),
                             reads=[("state32", h)], writes=[("stateb", h)])
                    if own:
                        S.op("act", lambda e, h=h, po=po: e.activation(out=osq[:], in_=po[:, (h % 2) * 256:(h % 2 + 1) * 256], func=AF.Square,
                                                                       accum_out=ssq[:, 0:1]),
                             reads=[pok], writes=["osq", "ssq"])
                        S.op("act", lambda e: e.activation(out=rr[:], in_=ssq[:], func=AF.Sqrt, bias=rms_eps[:, 0:1], scale=1.0 / 256.0),
                             reads=["ssq", "rms_eps"], writes=["rr0"])
                        S.op("dve", lambda e: e.reciprocal(out=rr[:], in_=rr[:]), reads=["rr0"], writes=["rr"])
                        S.op("dve", lambda e, h=h, tt=tt: e.tensor_tensor(out=nwsg[:], in0=nw_bc[:], in1=sg_tm[:, tt, h * 256:(h + 1) * 256], op=ALU.mult),
                             reads=["nw_bc", ("sg_tm", tt)], writes=["nwsg"])
                        S.op("dve", lambda e, h=h, po=po: e.scalar_tensor_tensor(
                            out=yB[:, h * 256:(h + 1) * 256], in0=po[:, (h % 2) * 256:(h % 2 + 1) * 256], scalar=rr[:, 0:1], in1=nwsg[:],
                            op0=ALU.mult, op1=ALU.mult), reads=[pok, "rr", "nwsg"], writes=[("yB", h)])
                if own:
                    PT = PTS[tt % 2]
                    ptk = "pt%d" % (tt % 2)
                    for j in range(8):
                        S.op("pe", lambda e, j=j, PT=PT: e.transpose(PT[:, j * 128:(j + 1) * 128], yB[:, j * 128:(j + 1) * 128], identb[:]),
                             reads=[("yB", j // 2), "identb"], writes=[ptk])
                    S.op("act", lambda e, tt=tt, PT=PT: e.activation(out=yT[:, 8:16, tt * 128:(tt + 1) * 128],
                                                              in_=PT[:, :].rearrange("p (j t) -> p j t", j=8), func=AF.Copy),
                         reads=[ptk], writes=[("yTb", tt)])
            if own:
                ykeys = [("yT", ch) for ch in range(8)] + [("yTb", tt) for tt in range(4)]
                S.dma("sp", ybufT[:, :, seg * SEG:(seg + 1) * SEG].rearrange("c p t -> p c t"), yT[:], chan="yT",
                      reads=ykeys, writes=[("ybufT", seg)])

        try:
            for seg in range(NSEG):
                mixer_segment(x_pre, seg, own=False, need_u=(seg == NSEG - 1))
            for seg in range(NSEG):
                mixer_segment(x_own, seg, own=True, need_u=True)
        except _Stop:
            S.barrier()
            ph.close()
            print("sems", len(S.dsem), "inst", S.n_inst, "waits", S.n_wait)
            return nc

        if debug:
            for seg in range(NSEG):
                S.dma("sp", yT[:], ybufT[:, :, seg * SEG:(seg + 1) * SEG].rearrange("c p t -> p c t"), chan="yT",
                      reads=[("ybufT", seg)], writes=["yT_dbg"] + [("yT", ch) for ch in range(8)] + [("yTb", tt) for tt in range(4)])
                S.dma("sp", dbg["yT"][:, :, seg * SEG:(seg + 1) * SEG].rearrange("c p t -> p c t"), yT[:], chan="yT",
                      reads=["yT_dbg"], writes=["dbg_yT"])
        S.barrier()
        ph.close()
        if stop_after == "A1":
            print("sems", len(S.dsem), "inst", S.n_inst, "waits", S.n_wait)
            return nc
        ph = ExitStack()
        cur[0] = ph
        PB = [PG[0], PG[1], PSC, PO[0], PO[1], PS_]
        PBK = ["pg0", "pg1", "psc", ("po", 0), ("po", 1), "pss"]
        pb_i = [0]

        def next_pb():
            i = pb_i[0] % len(PB)
            pb_i[0] += 1
            return PB[i], PBK[i]

        wout = sb("wout", [128, 16, D], BF16)
        for j in range(4):
            for h in range(2):
                S.dma("pool", wout[:, h * 8:(h + 1) * 8, j * 512:(j + 1) * 512],
                      w_out[h * 1024:(h + 1) * 1024, j * 512:(j + 1) * 512].rearrange("(kc p) n -> p kc n", p=128),
                      chan="wout", writes=["wout"])

        def bc_tile(name, src_row):
            t = sb(name, [128, D])
            S.dma("sp", t[:], src_row.partition_broadcast(128), chan=name, reads=["moddram"], writes=[name])
            return t
        g1_bc = bc_tile("g1_bc", moddram[0, 2 * D:3 * D])
        sh2_bc = bc_tile("sh2_bc", moddram[0, 3 * D:4 * D])
        sc2p_bc = bc_tile("sc2p_bc", moddram[0, 4 * D:5 * D])
        S.op("pool", lambda e: e.tensor_scalar(out=sc2p_bc[:], in0=sc2p_bc[:], scalar1=1.0, scalar2=None, op0=ALU.add),
             reads=["sc2p_bc"], writes=["sc2p_bc"])
        l1g_bc = bc_tile("l1g_bc", ln1_g[0, :])
        l1b_bc = bc_tile("l1b_bc", ln1_b[0, :])
        wr_sb = sb("wr_sb", [128, 16, NE])
        S.dma("sp", wr_sb[:], w_router.rearrange("(kc p) n -> p kc n", p=128), chan="wr_sb", writes=["wr_sb"])
        br_bc = sb("br_bc", [128, NE])
        S.dma("sp", br_bc[:], b_router[0, :].partition_broadcast(128), chan="br_bc", writes=["br_bc"])
        ltri_sb = sb("ltri_sb", [128, 128])
        S.dma("sp", ltri_sb[:], ltri, chan="ltri", writes=["ltri"])
        ones_sq = sb("ones_sq", [128, 128])
        S.op("pool", lambda e: e.memset(ones_sq[:], 1.0), writes=["ones_sq"])
        iotae_sb = sb("iotae_sb", [128, NE])
        S.dma("sp", iotae_sb[:], iota_e, chan="iotae", writes=["iotae"])
        iota32 = sb("iota32", [128, NE])
        S.dma("sp", iota32[:], iota_n, chan="iota32", writes=["iota32"])
        tokid_sb = sb("tokid_sb", [128, 16], I32)
        S.dma("sp", tokid_sb[:], tokid, chan="tokid", writes=["tokid"])
        carry = sb("carry", [128, NE])
        S.op("pool", lambda e: e.memset(carry[:], 0.0), writes=["carry"])
        zt = sb("zt", [128, D])
        S.op("pool", lambda e: e.memset(zt[:], 0.0), writes=["zt"])
        for r in range(17):
            S.dma("sp", yacc[r * 128:(r + 1) * 128, :], zt[:], chan="zt", reads=["zt"], writes=["yacc"])
        ztb = sb("ztb", [128, D], BF16)
        S.op("pool", lambda e: e.memset(ztb[:], 0.0), writes=["ztb"])
        S.dma("sp", h2buf[T:T + 128, :], ztb[:], chan="ztb", reads=["ztb"], writes=["h2dummy"])
        li_sb = sb("li_sb", [128, 1024], I32)
        S.dma("sp", li_sb[:], list_init.rearrange("(p r) two -> p (r two)", p=128), chan="li_sb", writes=["li_sb"])
        S.dma("sp", lists.rearrange("(p r) two -> p (r two)", p=128), li_sb[:], chan="li_sb", reads=["li_sb"], writes=["lists"])

        YTT = [sb("ytt%d" % i, [128, 16, 128], BF16) for i in range(2)]
        XA = [sb("xa%d" % i, [128, D]) for i in range(2)]
        x1pre = sb("x1pre", [128, D])
        x1t = sb("x1t", [128, D])
        h2t = sb("h2t", [128, D])
        h2b = sb("h2b", [128, D], BF16)
        h2T = sb("h2T", [128, 16, 128])
        stats2 = sb("stats2", [128, 4, 6])
        mv2 = sb("mv2", [128, 2])
        rstd2 = sb("rstd2", [128, 1])
        lg = sb("lg", [128, NE])
        max8 = sb("max8", [128, 8])
        idx8 = sb("idx8", [128, 8], U32)
        idxf = sb("idxf", [128, 8])
        negm = sb("negm", [128, 1])
        ew = sb("ew", [128, 4])
        den = sb("den", [128, 1])
        w4 = sb("w4", [128, 4])
        maskt = sb("maskt", [128, NE])
        slotf = sb("slotf", [128, NE])
        oh = sb("oh", [128, NE])
        sl4 = sb("sl4", [128, 4])
        sl4i = sb("sl4i", [128, 4], I32)
        pairs = sb("pairs", [128, 16, 4, 2], I32)

        def ln_stats(src, skey, st_t, mv_t, rs_t, tag):
            for j in range(4):
                S.op("dve", lambda e, j=j: e.bn_stats(out=st_t[:, j, :], in_=src[:, j * 512:(j + 1) * 512]),
                     reads=[skey], writes=[(tag + "st", j)])
            S.op("dve", lambda e: e.bn_aggr(out=mv_t[:], in_=st_t[:].rearrange("p a b -> p (a b)")),
                 reads=[(tag + "st", j) for j in range(4)], writes=[tag + "mv"])
            S.op("act", lambda e: e.activation(out=rs_t[:], in_=mv_t[:, 1:2], func=AF.Sqrt, bias=eps_sb[:, 0:1], scale=1.0),
                 reads=[tag + "mv", "eps"], writes=[tag + "rs0"])
            S.op("dve", lambda e: e.reciprocal(out=rs_t[:], in_=rs_t[:]), reads=[tag + "rs0"], writes=[tag + "rs"])

        for ti in range(16):
            b2 = ti % 2
            yk, xk = "ytt%d" % b2, "xa%d" % b2
            S.dma("sp", YTT[b2][:], ybufT[:, :, ti * 128:(ti + 1) * 128].rearrange("c p t -> p c t"), chan=yk,
                  reads=[("ybufT", ti // 4)], writes=[yk])
            S.dma("sp", XA[b2][:], x_own[ti * 128:(ti + 1) * 128, :], chan=xk, writes=[xk])
            for j in range(4):
                ps, pk = next_pb()
                for kc in range(16):
                    S.op("pe", lambda e, kc=kc, j=j, ps=ps, b2=b2: e.matmul(ps[:, :], lhsT=YTT[b2][:, kc, :], rhs=wout[:, kc, j * 512:(j + 1) * 512],
                                                                          start=(kc == 0), stop=(kc == 15)),
                         reads=[yk, "wout"], writes=[pk])
                S.op("dve", lambda e, j=j, ps=ps: e.tensor_tensor(out=x1pre[:, j * 512:(j + 1) * 512], in0=ps[:, :], in1=g1_bc[:, j * 512:(j + 1) * 512], op=ALU.mult),
                     reads=[pk, "g1_bc"], writes=[("x1pre", j)])
                S.op("dve", lambda e, j=j, b2=b2: e.scalar_tensor_tensor(out=x1pre[:, j * 512:(j + 1) * 512], in0=XA[b2][:, j * 512:(j + 1) * 512], scalar=ALPHA,
                                                                       in1=x1pre[:, j * 512:(j + 1) * 512], op0=ALU.mult, op1=ALU.add),
                     reads=[xk, ("x1pre", j)], writes=[("x1pre", j)])
            xpk = [("x1pre", j) for j in range(4)]
            for j in range(4):
                S.op("dve", lambda e, j=j: e.bn_stats(out=stats2[:, j, :], in_=x1pre[:, j * 512:(j + 1) * 512]),
                     reads=[("x1pre", j)], writes=[("ast", j)])
            S.op("dve", lambda e: e.bn_aggr(out=mv2[:], in_=stats2[:].rearrange("p a b -> p (a b)")),
                 reads=[("ast", j) for j in range(4)], writes=["amv"])
            S.op("act", lambda e: e.activation(out=rstd2[:], in_=mv2[:, 1:2], func=AF.Sqrt, bias=eps_sb[:, 0:1], scale=1.0),
                 reads=["amv", "eps"], writes=["ars0"])
            S.op("dve", lambda e: e.reciprocal(out=rstd2[:], in_=rstd2[:]), reads=["ars0"], writes=["ars"])
            S.op("dve", lambda e: e.tensor_scalar(out=x1t[:], in0=x1pre[:], scalar1=mv2[:, 0:1], scalar2=rstd2[:, 0:1], op0=ALU.subtract, op1=ALU.mult),
                 reads=xpk + ["amv", "ars"], writes=["x1t"])
            S.op("pool", lambda e: e.tensor_tensor(out=x1t[:], in0=x1t[:], in1=l1g_bc[:], op=ALU.mult), reads=["x1t", "l1g_bc"], writes=["x1t"])
            S.op("pool", lambda e: e.tensor_tensor(out=x1t[:], in0=x1t[:], in1=l1b_bc[:], op=ALU.add), reads=["x1t", "l1b_bc"], writes=["x1t"])
            S.dma("sp", x1buf[ti * 128:(ti + 1) * 128, :], x1t[:], chan="x1t", reads=["x1t"], writes=[("x1buf", ti)])
            ln_stats(x1t, "x1t", stats2, mv2, rstd2, "b")
            S.op("dve", lambda e: e.tensor_scalar(out=h2t[:], in0=x1t[:], scalar1=mv2[:, 0:1], scalar2=rstd2[:, 0:1], op0=ALU.subtract, op1=ALU.mult),
                 reads=["x1t", "bmv", "brs"], writes=["h2t"])
            S.op("pool", lambda e: e.tensor_tensor(out=h2t[:], in0=h2t[:], in1=sc2p_bc[:], op=ALU.mult), reads=["h2t", "sc2p_bc"], writes=["h2t"])
            S.op("pool", lambda e: e.tensor_tensor(out=h2t[:], in0=h2t[:], in1=sh2_bc[:], op=ALU.add), reads=["h2t", "sh2_bc"], writes=["h2t"])
            S.op("act", lambda e: e.activation(out=h2b[:], in_=h2t[:], func=AF.Copy), reads=["h2t"], writes=["h2b"])
            S.dma("sp", h2buf[ti * 128:(ti + 1) * 128, :], h2b[:], chan="h2b", reads=["h2b"], writes=[("h2buf", ti)])
            for q4 in range(4):
                ps, pk = next_pb()
                for j in range(4):
                    kc = q4 * 4 + j
                    S.op("pe", lambda e, kc=kc, j=j, ps=ps: e.transpose(ps[:, j * 128:(j + 1) * 128], h2t[:, kc * 128:(kc + 1) * 128], ident32[:]),
                         reads=["h2t", "ident32"], writes=[pk])
                S.op("act", lambda e, q4=q4, ps=ps: e.activation(out=h2T[:, q4 * 4:(q4 + 1) * 4, :], in_=ps[:, :].rearrange("p (j t) -> p j t", j=4), func=AF.Copy),
                     reads=[pk], writes=[("h2T", q4)])
            ps, pk = next_pb()
            for kc in range(16):
                S.op("pe", lambda e, kc=kc, ps=ps: e.matmul(ps[:, 0:NE], lhsT=h2T[:, kc, :], rhs=wr_sb[:, kc, :], start=(kc == 0), stop=(kc == 15)),
                     reads=[("h2T", kc // 4), "wr_sb"], writes=[pk])
            S.op("dve", lambda e, ps=ps: e.tensor_tensor(out=lg[:], in0=ps[:, 0:NE], in1=br_bc[:], op=ALU.add), reads=[pk, "br_bc"], writes=["lg"])
            if debug:
                S.dma("sp", dbg["lg"][ti * 128:(ti + 1) * 128, :], lg[:], chan="lg", reads=["lg"], writes=["dbg_lg"])
            S.op("dve", lambda e: e.max(out=max8[:], in_=lg[:]), reads=["lg"], writes=["max8"])
            S.op("dve", lambda e: e.max_index(out=idx8[:], in_max=max8[:], in_values=lg[:]), reads=["lg", "max8"], writes=["idx8"])
            S.op("dve", lambda e: e.tensor_copy(out=idxf[:], in_=idx8[:]), reads=["idx8"], writes=["idxf"])
            S.op("dve", lambda e: e.tensor_scalar(out=negm[:], in0=max8[:, 0:1], scalar1=-1.0, scalar2=None, op0=ALU.mult), reads=["max8"], writes=["negm"])
            S.op("act", lambda e: e.activation(out=ew[:], in_=max8[:, 0:4], func=AF.Exp, bias=negm[:, 0:1], scale=1.0, accum_out=den[:, 0:1]),
                 reads=["max8", "negm"], writes=["ew", "den"])
            S.op("dve", lambda e: e.reciprocal(out=den[:], in_=den[:]), reads=["den"], writes=["den"])
            S.op("dve", lambda e: e.tensor_scalar(out=w4[:], in0=ew[:], scalar1=den[:, 0:1], scalar2=None, op0=ALU.mult), reads=["ew", "den"], writes=["w4"])
            S.op("dve", lambda e: e.tensor_scalar(out=maskt[:], in0=lg[:], scalar1=max8[:, 3:4], scalar2=None, op0=ALU.is_ge), reads=["lg", "max8"], writes=["maskt"])
            ps, pk = next_pb()
            S.op("pe", lambda e, ps=ps: e.matmul(ps[:, 0:NE], lhsT=ltri_sb[:], rhs=maskt[:], start=True, stop=True), reads=["ltri", "maskt"], writes=[pk])
            S.op("dve", lambda e, ps=ps: e.tensor_tensor(out=slotf[:], in0=ps[:, 0:NE], in1=carry[:], op=ALU.add), reads=[pk, "carry"], writes=["slotf"])
            S.op("dve", lambda e: e.tensor_tensor(out=slotf[:], in0=slotf[:], in1=iotae_sb[:], op=ALU.add), reads=["slotf", "iotae"], writes=["slotf"])
            ps2, pk2 = next_pb()
            S.op("pe", lambda e, ps2=ps2: e.matmul(ps2[:, 0:NE], lhsT=ones_sq[:], rhs=maskt[:], start=True, stop=True), reads=["ones_sq", "maskt"], writes=[pk2])
            S.op("dve", lambda e, ps2=ps2: e.tensor_tensor(out=carry[:], in0=ps2[:, 0:NE], in1=carry[:], op=ALU.add), reads=[pk2, "carry"], writes=["carry"])
            for j in range(4):
                S.op("dve", lambda e, j=j: e.tensor_scalar(out=oh[:], in0=iota32[:], scalar1=idxf[:, j:j + 1], scalar2=None, op0=ALU.is_equal),
                     reads=["iota32", "idxf"], writes=["oh"])
                S.op("dve", lambda e: e.tensor_tensor(out=oh[:], in0=oh[:], in1=slotf[:], op=ALU.mult), reads=["oh", "slotf"], writes=["oh"])
                S.op("dve", lambda e, j=j: e.reduce_sum(out=sl4[:, j:j + 1], in_=oh[:], axis=AX.X), reads=["oh"], writes=["sl4"])
            S.op("dve", lambda e: e.tensor_copy(out=sl4i[:], in_=sl4[:]), reads=["sl4"], writes=["sl4i"])
            S.op("dve", lambda e, ti=ti: e.tensor_copy(out=pairs[:, ti, :, 0], in_=tokid_sb[:, ti:ti + 1].broadcast_to([128, 4])), reads=["tokid"], writes=[("pairs", ti)])
            S.op("dve", lambda e, ti=ti: e.tensor_copy(out=pairs[:, ti, :, 1].bitcast(F32), in_=w4[:]), reads=["w4", ("pairs", ti)], writes=[("pairs", ti)])
            for j in range(4):
                S.dma("pool", lists, pairs[:, ti, j, :], chan=("pairs", ti), reads=[("pairs", ti), "sl4i"], writes=["lists"],
                      indirect=dict(out_offset=bass.IndirectOffsetOnAxis(ap=sl4i[:, j:j + 1], axis=0), in_offset=None))
        flg_f = sb("flg_f", [1, 4 * NE])
        for g_ in range(4):
            S.op("dve", lambda e, g_=g_: e.tensor_scalar(out=flg_f[0:1, g_ * NE:(g_ + 1) * NE], in0=carry[0:1, :], scalar1=float(g_ * GRP),
                                                         scalar2=None, op0=ALU.is_gt), reads=["carry"], writes=["flg_f"])
        S.op("dve", lambda e: e.tensor_copy(out=flg_i[:], in_=flg_f[:]), reads=["flg_f"], writes=["flg_i"])
        S.barrier()
        ph.close()
        if stop_after == "A2":
            print("sems", len(S.dsem), "inst", S.n_inst, "waits", S.n_wait)
            return nc

        ph = ExitStack()
        cur[0] = ph
        WB = [sb("wbm%d" % i, [128, 16, 512], BF16) for i in range(5)]
        wb_i[0] = 0
        NWB = 5

        def load_w2(src_ap):
            i = wb_i[0] % NWB
            wb_i[0] += 1
            k = "wbm%d" % i
            S.dma("pool", WB[i][:, :, :], src_ap.rearrange("(kc p) n -> p kc n", p=128), chan=k, writes=[k])
            return WB[i], k

        lst = sb("lst", [128, 4, 2], I32)
        dum = sb("dum", [1, 64])
        dum_idx = {}
        xg = sb("xg", [128, 4, D], BF16)
        xT = sb("xT", [128, 16, GRP], BF16)
        actT = sb("actT", [128, 16, GRP], BF16)
        Yt = sb("Yt", [128, 4, D])
        bd_bc = sb("bd_bc", [128, D])
        bg_sb = sb("bg_sb", [128, 16])
        bu_sb = sb("bu_sb", [128, 16])
        gt = [sb("gt%d" % i, [128, GRP]) for i in range(2)]
        sgm = [sb("sgm%d" % i, [128, GRP]) for i in range(2)]
        ut = [sb("ut%d" % i, [128, GRP]) for i in range(2)]
        PGA = [(PG[0], "pg0"), (PG[1], "pg1")]
        PUP = [(PO[0], ("po", 0)), (PO[1], ("po", 1))]
        PDN = [(PSC, "psc"), (PS_, "pss")]
        it = [0]
        flag_regs = nc.alloc_registers("flg", engines=mybir.ALL_ENGINES)

        def pass_body(e_, g_):
            base = e_ * CAP + g_ * GRP
            S.dma("sp", lst[:], lists[base:base + GRP, :].rearrange("(b p) two -> p b two", p=128), chan="lst",
                  reads=["lists"], writes=["lst"])
            for blk in range(4):
                S.dma("pool", xg[:, blk, :], h2buf, chan="xg", reads=["lst", "h2dummy"] + [("h2buf", ti) for ti in range(16)],
                      writes=[("xg", blk)],
                      indirect=dict(out_offset=None, in_offset=bass.IndirectOffsetOnAxis(ap=lst[:, blk, 0:1], axis=0)))
            for blk in range(4):
                S.lastw[("xg", blk)] = ("dma", "xg", S.dcnt["xg"])
            for blk in range(4):
                for half in range(2):
                    PT = PTS[half]
                    ptk = "pt%d" % half
                    for j in range(8):
                        kc = half * 8 + j
                        S.op("pe", lambda e, kc=kc, j=j, PT=PT, blk=blk: e.transpose(PT[:, j * 128:(j + 1) * 128], xg[:, blk, kc * 128:(kc + 1) * 128], identb[:]),
                             reads=[("xg", blk), "identb"], writes=[ptk])
                    eng = "act" if half == 0 else "dve"
                    if eng == "act":
                        S.op("act", lambda e, PT=PT, blk=blk, half=half: e.activation(
                            out=xT[:, half * 8:(half + 1) * 8, blk * 128:(blk + 1) * 128], in_=PT[:, :].rearrange("p (j t) -> p j t", j=8), func=AF.Copy),
                            reads=[ptk], writes=[("xT", blk)])
                    else:
                        S.op("dve", lambda e, PT=PT, blk=blk, half=half: e.tensor_copy(
                            out=xT[:, half * 8:(half + 1) * 8, blk * 128:(blk + 1) * 128], in_=PT[:, :].rearrange("p (j t) -> p j t", j=8)),
                            reads=[ptk], writes=[("xT", blk)])
            xkeys = [("xT", blk) for blk in range(4)]
            for fq in range(4):
                wg, wgk = load_w2(w_gate[e_, :, fq * 512:(fq + 1) * 512])
                wu, wuk = load_w2(w_up[e_, :, fq * 512:(fq + 1) * 512])
                for m in range(4):
                    fc = fq * 4 + m
                    i2 = it[0] % 2
                    it[0] += 1
                    pg_, pgk = PGA[i2]
                    pu_, puk = PUP[i2]
                    for kc in range(16):
                        S.op("pe", lambda e, kc=kc, m=m, wg=wg, pg_=pg_: e.matmul(pg_[:, :], lhsT=wg[:, kc, m * 128:(m + 1) * 128], rhs=xT[:, kc, :],
                                                                                start=(kc == 0), stop=(kc == 15)), reads=[wgk] + xkeys, writes=[pgk])
                    for kc in range(16):
                        S.op("pe", lambda e, kc=kc, m=m, wu=wu, pu_=pu_: e.matmul(pu_[:, :], lhsT=wu[:, kc, m * 128:(m + 1) * 128], rhs=xT[:, kc, :],
                                                                                start=(kc == 0), stop=(kc == 15)), reads=[wuk] + xkeys, writes=[puk])
                    gk_, sk_, uk_ = "gt%d" % i2, "sgm%d" % i2, "ut%d" % i2
                    S.op("dve", lambda e, fc=fc, i2=i2, pg_=pg_: e.tensor_scalar(out=gt[i2][:], in0=pg_[:, :], scalar1=bg_sb[:, fc:fc + 1], scalar2=7.0,
                                                                               op0=ALU.add, op1=ALU.min), reads=[pgk, "bg_sb"], writes=[gk_])
                    S.op("act", lambda e, i2=i2: e.activation(out=sgm[i2][:], in_=gt[i2][:], func=AF.Sigmoid, scale=1.702), reads=[gk_], writes=[sk_])
                    S.op("dve", lambda e, fc=fc, i2=i2, pu_=pu_: e.tensor_scalar(out=ut[i2][:], in0=pu_[:, :], scalar1=bu_sb[:, fc:fc + 1], scalar2=7.0,
                                                                               op0=ALU.add, op1=ALU.min), reads=[puk, "bu_sb"], writes=[uk_])
                    S.op("dve", lambda e, i2=i2: e.tensor_scalar(out=ut[i2][:], in0=ut[i2][:], scalar1=-7.0, scalar2=1.0, op0=ALU.max, op1=ALU.add),
                         reads=[uk_], writes=[uk_])
                    S.op("dve", lambda e, i2=i2: e.tensor_tensor(out=gt[i2][:], in0=gt[i2][:], in1=sgm[i2][:], op=ALU.mult), reads=[gk_, sk_], writes=[gk_])
                    S.op("dve", lambda e, i2=i2, fc=fc: e.tensor_tensor(out=actT[:, fc, :], in0=gt[i2][:], in1=ut[i2][:], op=ALU.mult),
                         reads=[gk_, uk_], writes=[("actT", fc)])
            akeys = [("actT", fc) for fc in range(16)]
            for dq in range(4):
                wd, wdk = load_w2(w_down[e_, :, dq * 512:(dq + 1) * 512])
                for blk in range(4):
                    i2 = it[0] % 2
                    it[0] += 1
                    pd_, pdk = PDN[i2]
                    for fc in range(16):
                        S.op("pe", lambda e, fc=fc, blk=blk, wd=wd, pd_=pd_: e.matmul(pd_[:, :], lhsT=actT[:, fc, blk * 128:(blk + 1) * 128], rhs=wd[:, fc, :],
                                                                                    start=(fc == 0), stop=(fc == 15)), reads=[wdk] + akeys, writes=[pdk])
                    S.op("dve", lambda e, blk=blk, dq=dq, pd_=pd_: e.tensor_tensor(out=Yt[:, blk, dq * 512:(dq + 1) * 512], in0=pd_[:, :],
                                                                                 in1=bd_bc[:, dq * 512:(dq + 1) * 512], op=ALU.add),
                         reads=[pdk, "bd_bc"], writes=[("Yt", blk)])
                    S.op("dve", lambda e, blk=blk, dq=dq: e.tensor_scalar(out=Yt[:, blk, dq * 512:(dq + 1) * 512], in0=Yt[:, blk, dq * 512:(dq + 1) * 512],
                                                                         scalar1=lst[:, blk, 1:2].bitcast(F32), scalar2=None, op0=ALU.mult),
                         reads=[("Yt", blk), "lst"], writes=[("Yt", blk)])
            for blk in range(4):
                S.dma("pool", yacc, Yt[:, blk, :], chan="Ysc", reads=[("Yt", blk), "lst"], writes=["yacc"],
                      indirect=dict(out_offset=bass.IndirectOffsetOnAxis(ap=lst[:, blk, 0:1], axis=0), in_offset=None, compute_op=ALU.add))
            fin_ = ("dma", "Ysc", S.dcnt["Ysc"])
            for blk in range(4):
                S.readers[("Yt", blk)] = [fin_]
            S.readers["lst"] = [fin_]
            S.lastw["yacc"] = fin_

        def bump_skipped(snap_cnt, snap_d):
            for en_ in S.eng:
                dn = S.cnt[en_] - snap_cnt[en_]
                if dn > 0:
                    if snap_cnt[en_] > 0:
                        S.eng[en_].wait_ge(S.sem[en_], snap_cnt[en_])
                    S.eng[en_].sem_inc(S.sem[en_], dn)
            ci_ = 0
            for k_ in S.dcnt:
                dd = S.dcnt[k_] - snap_d.get(k_, 0)
                if dd > 0:
                    if S.dq[k_] == "pool":
                        if snap_d.get(k_, 0) > 0:
                            nc.gpsimd.wait_ge(S.dsem[k_], snap_d[k_])
                        ci_ = dum_idx.setdefault(k_, len(dum_idx))
                        j_ = 0
                        while dd > 0:
                            d1 = min(dd, 96)
                            nc.gpsimd.dma_start(out=dum[0:1, ci_ * 4 + j_:ci_ * 4 + j_ + 1], in_=flag[0:1, 0:1]).then_inc(S.dsem[k_], d1)
                            dd -= d1
                            j_ += 1
                    else:
                        if snap_d.get(k_, 0) > 0:
                            nc.sync.wait_ge(S.dsem[k_], snap_d[k_])
                        nc.sync.sem_inc(S.dsem[k_], dd)

        def guarded(e_, g_):
            nc.regs_load(flag_regs, flg_i[0:1, g_ * NE + e_: g_ * NE + e_ + 1])
            snap_cnt = dict(S.cnt)
            snap_d = dict(S.dcnt)
            snap_seen = {k_: dict(v_) for k_, v_ in S.seen.items()}
            with nc.If_cmp(flag_regs, 0, "IS_NE"):
                pass_body(e_, g_)
                if g_ + 1 < n_groups:
                    guarded(e_, g_ + 1)
            with nc.Else():
                bump_skipped(snap_cnt, snap_d)
            S.seen = snap_seen

        for e_ in range(NE):
            S.dma("sp", bd_bc[:], b_down[e_, :].partition_broadcast(128), chan="bd_bc", writes=["bd_bc"])
            S.dma("sp", bg_sb[:], b_gate[e_], chan="bg_sb", writes=["bg_sb"])
            S.dma("sp", bu_sb[:], b_up[e_], chan="bu_sb", writes=["bu_sb"])
            guarded(e_, 0)
        S.barrier()
        ph.close()

        ph = ExitStack()
        cur[0] = ph
        g2_bc = bc_tile("g2_bc", moddram[0, 5 * D:6 * D])
        l2g_bc = bc_tile("l2g_bc", ln2_g[0, :])
        l2b_bc = bc_tile("l2b_bc", ln2_b[0, :])
        YA = [sb("ya%d" % i, [128, D]) for i in range(2)]
        X1 = [sb("x1_%d" % i, [128, D]) for i in range(2)]
        OT = [sb("ot%d" % i, [128, D]) for i in range(2)]
        stats3 = sb("stats3", [128, 4, 6])
        mv3 = sb("mv3", [128, 2])
        rstd3 = sb("rstd3", [128, 1])
        for ti in range(16):
            b2 = ti % 2
            yk, xk, ok = "ya%d" % b2, "x1_%d" % b2, "ot%d" % b2
            S.dma("sp", YA[b2][:], yacc[ti * 128:(ti + 1) * 128, :], chan=yk, reads=["yacc"], writes=[yk])
            S.dma("sp", X1[b2][:], x1buf[ti * 128:(ti + 1) * 128, :], chan=xk, reads=[("x1buf", ti)], writes=[xk])
            S.op("pool", lambda e, b2=b2: e.tensor_tensor(out=YA[b2][:], in0=YA[b2][:], in1=g2_bc[:], op=ALU.mult), reads=[yk, "g2_bc"], writes=[yk])
            S.op("dve", lambda e, b2=b2: e.scalar_tensor_tensor(out=YA[b2][:], in0=X1[b2][:], scalar=ALPHA, in1=YA[b2][:], op0=ALU.mult, op1=ALU.add),
                 reads=[xk, yk], writes=[yk])
            ln_stats(YA[b2], yk, stats3, mv3, rstd3, "c")
            S.op("dve", lambda e, b2=b2: e.tensor_scalar(out=OT[b2][:], in0=YA[b2][:], scalar1=mv3[:, 0:1], scalar2=rstd3[:, 0:1], op0=ALU.subtract, op1=ALU.mult),
                 reads=[yk, "cmv", "crs"], writes=[ok])
            S.op("pool", lambda e, b2=b2: e.tensor_tensor(out=OT[b2][:], in0=OT[b2][:], in1=l2g_bc[:], op=ALU.mult), reads=[ok, "l2g_bc"], writes=[ok])
            S.op("pool", lambda e, b2=b2: e.tensor_tensor(out=OT[b2][:], in0=OT[b2][:], in1=l2b_bc[:], op=ALU.add), reads=[ok, "l2b_bc"], writes=[ok])
            S.dma("sp", out[ti * 128:(ti + 1) * 128, :], OT[b2][:], chan=ok, reads=[ok], writes=["out"])
        S.barrier()
        ph.close()
        print("sems", len(S.dsem), "inst", S.n_inst, "waits", S.n_wait)
    return nc


def host_consts():
    ident = np.eye(128, dtype=np.float32)
    triu2 = np.zeros((128, 128), np.float32)
    for b in range(2):
        triu2[b * 64:(b + 1) * 64, b * 64:(b + 1) * 64] = np.triu(np.ones((64, 64), np.float32))
    rmask = np.ones((128, SEG), np.float32)
    rmask[:, ::64] = 0.0
    ltri = np.triu(np.ones((128, 128), np.float32), 1)
    iota_e = np.tile((np.arange(NE, dtype=np.float32) * CAP)[None, :], (128, 1))
    tokid = (np.arange(16, dtype=np.int32)[None, :] * 128 + np.arange(128, dtype=np.int32)[:, None]).astype(np.int32)
    iota_n = np.tile(np.arange(NE, dtype=np.float32)[None, :], (128, 1))
    list_init = np.zeros((NE * CAP, 2), np.int32)
    list_init[:, 0] = T + (np.arange(NE * CAP) % 128)
    return dict(ident_f=ident, triu2=triu2, rmask=rmask, ltri=ltri, iota_e=iota_e, tokid=tokid, iota_n=iota_n, list_init=list_init)


def make_in_maps(inp):
    f = lambda a: np.ascontiguousarray(a, dtype=np.float32)
    x = inp["x"]
    consts = host_consts()
    shared = dict(
        w_ada=f(inp["w_ada"][0]), b_ada=f(inp["b_ada"][0][None, :]), w_in=f(inp["w_in"][0]), w_gk=f(inp["w_gk"][0]),
        b_gk=f(inp["b_gk"][0].reshape(4, 128).T), w_pool=f(inp["w_pool"][0]),
        b_pool=f(inp["b_pool"][0].reshape(8, 128).T), pool_scale=f(inp["pool_scale"][0].reshape(8, 128).T),
        gla_norm_w=f(inp["gla_norm_w"][0][None, :]), w_out=f(inp["w_out"][0]),
        ln1_g=f(inp["ln1_g"][0][None, :]), ln1_b=f(inp["ln1_b"][0][None, :]),
        w_router=f(inp["w_router"][0]), b_router=f(inp["b_router"][0][None, :]),
        w_gate=f(inp["w_gate"][0]), b_gate=f(inp["b_gate"][0].reshape(NE, 16, 128).transpose(0, 2, 1)),
        w_up=f(inp["w_up"][0]), b_up=f(inp["b_up"][0].reshape(NE, 16, 128).transpose(0, 2, 1)),
        w_down=f(inp["w_down"][0]), b_down=f(inp["b_down"][0]),
        ln2_g=f(inp["ln2_g"][0][None, :]), ln2_b=f(inp["ln2_b"][0][None, :]),
    )
    shared.update(consts)
    maps = []
    for core in range(8):
        b, half = core // 2, core % 2
        m = dict(shared)
        m["x_own"] = f(x[b, half * T:(half + 1) * T, :])
        m["x_pre"] = f(x[b, 0:T, :])
        m["c_l"] = f(inp["c"][b].reshape(16, 128).T)
        m["flag"] = np.full((128, 1), float(half), np.float32)
        ic = np.zeros((128, 4, 16), np.float32)
        for g in range(4):
            w = 2 ** (g + 1)
            pos = np.arange(1, 17, dtype=np.float32)
            ic[:, g, :] = (1.0 / np.minimum(pos, w) if half == 0 else np.full(16, 1.0 / w, np.float32))[None, :]
        m["invcnt"] = ic
        maps.append(m)
    return maps


N_GROUPS = 4


def kernel(**inputs):
    nc = build_program(n_groups=N_GROUPS)
    in_maps = make_in_maps(inputs)
    res = run_bass_kernel_spmd(nc, in_maps, core_ids=list(range(8)))
    outs = [res.results[i]["out"] for i in range(8)]
    full = np.stack([np.concatenate([outs[2 * b], outs[2 * b + 1]], axis=0) for b in range(4)], axis=0)
    return full.astype(np.float32)
```

```python
from contextlib import ExitStack
import numpy as np
import concourse.bass as bass
import concourse.mybir as mybir
from concourse.bass_utils import run_bass_kernel_spmd

F32 = mybir.dt.float32
BF16 = mybir.dt.bfloat16
I32 = mybir.dt.int32
U32 = mybir.dt.uint32
AF = mybir.ActivationFunctionType
ALU = mybir.AluOpType
AX = mybir.AxisListType

D = 2048
T = 2048
SEG = 512
NSEG = T // SEG
NE = 32
CAP = 2048
GRP = 512
ALPHA = 2.0 ** 0.25
LN_EPS = 1e-5
IN_W = 4112


class _Stop(Exception):
    pass


class Sched:
    def __init__(self, nc, stack):
        self.nc = nc
        self.eng = {"pe": nc.tensor, "act": nc.scalar, "dve": nc.vector,
                    "pool": nc.gpsimd, "sp": nc.sync}
        self.sem = {}
        self.cnt = {}
        self.stack = stack
        for e in self.eng:
            self.sem[e] = stack.enter_context(nc.semaphore("prog_" + e))
            self.cnt[e] = 0
        self.dsem = {}
        self.dcnt = {}
        self.dq = {}
        self.seen = {e: {} for e in self.eng}
        self.lastw = {}
        self.readers = {}
        self.n_wait = 0
        self.n_inst = 0

    def _chan(self, key):
        if key not in self.dsem:
            self.dsem[key] = self.stack.enter_context(
                self.nc.semaphore("d_" + str(len(self.dsem))))
            self.dcnt[key] = 0
        return self.dsem[key]

    def _wait(self, e, tok):
        kind, k, c = tok
        semkey = (kind, k)
        if self.seen[e].get(semkey, 0) >= c:
            return
        sem = self.sem[k] if kind == "eng" else self.dsem[k]
        self.eng[e].wait_ge(sem, c)
        self.seen[e][semkey] = c
        self.n_wait += 1

    def _deps(self, e, reads, writes, skip_same=False):
        toks = []
        for r in reads:
            w = self.lastw.get(r)
            if w is not None:
                toks.append(w)
        for w_ in writes:
            w = self.lastw.get(w_)
            if w is not None:
                toks.append(w)
            toks.extend(self.readers.get(w_, []))
        for t in toks:
            if skip_same and t[0] == "eng" and t[1] == e:
                continue
            self._wait(e, t)

    def _record(self, tok, reads, writes):
        for r in reads:
            self.readers.setdefault(r, []).append(tok)
        for w in writes:
            self.lastw[w] = tok
            self.readers[w] = []

    def op(self, e, fn, reads=(), writes=()):
        self._deps(e, reads, writes, skip_same=(e == "pe"))
        ins = fn(self.eng[e])
        self.cnt[e] += 1
        ins.then_inc(self.sem[e], 1)
        self._record(("eng", e, self.cnt[e]), reads, writes)
        self.n_inst += 1
        return ins

    def dma(self, q, out, in_, chan, reads=(), writes=(), indirect=None, **kw):
        self._deps(q, reads, writes)
        sem = self._chan(chan)
        if indirect is None:
            ins = self.eng[q].dma_start(out=out, in_=in_, **kw)
        else:
            ins = self.eng[q].indirect_dma_start(out=out, in_=in_, **indirect)
        self.dcnt[chan] += 16
        self.dq[chan] = q
        ins.then_inc(sem, 16)
        self._record(("dma", chan, self.dcnt[chan]), reads, writes)
        self.n_inst += 1
        return ins

    def barrier(self):
        for e in self.eng:
            for o in self.eng:
                if self.cnt[o] > 0:
                    self._wait(e, ("eng", o, self.cnt[o]))
            for k, c in self.dcnt.items():
                if c > 0:
                    self._wait(e, ("dma", k, c))

    def finish(self, e="sp"):
        for k, w in list(self.lastw.items()):
            if w is not None:
                self._wait(e, w)
            for r in self.readers.get(k, []):
                self._wait(e, r)


def build_program(debug=False, n_groups=4, stop_after=None):
    nc = bass.Bass("TRN2", target_bir_lowering=False)

    in_names = []
    lean = stop_after in ("A0", "A1", "A1s", "A1a", "A1b", "A1c", "A1d", "A2")

    def din(name, shape, dt=F32):
        if lean and name in ("w_gate", "w_up", "w_down"):
            return None
        in_names.append(name)
        return nc.dram_tensor(name, list(shape), dt, kind="ExternalInput").ap()

    x_own = din("x_own", [T, D])
    x_pre = din("x_pre", [T, D])
    c_l = din("c_l", [128, 16])
    flag = din("flag", [128, 1])
    invcnt = din("invcnt", [128, 4, 16])
    ident_f = din("ident_f", [128, 128])
    triu2 = din("triu2", [128, 128])
    rmask = din("rmask", [128, SEG])
    ltri = din("ltri", [128, 128])
    iota_e = din("iota_e", [128, NE])
    tokid = din("tokid", [128, 16], I32)
    iota_n = din("iota_n", [128, NE])
    list_init = din("list_init", [NE * CAP, 2], I32)
    w_ada = din("w_ada", [D, 6 * D])
    b_ada = din("b_ada", [1, 6 * D])
    w_in = din("w_in", [D, IN_W])
    w_gk = din("w_gk", [16, 512])
    b_gk = din("b_gk", [128, 4])
    w_pool = din("w_pool", [4, 256, 256])
    b_pool = din("b_pool", [128, 8])
    pool_scale = din("pool_scale", [128, 8])
    gla_norm_w = din("gla_norm_w", [1, 256])
    w_out = din("w_out", [D, D])
    ln1_g = din("ln1_g", [1, D])
    ln1_b = din("ln1_b", [1, D])
    w_router = din("w_router", [D, NE])
    b_router = din("b_router", [1, NE])
    w_gate = din("w_gate", [NE, D, D])
    b_gate = din("b_gate", [NE, 128, 16])
    w_up = din("w_up", [NE, D, D])
    b_up = din("b_up", [NE, 128, 16])
    w_down = din("w_down", [NE, D, D])
    b_down = din("b_down", [NE, D])
    ln2_g = din("ln2_g", [1, D])
    ln2_b = din("ln2_b", [1, D])

    out = nc.dram_tensor("out", [T, D], F32, kind="ExternalOutput").ap()

    def dscratch(name, shape, dt):
        return nc.dram_tensor(name, list(shape), dt, kind="Internal").ap()

    ybufT = dscratch("ybufT", [16, 128, T], BF16)
    x1buf = dscratch("x1buf", [T, D], F32)
    h2buf = dscratch("h2buf", [T + 128, D], BF16)
    lists = dscratch("lists", [NE * CAP, 2], I32)
    yacc = dscratch("yacc", [T + 128, D], F32)
    moddram = dscratch("moddram", [1, 6 * D], F32)
    dbg = None
    if debug:
        dbg = {
            "x1": nc.dram_tensor("dbg_x1", [T, D], F32, kind="ExternalOutput").ap(),
            "yT": nc.dram_tensor("dbg_yT", [16, 128, T], BF16, kind="ExternalOutput").ap(),
            "lg": nc.dram_tensor("dbg_lg", [T, NE], F32, kind="ExternalOutput").ap(),
        }

    nc.in_names = in_names
    with ExitStack() as st:
        S = Sched(nc, st)

        def sb(name, shape, dt=F32):
            return st.enter_context(nc.sbuf_tensor(name, list(shape), dt))

        def pst(name, shape, dt=F32):
            return st.enter_context(nc.psum_tensor(name, list(shape), dt))

        ident32 = sb("ident32", [128, 128])
        identb = sb("identb", [128, 128], BF16)
        triu_sb = sb("triu_sb", [128, 128])
        rmask_sb = sb("rmask_sb", [128, SEG])
        flag_sb = sb("flag_sb", [128, 1])
        invc_sb = sb("invc_sb", [128, 4, 16])
        ones_row = sb("ones_row", [1, 128])
        one11 = sb("one11", [1, 1])
        eps_sb = sb("eps_sb", [128, 1])
        S.dma("sp", ident32[:], ident_f, chan="ident32", writes=["ident32"])
        S.dma("pool", identb[:], ident_f, chan="identb", writes=["identb"])
        S.dma("sp", triu_sb[:], triu2, chan="triu", writes=["triu"])
        S.dma("sp", rmask_sb[:], rmask, chan="rmask", writes=["rmask"])
        S.dma("sp", flag_sb[:], flag, chan="flag", writes=["flag"])
        S.dma("sp", invc_sb[:], invcnt, chan="invc", writes=["invc"])
        S.op("dve", lambda e: e.memset(ones_row[:], 1.0), writes=["ones_row"])
        S.op("dve", lambda e: e.memset(one11[:], 1.0), writes=["one11"])
        S.op("dve", lambda e: e.memset(eps_sb[:], LN_EPS), writes=["eps"])

        PG = [pst("pg%d" % i, [128, 512]) for i in range(2)]
        PTS = [pst("ptr%d" % i, [128, 1024], BF16) for i in range(2)]
        PSC = pst("psc", [128, 512])
        PO = [pst("po%d" % i, [128, 512]) for i in range(2)]
        PS_ = pst("pss", [128, 512])
        pg_i = [0]

        def next_pg():
            i = pg_i[0] % 2
            pg_i[0] += 1
            return PG[i], "pg%d" % i

        flg_i = sb("flg_i", [1, 4 * NE], I32)
        cur = [st]

        def sb(name, shape, dt=F32):
            return cur[0].enter_context(nc.sbuf_tensor(name, list(shape), dt))

        ph = ExitStack()
        cur[0] = ph
        c_sb = sb("c_sb", [128, 16])
        sc_sb = sb("sc_sb", [128, 16])
        S.dma("sp", c_sb[:], c_l, chan="c_sb", writes=["c_sb"])
        S.op("act", lambda e: e.activation(out=sc_sb[:], in_=c_sb[:], func=AF.Silu),
             reads=["c_sb"], writes=["sc_sb"])
        WF = [sb("wf%d" % i, [128, 16, 512]) for i in range(2)]
        brow = [sb("brow%d" % i, [1, 512]) for i in range(2)]
        mrow = [sb("mrow%d" % i, [1, 512]) for i in range(2)]
        for j in range(24):
            i = j % 2
            wk = "wf%d" % i
            S.dma("sp", WF[i][:], w_ada[:, j * 512:(j + 1) * 512].rearrange("(kc p) n -> p kc n", p=128),
                  chan=wk, writes=[wk])
            S.dma("sp", brow[i][:], b_ada[0:1, j * 512:(j + 1) * 512], chan="brow%d" % i, writes=["brow%d" % i])
            ps, pk = next_pg()
            for kc in range(16):
                S.op("pe", lambda e, kc=kc, i=i, ps=ps: e.matmul(ps[0:1, :], lhsT=sc_sb[:, kc:kc + 1], rhs=WF[i][:, kc, :],
                                                                 start=(kc == 0), stop=(kc == 15)),
                     reads=[wk, "sc_sb"], writes=[pk])
            S.op("dve", lambda e, i=i, ps=ps: e.tensor_tensor(out=mrow[i][:], in0=ps[0:1, :], in1=brow[i][:], op=ALU.add),
                 reads=[pk, "brow%d" % i], writes=["mrow%d" % i])
            S.dma("sp", moddram[0:1, j * 512:(j + 1) * 512], mrow[i][:], chan="mrow%d" % i, reads=["mrow%d" % i],
                  writes=["moddram"])
        S.barrier()
        ph.close()
        if stop_after == "A0":
            print("sems", len(S.dsem), "inst", S.n_inst, "waits", S.n_wait)
            return nc

        ph = ExitStack()
        cur[0] = ph
        sh1_fm = sb("sh1_fm", [128, 16])
        sc1p_fm = sb("sc1p_fm", [128, 16])
        with nc.allow_non_contiguous_dma(reason="tiny strided vector load"):
            S.dma("sp", sh1_fm[:], moddram[0, 0:D].rearrange("(kc p) -> p kc", p=128), chan="sh1_fm",
                  reads=["moddram"], writes=["sh1_fm"])
            S.dma("sp", sc1p_fm[:], moddram[0, D:2 * D].rearrange("(kc p) -> p kc", p=128), chan="sc1p_fm",
                  reads=["moddram"], writes=["sc1p_0"])
        S.op("dve", lambda e: e.tensor_scalar(out=sc1p_fm[:], in0=sc1p_fm[:], scalar1=1.0, scalar2=None, op0=ALU.add),
             reads=["sc1p_0"], writes=["sc1p_fm"])
        wgkin = sb("wgkin", [128, 16, 16], BF16)
        S.dma("pool", wgkin[:], w_in[:, 3072:3088].rearrange("(kc p) n -> p kc n", p=128),
              chan="wgkin", writes=["wgkin"])
        wgk_sb = sb("wgk_sb", [16, 512])
        S.dma("sp", wgk_sb[:], w_gk, chan="wgk", writes=["wgk"])
        nbgk = sb("nbgk", [128, 4])
        S.dma("sp", nbgk[:], b_gk, chan="nbgk", writes=["nbgk0"])
        S.op("dve", lambda e: e.tensor_scalar(out=nbgk[:], in0=nbgk[:], scalar1=-1.0, scalar2=None, op0=ALU.mult),
             reads=["nbgk0"], writes=["nbgk"])
        wpool_sb = sb("wpool_sb", [128, 4, 2, 256], BF16)
        S.dma("pool", wpool_sb[:], w_pool.rearrange("g (cc p) d -> p g cc d", p=128),
              chan="wpool", writes=["wpool"])
        bpool_sb = sb("bpool_sb", [128, 8])
        pscale_sb = sb("pscale_sb", [128, 8])
        S.dma("sp", bpool_sb[:], b_pool, chan="bpool", writes=["bpool"])
        S.dma("sp", pscale_sb[:], pool_scale, chan="pscale", writes=["pscale"])
        nw_row = sb("nw_row", [1, 256])
        S.dma("sp", nw_row[:], gla_norm_w, chan="nw_row", writes=["nw_row"])
        nw_bc = sb("nw_bc", [128, 256])
        ps, pk = next_pg()
        S.op("pe", lambda e: e.matmul(ps[:, 0:256], lhsT=ones_row[0:1, :], rhs=nw_row[0:1, :], start=True, stop=True),
             reads=["nw_row", "ones_row"], writes=[pk])
        S.op("act", lambda e: e.activation(out=nw_bc[:], in_=ps[:, 0:256], func=AF.Copy), reads=[pk], writes=["nw_bc"])
        lnq_sb = sb("lnq_sb", [128, 1])
        S.op("dve", lambda e: e.memset(lnq_sb[:], float(np.log(128.0 ** -0.5))), writes=["lnq"])
        rms_eps = sb("rms_eps", [128, 1])
        S.op("dve", lambda e: e.memset(rms_eps[:], 1e-6), writes=["rms_eps"])

        if stop_after == "A1s":
            S.barrier()
            ph.close()
            return nc
        WB = [sb("wb%d" % i, [128, 16, 512], BF16) for i in range(3)]
        wb_i = [0]

        def load_w(src_ap):
            i = wb_i[0] % 3
            wb_i[0] += 1
            k = "wb%d" % i
            S.dma("pool", WB[i][:, :, :], src_ap.rearrange("(kc p) n -> p kc n", p=128), chan=k, writes=[k])
            return WB[i], k

        XT = [sb("xt%d" % i, [128, D]) for i in range(2)]
        xt_i = [0]
        xn = sb("xn", [128, D], BF16)
        stats = sb("stats", [128, 4, 6])
        mv = sb("mv", [128, 2])
        rstd = sb("rstd", [128, 1])
        hT = sb("hT", [128, 16, SEG], BF16)
        uT = sb("uT", [128, 8, 16 + SEG])
        sA = sb("sA", [128, 2, 16 + SEG])
        sB = sb("sB", [128, 2, 16 + SEG])
        pooled = sb("pooled", [128, 2, SEG], BF16)
        fixs = sb("fixs", [128, 2, 16])
        qtil = sb("qtil", [128, 4, SEG], BF16)
        ktil = sb("ktil", [128, 4, SEG], BF16)
        khatT = sb("khatT", [128, 4, SEG], BF16)
        khat_tm = sb("khat_tm", [128, 4, 4, 128], BF16)
        v_tm = sb("v_tm", [128, 4, 1024], BF16)
        sg_tm = sb("sg_tm", [128, 4, 1024], BF16)
        gkT = sb("gkT", [16, SEG])
        spl = sb("spl", [128, SEG])
        cs = sb("cs", [128, 4, SEG])
        eq = sb("eq", [128, SEG])
        enb = sb("enb", [128, SEG])
        ehat = sb("ehat", [128, SEG])
        eL = sb("eL", [128, 4, 8])
        state32 = sb("state32", [128, 4, 256])
        stateb = sb("stateb", [128, 4, 256], BF16)
        scm = sb("scm", [128, 2, 128], BF16)
        yB = sb("yB", [128, 1024], BF16)
        nwsg = sb("nwsg", [128, 256])
        ssq = sb("ssq", [128, 1])
        rr = sb("rr", [128, 1])
        osq = sb("osq", [128, 256])
        yT = sb("yT", [128, 16, SEG], BF16)
        S.op("pool", lambda e: e.memset(state32[:], 0.0), writes=[("state32", h) for h in range(4)])
        S.op("pool", lambda e: e.memset(stateb[:], 0.0), writes=[("stateb", h) for h in range(4)])
        S.op("pool", lambda e: e.memset(uT[:], 0.0), writes=[("uT", ch) for ch in range(8)])
        S.op("pool", lambda e: e.memset(sA[:], 0.0), writes=["sA"])
        S.op("pool", lambda e: e.memset(sB[:], 0.0), writes=["sB"])

        def layer_norm_stats(xt, xk):
            for j in range(4):
                S.op("dve", lambda e, j=j: e.bn_stats(out=stats[:, j, :], in_=xt[:, j * 512:(j + 1) * 512]),
                     reads=[xk], writes=[("stats", j)])
            S.op("dve", lambda e: e.bn_aggr(out=mv[:], in_=stats[:].rearrange("p a b -> p (a b)")),
                 reads=[("stats", j) for j in range(4)], writes=["mv"])
            S.op("act", lambda e: e.activation(out=rstd[:], in_=mv[:, 1:2], func=AF.Sqrt, bias=eps_sb[:, 0:1], scale=1.0),
                 reads=["mv", "eps"], writes=["rstd0"])
            S.op("dve", lambda e: e.reciprocal(out=rstd[:], in_=rstd[:]), reads=["rstd0"], writes=["rstd"])

        def mixer_segment(xsrc, seg, own, need_u):
            for tt in range(4):
                i = xt_i[0] % 2
                xt_i[0] += 1
                xk = "xt%d" % i
                r0 = seg * SEG + tt * 128
                S.dma("sp", XT[i][:], xsrc[r0:r0 + 128, :], chan=xk, writes=[xk])
                layer_norm_stats(XT[i], xk)
                S.op("dve", lambda e, i=i: e.tensor_scalar(out=xn[:], in0=XT[i][:], scalar1=mv[:, 0:1], scalar2=rstd[:, 0:1],
                                                           op0=ALU.subtract, op1=ALU.mult),
                     reads=[xk, "mv", "rstd"], writes=["xn"])
                for half in range(2):
                    PT = PTS[half]
                    ptk = "pt%d" % half
                    for j in range(8):
                        kc = half * 8 + j
                        S.op("pe", lambda e, kc=kc, j=j, PT=PT: e.transpose(PT[:, j * 128:(j + 1) * 128], xn[:, kc * 128:(kc + 1) * 128], identb[:]),
                             reads=["xn", "identb"], writes=[ptk])
                    for j in range(8):
                        kc = half * 8 + j
                        eng = "act" if j % 2 == 0 else "dve"
                        if eng == "act":
                            S.op("act", lambda e, kc=kc, j=j, tt=tt, PT=PT: e.activation(
                                out=hT[:, kc, tt * 128:(tt + 1) * 128], in_=PT[:, j * 128:(j + 1) * 128], func=AF.Identity,
                                bias=sh1_fm[:, kc:kc + 1], scale=sc1p_fm[:, kc:kc + 1]),
                                reads=[ptk, "sh1_fm", "sc1p_fm"], writes=[("hT", tt)])
                        else:
                            S.op("dve", lambda e, kc=kc, j=j, tt=tt, PT=PT: e.tensor_scalar(
                                out=hT[:, kc, tt * 128:(tt + 1) * 128], in0=PT[:, j * 128:(j + 1) * 128],
                                scalar1=sc1p_fm[:, kc:kc + 1], scalar2=sh1_fm[:, kc:kc + 1], op0=ALU.mult, op1=ALU.add),
                                reads=[ptk, "sh1_fm", "sc1p_fm"], writes=[("hT", tt)])
            hkeys = [("hT", tt) for tt in range(4)]
            if stop_after == "A1a":
                raise _Stop()

            def fm_group(wt, wk, m, evac):
                ps, pk = next_pg()
                for kc in range(16):
                    S.op("pe", lambda e, kc=kc: e.matmul(ps[:, :], lhsT=wt[:, kc, m * 128:(m + 1) * 128], rhs=hT[:, kc, :],
                                                         start=(kc == 0), stop=(kc == 15)),
                         reads=[wk] + hkeys, writes=[pk])
                evac(ps, pk)

            def tm_group(wt, wk, tt, evac):
                ps, pk = next_pg()
                for kc in range(16):
                    S.op("pe", lambda e, kc=kc: e.matmul(ps[:, :], lhsT=hT[:, kc, tt * 128:(tt + 1) * 128], rhs=wt[:, kc, :],
                                                         start=(kc == 0), stop=(kc == 15)),
                         reads=[wk, ("hT", tt)], writes=[pk])
                evac(ps, pk)

            ps, pk = next_pg()
            for kc in range(16):
                S.op("pe", lambda e, kc=kc: e.matmul(ps[0:16, :], lhsT=wgkin[:, kc, :], rhs=hT[:, kc, :],
                                                     start=(kc == 0), stop=(kc == 15)),
                     reads=["wgkin"] + hkeys, writes=[pk])
            S.op("act", lambda e: e.activation(out=gkT[:], in_=ps[0:16, :], func=AF.Copy), reads=[pk], writes=["gkT"])
            for h in range(4):
                ps, pk = next_pg()
                S.op("pe", lambda e, h=h: e.matmul(ps[:, :], lhsT=wgk_sb[:, h * 128:(h + 1) * 128], rhs=gkT[:, :],
                                                   start=True, stop=True), reads=["wgk", "gkT"], writes=[pk])
                S.op("act", lambda e, h=h: e.activation(out=spl[:], in_=ps[:, :], func=AF.Exp, bias=nbgk[:, h:h + 1], scale=-1.0),
                     reads=[pk, "nbgk"], writes=["spl"])
                S.op("act", lambda e: e.activation(out=spl[:], in_=spl[:], func=AF.Ln, bias=1.0, scale=1.0),
                     reads=["spl"], writes=["spl"])
                S.op("dve", lambda e, h=h: e.tensor_tensor_scan(out=cs[:, h, :], data0=rmask_sb[:], data1=spl[:], initial=0.0,
                                                                op0=ALU.mult, op1=ALU.add),
                     reads=["spl", "rmask"], writes=[("cs", h)])
            if stop_after == "A1b":
                raise _Stop()
            wt, wk = load_w(w_in[:, 1536:2048])
            for h in range(4):
                S.op("act", lambda e, h=h: e.activation(out=enb[:], in_=cs[:, h, :], func=AF.Exp, scale=1.0 / 16.0),
                     reads=[("cs", h)], writes=["enb"])
                S.op("act", lambda e, h=h: e.activation(
                    out=eL[:, h, :], in_=cs[:, h, :].rearrange("p (c t) -> p c t", t=64)[:, :, 63], func=AF.Exp, scale=-1.0 / 16.0),
                    reads=[("cs", h)], writes=[("eL", h)])
                S.op("dve", lambda e, h=h: e.tensor_tensor(
                    out=ehat[:].rearrange("p (c t) -> p c t", t=64), in0=enb[:].rearrange("p (c t) -> p c t", t=64),
                    in1=eL[:, h, :].unsqueeze(2).broadcast_to([128, 8, 64]), op=ALU.mult),
                    reads=["enb", ("eL", h)], writes=["ehat"])

                def evac_k(ps, pk, h=h):
                    if own:
                        S.op("dve", lambda e: e.tensor_tensor(out=ktil[:, h, :], in0=ps[:, :], in1=enb[:], op=ALU.mult),
                             reads=[pk, "enb"], writes=[("ktil", h)])
                    S.op("dve", lambda e: e.tensor_tensor(out=khatT[:, h, :], in0=ps[:, :], in1=ehat[:], op=ALU.mult),
                         reads=[pk, "ehat"], writes=[("khatT", h)])
                fm_group(wt, wk, h, evac_k)
            if stop_after == "A1c":
                raise _Stop()
            for tt in range(4):
                PT = PTS[tt % 2]
                ptk = "pt%d" % (tt % 2)
                for h in range(4):
                    S.op("pe", lambda e, tt=tt, h=h, PT=PT: e.transpose(PT[:, h * 128:(h + 1) * 128], khatT[:, h, tt * 128:(tt + 1) * 128], identb[:]),
                         reads=[("khatT", h), "identb"], writes=[ptk])
                S.op("act", lambda e, tt=tt, PT=PT: e.activation(out=khat_tm[:, tt, :, :], in_=PT[:, 0:512].rearrange("p (h d) -> p h d", h=4),
                                                          func=AF.Copy),
                     reads=[ptk], writes=[("khat_tm", tt)])
            for half in range(2):
                wt, wk = load_w(w_in[:, 2048 + half * 512: 2048 + (half + 1) * 512])
                for tt in range(4):
                    def evac_v(ps, pk, tt=tt, half=half):
                        S.op("act", lambda e: e.activation(out=v_tm[:, tt, half * 512:(half + 1) * 512], in_=ps[:, :], func=AF.Copy),
                             reads=[pk], writes=[("v_tm", tt)])
                    tm_group(wt, wk, tt, evac_v)
            if own:
                wt, wk = load_w(w_in[:, 1024:1536])
                for h in range(4):
                    S.op("act", lambda e, h=h: e.activation(out=eq[:], in_=cs[:, h, :], func=AF.Exp, scale=-1.0 / 16.0, bias=lnq_sb[:, 0:1]),
                         reads=[("cs", h), "lnq"], writes=["eq"])

                    def evac_q(ps, pk, h=h):
                        S.op("dve", lambda e: e.tensor_tensor(out=qtil[:, h, :], in0=ps[:, :], in1=eq[:], op=ALU.mult),
                             reads=[pk, "eq"], writes=[("qtil", h)])
                    fm_group(wt, wk, h, evac_q)
                for half in range(2):
                    wt, wk = load_w(w_in[:, 3088 + half * 512: 3088 + (half + 1) * 512])
                    for tt in range(4):
                        def evac_g(ps, pk, tt=tt, half=half):
                            S.op("act", lambda e: e.activation(out=sg_tm[:, tt, half * 512:(half + 1) * 512], in_=ps[:, :], func=AF.Silu),
                                 reads=[pk], writes=[("sg_tm", tt)])
                        tm_group(wt, wk, tt, evac_g)
            if need_u:
                for half in range(2):
                    wt, wk = load_w(w_in[:, half * 512:(half + 1) * 512])
                    for m in range(4):
                        def evac_u(ps, pk, ch=half * 4 + m):
                            S.op("act", lambda e: e.activation(out=uT[:, ch, 16:16 + SEG], in_=ps[:, :], func=AF.Copy),
                                 reads=[pk], writes=[("uT", ch)])
                        fm_group(wt, wk, m, evac_u)
            if own and seg == 0:
                S.op("dve", lambda e: e.tensor_scalar(out=state32[:].rearrange("p a b -> p (a b)"), in0=state32[:].rearrange("p a b -> p (a b)"),
                                                      scalar1=flag_sb[:, 0:1], scalar2=None, op0=ALU.mult),
                     reads=[("state32", h) for h in range(4)] + ["flag"], writes=[("state32", h) for h in range(4)])
                S.op("dve", lambda e: e.tensor_scalar(out=stateb[:].rearrange("p a b -> p (a b)"), in0=state32[:].rearrange("p a b -> p (a b)"),
                                                      scalar1=1.0, scalar2=None, op0=ALU.mult),
                     reads=[("state32", h) for h in range(4)], writes=[("stateb", h) for h in range(4)])
                for ch in range(8):
                    S.op("pool", lambda e, ch=ch: e.tensor_scalar(out=uT[:, ch, 0:16], in0=uT[:, ch, 0:16], scalar1=flag_sb[:, 0:1], scalar2=None,
                                                                  op0=ALU.mult),
                         reads=[("uT", ch), "flag"], writes=[("uT", ch)])
            if own:
                for g in range(4):
                    w = 2 ** (g + 1)
                    chs = [("uT", 2 * g), ("uT", 2 * g + 1)]
                    src = uT[:, 2 * g:2 * g + 2, :]
                    srck = chs
                    bufs = [(sA, "sA"), (sB, "sB")]
                    bi = 0
                    step = 1
                    L = 16 + SEG
                    while step < w:
                        dst, dk = bufs[bi]
                        S.op("pool", lambda e, src=src, dst=dst, step=step: e.tensor_tensor(
                            out=dst[:, :, step:L], in0=src[:, :, step:L], in1=src[:, :, 0:L - step], op=ALU.add),
                            reads=srck, writes=[dk])
                        src, srck = dst[:, :, :], [dk]
                        bi ^= 1
                        step *= 2
                    S.op("dve", lambda e, src=src, g=g, w=w: e.scalar_tensor_tensor(
                        out=pooled[:, :, :], in0=src[:, :, 16:16 + SEG], scalar=1.0 / w, in1=uT[:, 2 * g:2 * g + 2, 16:16 + SEG],
                        op0=ALU.mult, op1=ALU.subtract), reads=srck + chs, writes=["pooled"])
                    if seg == 0:
                        for cc in range(2):
                            S.op("dve", lambda e, src=src, g=g, cc=cc: e.tensor_tensor(
                                out=fixs[:, cc, :], in0=src[:, cc, 16:32], in1=invc_sb[:, g, :], op=ALU.mult),
                                reads=srck + ["invc"], writes=["fixs"])
                            S.op("dve", lambda e, g=g, cc=cc: e.tensor_tensor(
                                out=pooled[:, cc, 0:16], in0=fixs[:, cc, :], in1=uT[:, 2 * g + cc, 16:32], op=ALU.subtract),
                                reads=["fixs"] + chs, writes=["pooled"])
                    for dh in range(2):
                        ps, pk = next_pg()
                        for cc in range(2):
                            S.op("pe", lambda e, g=g, cc=cc, dh=dh: e.matmul(ps[:, :], lhsT=wpool_sb[:, g, cc, dh * 128:(dh + 1) * 128],
                                                                           rhs=pooled[:, cc, :], start=(cc == 0), stop=(cc == 1)),
                                 reads=["wpool", "pooled"], writes=[pk])
                        ch = 2 * g + dh
                        S.op("dve", lambda e, ch=ch, ps=ps: e.tensor_scalar(out=yT[:, ch, :], in0=ps[:, :], scalar1=bpool_sb[:, ch:ch + 1],
                                                                            scalar2=pscale_sb[:, ch:ch + 1], op0=ALU.add, op1=ALU.mult),
                             reads=[pk, "bpool", "pscale"], writes=[("yT", ch)])
            if need_u:
                for ch in range(8):
                    S.op("pool", lambda e, ch=ch: e.tensor_copy(out=uT[:, ch, 0:16], in_=uT[:, ch, SEG:SEG + 16]),
                         reads=[("uT", ch)], writes=[("uT", ch)])
            if stop_after == "A1d":
                raise _Stop()
            for tt in range(4):
                if own:
                    for h in range(4):
                        sl = slice(tt * 128, (tt + 1) * 128)
                        S.op("pe", lambda e, h=h, sl=sl: e.matmul(PSC[:, (h % 4) * 128:(h % 4 + 1) * 128], lhsT=ktil[:, h, sl], rhs=qtil[:, h, sl],
                                                                  start=True, stop=True),
                             reads=[("ktil", h), ("qtil", h)], writes=["psc"])
                for h in range(4):
                    if own:
                        S.op("dve", lambda e, h=h: e.tensor_tensor(out=scm[:, h % 2, :], in0=PSC[:, h * 128:(h + 1) * 128], in1=triu_sb[:], op=ALU.mult),
                             reads=["psc", "triu"], writes=[("scm", h % 2)])
                        po = PO[h // 2]
                        pok = ("po", h // 2)
                    for c in range(2):
                        rows = slice(c * 64, (c + 1) * 64)
                        cg = tt * 2 + c
                        if own:
                            S.op("pe", lambda e, h=h, c=c, rows=rows, po=po: e.matmul(
                                po[rows, (h % 2) * 256:(h % 2 + 1) * 256], lhsT=scm[rows, h % 2, c * 64:(c + 1) * 64],
                                rhs=v_tm[rows, tt, h * 256:(h + 1) * 256], start=True, stop=False),
                                reads=[("scm", h % 2), ("v_tm", tt)], writes=[pok])
                            S.op("pe", lambda e, h=h, c=c, rows=rows, po=po: e.matmul(
                                po[rows, (h % 2) * 256:(h % 2 + 1) * 256], lhsT=qtil[:, h, tt * 128 + c * 64: tt * 128 + (c + 1) * 64],
                                rhs=stateb[:, h, :], start=False, stop=True),
                                reads=[("qtil", h), ("stateb", h)], writes=[pok])
                        slot = (h * 2 + c) % 2
                        S.op("pe", lambda e, h=h, rows=rows, slot=slot: e.matmul(
                            PS_[:, slot * 256:(slot + 1) * 256], lhsT=khat_tm[rows, tt, h, :], rhs=v_tm[rows, tt, h * 256:(h + 1) * 256],
                            start=True, stop=True),
                            reads=[("khat_tm", tt), ("v_tm", tt)], writes=["pss"])
                        S.op("dve", lambda e, h=h, cg=cg, slot=slot: e.scalar_tensor_tensor(
                            out=state32[:, h, :], in0=state32[:, h, :], scalar=eL[:, h, cg:cg + 1], in1=PS_[:, slot * 256:(slot + 1) * 256],
                            op0=ALU.mult, op1=ALU.add),
                            reads=["pss", ("eL", h), ("state32", h)], writes=[("state32", h)])
                        S.op("act", lambda e, h=h: e.activation(out=stateb[:, h, :], in_=state32[:, h, :], func=AF.Copy),
                             reads=[("state32", h)], writes=[("stateb", h)])
                    if own:
                        S.op("act", lambda e, h=h, po=po: e.activation(out=osq[:], in_=po[:, (h % 2) * 256:(h % 2 + 1) * 256], func=AF.Square,
                                                                       accum_out=ssq[:, 0:1]),
                             reads=[pok], writes=["osq", "ssq"])
                        S.op("act", lambda e: e.activation(out=rr[:], in_=ssq[:], func=AF.Sqrt, bias=rms_eps[:, 0:1], scale=1.0 / 256.0),
                             reads=["ssq", "rms_eps"], writes=["rr0"])
                        S.op("dve", lambda e: e.reciprocal(out=rr[:], in_=rr[:]), reads=["rr0"], writes=["rr"])
                        S.op("dve", lambda e, h=h, tt=tt: e.tensor_tensor(out=nwsg[:], in0=nw_bc[:], in1=sg_tm[:, tt, h * 256:(h + 1) * 256], op=ALU.mult),
                             reads=["nw_bc", ("sg_tm", tt)], writes=["nwsg"])
                        S.op("dve", lambda e, h=h, po=po: e.scalar_tensor_tensor(
                            out=yB[:, h * 256:(h + 1) * 256], in0=po[:, (h % 2) * 256:(h % 2 + 1) * 256], scalar=rr[:, 0:1], in1=nwsg[:],
                            op0=ALU.mult, op1=ALU.mult), reads=[pok, "rr", "nwsg"], writes=[("yB", h)])
                if own:
                    PT = PTS[tt % 2]
                    ptk = "pt%d" % (tt % 2)
                    for j in range(8):
                        S.op("pe", lambda e, j=j, PT=PT: e.transpose(PT[:, j * 128:(j + 1) * 128], yB[:, j * 128:(j + 1) * 128], identb[:]),
                             reads=[("yB", j // 2), "identb"], writes=[ptk])
                    S.op("act", lambda e, tt=tt, PT=PT: e.activation(out=yT[:, 8:16, tt * 128:(tt + 1) * 128],
                                                              in_=PT[:, :].rearrange("p (j t) -> p j t", j=8), func=AF.Copy),
                         reads=[ptk], writes=[("yTb", tt)])
            if own:
                ykeys = [("yT", ch) for ch in range(8)] + [("yTb", tt) for tt in range(4)]
                S.dma("sp", ybufT[:, :, seg * SEG:(seg + 1) * SEG].rearrange("c p t -> p c t"), yT[:], chan="yT",
                      reads=ykeys, writes=[("ybufT", seg)])

        try:
            for seg in range(NSEG):
                mixer_segment(x_pre, seg, own=False, need_u=(seg == NSEG - 1))
            for seg in range(NSEG):
                mixer_segment(x_own, seg, own=True, need_u=True)
        except _Stop:
            S.barrier()
            ph.close()
            print("sems", len(S.dsem), "inst", S.n_inst, "waits", S.n_wait)
            return nc

        if debug:
            for seg in range(NSEG):
                S.dma("sp", yT[:], ybufT[:, :, seg * SEG:(seg + 1) * SEG].rearrange("c p t -> p c t"), chan="yT",
                      reads=[("ybufT", seg)], writes=["yT_dbg"] + [("yT", ch) for ch in range(8)] + [("yTb", tt) for tt in range(4)])
                S.dma("sp", dbg["yT"][:, :, seg * SEG:(seg + 1) * SEG].rearrange("c p t -> p c t"), yT[:], chan="yT",
                      reads=["yT_dbg"], writes=["dbg_yT"])
        S.barrier()
        ph.close()
        if stop_after == "A1":
            print("sems", len(S.dsem), "inst", S.n_inst, "waits", S.n_wait)
            return nc
        ph = ExitStack()
        cur[0] = ph
        PB = [PG[0], PG[1], PSC, PO[0], PO[1], PS_]
        PBK = ["pg0", "pg1", "psc", ("po", 0), ("po", 1), "pss"]
        pb_i = [0]

        def next_pb():
            i = pb_i[0] % len(PB)
            pb_i[0] += 1
            return PB[i], PBK[i]

        wout = sb("wout", [128, 16, D], BF16)
        for j in range(4):
            S.dma("pool", wout[:, :, j * 512:(j + 1) * 512],
                  w_out[:, j * 512:(j + 1) * 512].rearrange("(kc p) n -> p kc n", p=128),
                  chan="wout", writes=["wout"])

        def bc_tile(name, src_row):
            t = sb(name, [128, D])
            S.dma("sp", t[:], src_row.partition_broadcast(128), chan=name, reads=["moddram"], writes=[name])
            return t
        g1_bc = bc_tile("g1_bc", moddram[0, 2 * D:3 * D])
        sh2_bc = bc_tile("sh2_bc", moddram[0, 3 * D:4 * D])
        sc2p_bc = bc_tile("sc2p_bc", moddram[0, 4 * D:5 * D])
        S.op("pool", lambda e: e.tensor_scalar(out=sc2p_bc[:], in0=sc2p_bc[:], scalar1=1.0, scalar2=None, op0=ALU.add),
             reads=["sc2p_bc"], writes=["sc2p_bc"])
        l1g_bc = bc_tile("l1g_bc", ln1_g[0, :])
        l1b_bc = bc_tile("l1b_bc", ln1_b[0, :])
        wr_sb = sb("wr_sb", [128, 16, NE])
        S.dma("sp", wr_sb[:], w_router.rearrange("(kc p) n -> p kc n", p=128), chan="wr_sb", writes=["wr_sb"])
        br_bc = sb("br_bc", [128, NE])
        S.dma("sp", br_bc[:], b_router[0, :].partition_broadcast(128), chan="br_bc", writes=["br_bc"])
        ltri_sb = sb("ltri_sb", [128, 128])
        S.dma("sp", ltri_sb[:], ltri, chan="ltri", writes=["ltri"])
        ones_sq = sb("ones_sq", [128, 128])
        S.op("pool", lambda e: e.memset(ones_sq[:], 1.0), writes=["ones_sq"])
        iotae_sb = sb("iotae_sb", [128, NE])
        S.dma("sp", iotae_sb[:], iota_e, chan="iotae", writes=["iotae"])
        iota32 = sb("iota32", [128, NE])
        S.dma("sp", iota32[:], iota_n, chan="iota32", writes=["iota32"])
        tokid_sb = sb("tokid_sb", [128, 16], I32)
        S.dma("sp", tokid_sb[:], tokid, chan="tokid", writes=["tokid"])
        carry = sb("carry", [128, NE])
        S.op("pool", lambda e: e.memset(carry[:], 0.0), writes=["carry"])
        zt = sb("zt", [128, D])
        S.op("pool", lambda e: e.memset(zt[:], 0.0), writes=["zt"])
        for r in range(17):
            S.dma("sp", yacc[r * 128:(r + 1) * 128, :], zt[:], chan="zt", reads=["zt"], writes=["yacc"])
        ztb = sb("ztb", [128, D], BF16)
        S.op("pool", lambda e: e.memset(ztb[:], 0.0), writes=["ztb"])
        S.dma("sp", h2buf[T:T + 128, :], ztb[:], chan="ztb", reads=["ztb"], writes=["h2dummy"])
        li_sb = sb("li_sb", [128, 1024], I32)
        S.dma("sp", li_sb[:], list_init.rearrange("(p r) two -> p (r two)", p=128), chan="li_sb", writes=["li_sb"])
        S.dma("sp", lists.rearrange("(p r) two -> p (r two)", p=128), li_sb[:], chan="li_sb", reads=["li_sb"], writes=["lists"])

        YTT = [sb("ytt%d" % i, [128, 16, 128], BF16) for i in range(2)]
        XA = [sb("xa%d" % i, [128, D]) for i in range(2)]
        x1pre = sb("x1pre", [128, D])
        x1t = sb("x1t", [128, D])
        h2t = sb("h2t", [128, D])
        h2b = sb("h2b", [128, D], BF16)
        h2T = sb("h2T", [128, 16, 128])
        stats2 = sb("stats2", [128, 4, 6])
        mv2 = sb("mv2", [128, 2])
        rstd2 = sb("rstd2", [128, 1])
        lg = sb("lg", [128, NE])
        max8 = sb("max8", [128, 8])
        idx8 = sb("idx8", [128, 8], U32)
        idxf = sb("idxf", [128, 8])
        negm = sb("negm", [128, 1])
        ew = sb("ew", [128, 4])
        den = sb("den", [128, 1])
        w4 = sb("w4", [128, 4])
        maskt = sb("maskt", [128, NE])
        slotf = sb("slotf", [128, NE])
        oh = sb("oh", [128, NE])
        sl4 = sb("sl4", [128, 4])
        sl4i = sb("sl4i", [128, 4], I32)
        pairs = sb("pairs", [128, 16, 4, 2], I32)

        def ln_stats(src, skey, st_t, mv_t, rs_t, tag):
            for j in range(4):
                S.op("dve", lambda e, j=j: e.bn_stats(out=st_t[:, j, :], in_=src[:, j * 512:(j + 1) * 512]),
                     reads=[skey], writes=[(tag + "st", j)])
            S.op("dve", lambda e: e.bn_aggr(out=mv_t[:], in_=st_t[:].rearrange("p a b -> p (a b)")),
                 reads=[(tag + "st", j) for j in range(4)], writes=[tag + "mv"])
            S.op("act", lambda e: e.activation(out=rs_t[:], in_=mv_t[:, 1:2], func=AF.Sqrt, bias=eps_sb[:, 0:1], scale=1.0),
                 reads=[tag + "mv", "eps"], writes=[tag + "rs0"])
            S.op("dve", lambda e: e.reciprocal(out=rs_t[:], in_=rs_t[:]), reads=[tag + "rs0"], writes=[tag + "rs"])

        for ti in range(16):
            b2 = ti % 2
            yk, xk = "ytt%d" % b2, "xa%d" % b2
            S.dma("sp", YTT[b2][:], ybufT[:, :, ti * 128:(ti + 1) * 128].rearrange("c p t -> p c t"), chan=yk,
                  reads=[("ybufT", ti // 4)], writes=[yk])
            S.dma("sp", XA[b2][:], x_own[ti * 128:(ti + 1) * 128, :], chan=xk, writes=[xk])
            for j in range(4):
                ps, pk = next_pb()
                for kc in range(16):
                    S.op("pe", lambda e, kc=kc, j=j, ps=ps, b2=b2: e.matmul(ps[:, :], lhsT=YTT[b2][:, kc, :], rhs=wout[:, kc, j * 512:(j + 1) * 512],
                                                                          start=(kc == 0), stop=(kc == 15)),
                         reads=[yk, "wout"], writes=[pk])
                S.op("dve", lambda e, j=j, ps=ps: e.tensor_tensor(out=x1pre[:, j * 512:(j + 1) * 512], in0=ps[:, :], in1=g1_bc[:, j * 512:(j + 1) * 512], op=ALU.mult),
                     reads=[pk, "g1_bc"], writes=[("x1pre", j)])
                S.op("dve", lambda e, j=j, b2=b2: e.scalar_tensor_tensor(out=x1pre[:, j * 512:(j + 1) * 512], in0=XA[b2][:, j * 512:(j + 1) * 512], scalar=ALPHA,
                                                                       in1=x1pre[:, j * 512:(j + 1) * 512], op0=ALU.mult, op1=ALU.add),
                     reads=[xk, ("x1pre", j)], writes=[("x1pre", j)])
            xpk = [("x1pre", j) for j in range(4)]
            for j in range(4):
                S.op("dve", lambda e, j=j: e.bn_stats(out=stats2[:, j, :], in_=x1pre[:, j * 512:(j + 1) * 512]),
                     reads=[("x1pre", j)], writes=[("ast", j)])
            S.op("dve", lambda e: e.bn_aggr(out=mv2[:], in_=stats2[:].rearrange("p a b -> p (a b)")),
                 reads=[("ast", j) for j in range(4)], writes=["amv"])
            S.op("act", lambda e: e.activation(out=rstd2[:], in_=mv2[:, 1:2], func=AF.Sqrt, bias=eps_sb[:, 0:1], scale=1.0),
                 reads=["amv", "eps"], writes=["ars0"])
            S.op("dve", lambda e: e.reciprocal(out=rstd2[:], in_=rstd2[:]), reads=["ars0"], writes=["ars"])
            S.op("dve", lambda e: e.tensor_scalar(out=x1t[:], in0=x1pre[:], scalar1=mv2[:, 0:1], scalar2=rstd2[:, 0:1], op0=ALU.subtract, op1=ALU.mult),
                 reads=xpk + ["amv", "ars"], writes=["x1t"])
            S.op("pool", lambda e: e.tensor_tensor(out=x1t[:], in0=x1t[:], in1=l1g_bc[:], op=ALU.mult), reads=["x1t", "l1g_bc"], writes=["x1t"])
            S.op("pool", lambda e: e.tensor_tensor(out=x1t[:], in0=x1t[:], in1=l1b_bc[:], op=ALU.add), reads=["x1t", "l1b_bc"], writes=["x1t"])
            S.dma("sp", x1buf[ti * 128:(ti + 1) * 128, :], x1t[:], chan="x1t", reads=["x1t"], writes=[("x1buf", ti)])
            ln_stats(x1t, "x1t", stats2, mv2, rstd2, "b")
            S.op("dve", lambda e: e.tensor_scalar(out=h2t[:], in0=x1t[:], scalar1=mv2[:, 0:1], scalar2=rstd2[:, 0:1], op0=ALU.subtract, op1=ALU.mult),
                 reads=["x1t", "bmv", "brs"], writes=["h2t"])
            S.op("pool", lambda e: e.tensor_tensor(out=h2t[:], in0=h2t[:], in1=sc2p_bc[:], op=ALU.mult), reads=["h2t", "sc2p_bc"], writes=["h2t"])
            S.op("pool", lambda e: e.tensor_tensor(out=h2t[:], in0=h2t[:], in1=sh2_bc[:], op=ALU.add), reads=["h2t", "sh2_bc"], writes=["h2t"])
            S.op("act", lambda e: e.activation(out=h2b[:], in_=h2t[:], func=AF.Copy), reads=["h2t"], writes=["h2b"])
            S.dma("sp", h2buf[ti * 128:(ti + 1) * 128, :], h2b[:], chan="h2b", reads=["h2b"], writes=[("h2buf", ti)])
            for q4 in range(4):
                ps, pk = next_pb()
                for j in range(4):
                    kc = q4 * 4 + j
                    S.op("pe", lambda e, kc=kc, j=j, ps=ps: e.transpose(ps[:, j * 128:(j + 1) * 128], h2t[:, kc * 128:(kc + 1) * 128], ident32[:]),
                         reads=["h2t", "ident32"], writes=[pk])
                S.op("act", lambda e, q4=q4, ps=ps: e.activation(out=h2T[:, q4 * 4:(q4 + 1) * 4, :], in_=ps[:, :].rearrange("p (j t) -> p j t", j=4), func=AF.Copy),
                     reads=[pk], writes=[("h2T", q4)])
            ps, pk = next_pb()
            for kc in range(16):
                S.op("pe", lambda e, kc=kc, ps=ps: e.matmul(ps[:, 0:NE], lhsT=h2T[:, kc, :], rhs=wr_sb[:, kc, :], start=(kc == 0), stop=(kc == 15)),
                     reads=[("h2T", kc // 4), "wr_sb"], writes=[pk])
            S.op("dve", lambda e, ps=ps: e.tensor_tensor(out=lg[:], in0=ps[:, 0:NE], in1=br_bc[:], op=ALU.add), reads=[pk, "br_bc"], writes=["lg"])
            if debug:
                S.dma("sp", dbg["lg"][ti * 128:(ti + 1) * 128, :], lg[:], chan="lg", reads=["lg"], writes=["dbg_lg"])
            S.op("dve", lambda e: e.max(out=max8[:], in_=lg[:]), reads=["lg"], writes=["max8"])
            S.op("dve", lambda e: e.max_index(out=idx8[:], in_max=max8[:], in_values=lg[:]), reads=["lg", "max8"], writes=["idx8"])
            S.op("dve", lambda e: e.tensor_copy(out=idxf[:], in_=idx8[:]), reads=["idx8"], writes=["idxf"])
            S.op("dve", lambda e: e.tensor_scalar(out=negm[:], in0=max8[:, 0:1], scalar1=-1.0, scalar2=None, op0=ALU.mult), reads=["max8"], writes=["negm"])
            S.op("act", lambda e: e.activation(out=ew[:], in_=max8[:, 0:4], func=AF.Exp, bias=negm[:, 0:1], scale=1.0, accum_out=den[:, 0:1]),
                 reads=["max8", "negm"], writes=["ew", "den"])
            S.op("dve", lambda e: e.reciprocal(out=den[:], in_=den[:]), reads=["den"], writes=["den"])
            S.op("dve", lambda e: e.tensor_scalar(out=w4[:], in0=ew[:], scalar1=den[:, 0:1], scalar2=None, op0=ALU.mult), reads=["ew", "den"], writes=["w4"])
            S.op("dve", lambda e: e.tensor_scalar(out=maskt[:], in0=lg[:], scalar1=max8[:, 3:4], scalar2=None, op0=ALU.is_ge), reads=["lg", "max8"], writes=["maskt"])
            ps, pk = next_pb()
            S.op("pe", lambda e, ps=ps: e.matmul(ps[:, 0:NE], lhsT=ltri_sb[:], rhs=maskt[:], start=True, stop=True), reads=["ltri", "maskt"], writes=[pk])
            S.op("dve", lambda e, ps=ps: e.tensor_tensor(out=slotf[:], in0=ps[:, 0:NE], in1=carry[:], op=ALU.add), reads=[pk, "carry"], writes=["slotf"])
            S.op("dve", lambda e: e.tensor_tensor(out=slotf[:], in0=slotf[:], in1=iotae_sb[:], op=ALU.add), reads=["slotf", "iotae"], writes=["slotf"])
            ps2, pk2 = next_pb()
            S.op("pe", lambda e, ps2=ps2: e.matmul(ps2[:, 0:NE], lhsT=ones_sq[:], rhs=maskt[:], start=True, stop=True), reads=["ones_sq", "maskt"], writes=[pk2])
            S.op("dve", lambda e, ps2=ps2: e.tensor_tensor(out=carry[:], in0=ps2[:, 0:NE], in1=carry[:], op=ALU.add), reads=[pk2, "carry"], writes=["carry"])
            for j in range(4):
                S.op("dve", lambda e, j=j: e.tensor_scalar(out=oh[:], in0=iota32[:], scalar1=idxf[:, j:j + 1], scalar2=None, op0=ALU.is_equal),
                     reads=["iota32", "idxf"], writes=["oh"])
                S.op("dve", lambda e: e.tensor_tensor(out=oh[:], in0=oh[:], in1=slotf[:], op=ALU.mult), reads=["oh", "slotf"], writes=["oh"])
                S.op("dve", lambda e, j=j: e.reduce_sum(out=sl4[:, j:j + 1], in_=oh[:], axis=AX.X), reads=["oh"], writes=["sl4"])
            S.op("dve", lambda e: e.tensor_copy(out=sl4i[:], in_=sl4[:]), reads=["sl4"], writes=["sl4i"])
            S.op("dve", lambda e, ti=ti: e.tensor_copy(out=pairs[:, ti, :, 0], in_=tokid_sb[:, ti:ti + 1].broadcast_to([128, 4])), reads=["tokid"], writes=[("pairs", ti)])
            S.op("dve", lambda e, ti=ti: e.tensor_copy(out=pairs[:, ti, :, 1].bitcast(F32), in_=w4[:]), reads=["w4", ("pairs", ti)], writes=[("pairs", ti)])
            for j in range(4):
                S.dma("pool", lists, pairs[:, ti, j, :], chan=("pairs", ti), reads=[("pairs", ti), "sl4i"], writes=["lists"],
                      indirect=dict(out_offset=bass.IndirectOffsetOnAxis(ap=sl4i[:, j:j + 1], axis=0), in_offset=None))
        flg_f = sb("flg_f", [1, 4 * NE])
        for g_ in range(4):
            S.op("dve", lambda e, g_=g_: e.tensor_scalar(out=flg_f[0:1, g_ * NE:(g_ + 1) * NE], in0=carry[0:1, :], scalar1=float(g_ * GRP),
                                                         scalar2=None, op0=ALU.is_gt), reads=["carry"], writes=["flg_f"])
        S.op("dve", lambda e: e.tensor_copy(out=flg_i[:], in_=flg_f[:]), reads=["flg_f"], writes=["flg_i"])
        S.barrier()
        ph.close()
        if stop_after == "A2":
            print("sems", len(S.dsem), "inst", S.n_inst, "waits", S.n_wait)
            return nc

        ph = ExitStack()
        cur[0] = ph
        WB = [sb("wbm%d" % i, [128, 16, 512], BF16) for i in range(5)]
        wb_i[0] = 0
        NWB = 5

        def load_w2(src_ap):
            i = wb_i[0] % NWB
            wb_i[0] += 1
            k = "wbm%d" % i
            S.dma("pool", WB[i][:, :, :], src_ap.rearrange("(kc p) n -> p kc n", p=128), chan=k, writes=[k])
            return WB[i], k

        lst = sb("lst", [128, 4, 2], I32)
        dum = sb("dum", [1, 64])
        dum_idx = {}
        xg = sb("xg", [128, 4, D], BF16)
        xT = sb("xT", [128, 16, GRP], BF16)
        actT = sb("actT", [128, 16, GRP], BF16)
        Yt = sb("Yt", [128, 4, D])
        bd_bc = sb("bd_bc", [128, D])
        bg_sb = sb("bg_sb", [128, 16])
        bu_sb = sb("bu_sb", [128, 16])
        gt = [sb("gt%d" % i, [128, GRP]) for i in range(2)]
        sgm = [sb("sgm%d" % i, [128, GRP]) for i in range(2)]
        ut = [sb("ut%d" % i, [128, GRP]) for i in range(2)]
        PGA = [(PG[0], "pg0"), (PG[1], "pg1")]
        PUP = [(PO[0], ("po", 0)), (PO[1], ("po", 1))]
        PDN = [(PSC, "psc"), (PS_, "pss")]
        it = [0]
        flag_regs = nc.alloc_registers("flg", engines=mybir.ALL_ENGINES)

        def pass_body(e_, g_):
            base = e_ * CAP + g_ * GRP
            S.dma("sp", lst[:], lists[base:base + GRP, :].rearrange("(b p) two -> p b two", p=128), chan="lst",
                  reads=["lists"], writes=["lst"])
            for blk in range(4):
                S.dma("pool", xg[:, blk, :], h2buf, chan="xg", reads=["lst", "h2dummy"] + [("h2buf", ti) for ti in range(16)],
                      writes=[("xg", blk)],
                      indirect=dict(out_offset=None, in_offset=bass.IndirectOffsetOnAxis(ap=lst[:, blk, 0:1], axis=0)))
            for blk in range(4):
                S.lastw[("xg", blk)] = ("dma", "xg", S.dcnt["xg"])
            for blk in range(4):
                for half in range(2):
                    PT = PTS[half]
                    ptk = "pt%d" % half
                    for j in range(8):
                        kc = half * 8 + j
                        S.op("pe", lambda e, kc=kc, j=j, PT=PT, blk=blk: e.transpose(PT[:, j * 128:(j + 1) * 128], xg[:, blk, kc * 128:(kc + 1) * 128], identb[:]),
                             reads=[("xg", blk), "identb"], writes=[ptk])
                    eng = "act" if half == 0 else "dve"
                    if eng == "act":
                        S.op("act", lambda e, PT=PT, blk=blk, half=half: e.activation(
                            out=xT[:, half * 8:(half + 1) * 8, blk * 128:(blk + 1) * 128], in_=PT[:, :].rearrange("p (j t) -> p j t", j=8), func=AF.Copy),
                            reads=[ptk], writes=[("xT", blk)])
                    else:
                        S.op("dve", lambda e, PT=PT, blk=blk, half=half: e.tensor_copy(
                            out=xT[:, half * 8:(half + 1) * 8, blk * 128:(blk + 1) * 128], in_=PT[:, :].rearrange("p (j t) -> p j t", j=8)),
                            reads=[ptk], writes=[("xT", blk)])
            xkeys = [("xT", blk) for blk in range(4)]
            for fq in range(4):
                wg, wgk = load_w2(w_gate[e_, :, fq * 512:(fq + 1) * 512])
                wu, wuk = load_w2(w_up[e_, :, fq * 512:(fq + 1) * 512])
                for m in range(4):
                    fc = fq * 4 + m
                    i2 = it[0] % 2
                    it[0] += 1
                    pg_, pgk = PGA[i2]
                    pu_, puk = PUP[i2]
                    for kc in range(16):
                        S.op("pe", lambda e, kc=kc, m=m, wg=wg, pg_=pg_: e.matmul(pg_[:, :], lhsT=wg[:, kc, m * 128:(m + 1) * 128], rhs=xT[:, kc, :],
                                                                                start=(kc == 0), stop=(kc == 15)), reads=[wgk] + xkeys, writes=[pgk])
                    for kc in range(16):
                        S.op("pe", lambda e, kc=kc, m=m, wu=wu, pu_=pu_: e.matmul(pu_[:, :], lhsT=wu[:, kc, m * 128:(m + 1) * 128], rhs=xT[:, kc, :],
                                                                                start=(kc == 0), stop=(kc == 15)), reads=[wuk] + xkeys, writes=[puk])
                    gk_, sk_, uk_ = "gt%d" % i2, "sgm%d" % i2, "ut%d" % i2
                    S.op("dve", lambda e, fc=fc, i2=i2, pg_=pg_: e.tensor_scalar(out=gt[i2][:], in0=pg_[:, :], scalar1=bg_sb[:, fc:fc + 1], scalar2=7.0,
                                                                               op0=ALU.add, op1=ALU.min), reads=[pgk, "bg_sb"], writes=[gk_])
                    S.op("act", lambda e, i2=i2: e.activation(out=sgm[i2][:], in_=gt[i2][:], func=AF.Sigmoid, scale=1.702), reads=[gk_], writes=[sk_])
                    S.op("dve", lambda e, fc=fc, i2=i2, pu_=pu_: e.tensor_scalar(out=ut[i2][:], in0=pu_[:, :], scalar1=bu_sb[:, fc:fc + 1], scalar2=7.0,
                                                                               op0=ALU.add, op1=ALU.min), reads=[puk, "bu_sb"], writes=[uk_])
                    S.op("dve", lambda e, i2=i2: e.tensor_scalar(out=ut[i2][:], in0=ut[i2][:], scalar1=-7.0, scalar2=1.0, op0=ALU.max, op1=ALU.add),
                         reads=[uk_], writes=[uk_])
                    S.op("dve", lambda e, i2=i2: e.tensor_tensor(out=gt[i2][:], in0=gt[i2][:], in1=sgm[i2][:], op=ALU.mult), reads=[gk_, sk_], writes=[gk_])
                    S.op("dve", lambda e, i2=i2, fc=fc: e.tensor_tensor(out=actT[:, fc, :], in0=gt[i2][:], in1=ut[i2][:], op=ALU.mult),
                         reads=[gk_, uk_], writes=[("actT", fc)])
            akeys = [("actT", fc) for fc in range(16)]
            for dq in range(4):
                wd, wdk = load_w2(w_down[e_, :, dq * 512:(dq + 1) * 512])
                for blk in range(4):
                    i2 = it[0] % 2
                    it[0] += 1
                    pd_, pdk = PDN[i2]
                    for fc in range(16):
                        S.op("pe", lambda e, fc=fc, blk=blk, wd=wd, pd_=pd_: e.matmul(pd_[:, :], lhsT=actT[:, fc, blk * 128:(blk + 1) * 128], rhs=wd[:, fc, :],
                                                                                    start=(fc == 0), stop=(fc == 15)), reads=[wdk] + akeys, writes=[pdk])
                    S.op("dve", lambda e, blk=blk, dq=dq, pd_=pd_: e.tensor_tensor(out=Yt[:, blk, dq * 512:(dq + 1) * 512], in0=pd_[:, :],
                                                                                 in1=bd_bc[:, dq * 512:(dq + 1) * 512], op=ALU.add),
                         reads=[pdk, "bd_bc"], writes=[("Yt", blk)])
                    S.op("dve", lambda e, blk=blk, dq=dq: e.tensor_scalar(out=Yt[:, blk, dq * 512:(dq + 1) * 512], in0=Yt[:, blk, dq * 512:(dq + 1) * 512],
                                                                         scalar1=lst[:, blk, 1:2].bitcast(F32), scalar2=None, op0=ALU.mult),
                         reads=[("Yt", blk), "lst"], writes=[("Yt", blk)])
            for blk in range(4):
                S.dma("pool", yacc, Yt[:, blk, :], chan="Ysc", reads=[("Yt", blk), "lst"], writes=["yacc"],
                      indirect=dict(out_offset=bass.IndirectOffsetOnAxis(ap=lst[:, blk, 0:1], axis=0), in_offset=None, compute_op=ALU.add))
            fin_ = ("dma", "Ysc", S.dcnt["Ysc"])
            for blk in range(4):
                S.readers[("Yt", blk)] = [fin_]
            S.readers["lst"] = [fin_]
            S.lastw["yacc"] = fin_

        def bump_skipped(snap_cnt, snap_d):
            for en_ in S.eng:
                dn = S.cnt[en_] - snap_cnt[en_]
                if dn > 0:
                    if snap_cnt[en_] > 0:
                        S.eng[en_].wait_ge(S.sem[en_], snap_cnt[en_])
                    S.eng[en_].sem_inc(S.sem[en_], dn)
            ci_ = 0
            for k_ in S.dcnt:
                dd = S.dcnt[k_] - snap_d.get(k_, 0)
                if dd > 0:
                    if S.dq[k_] == "pool":
                        if snap_d.get(k_, 0) > 0:
                            nc.gpsimd.wait_ge(S.dsem[k_], snap_d[k_])
                        ci_ = dum_idx.setdefault(k_, len(dum_idx))
                        j_ = 0
                        while dd > 0:
                            d1 = min(dd, 96)
                            nc.gpsimd.dma_start(out=dum[0:1, ci_ * 4 + j_:ci_ * 4 + j_ + 1], in_=flag[0:1, 0:1]).then_inc(S.dsem[k_], d1)
                            dd -= d1
                            j_ += 1
                    else:
                        if snap_d.get(k_, 0) > 0:
                            nc.sync.wait_ge(S.dsem[k_], snap_d[k_])
                        nc.sync.sem_inc(S.dsem[k_], dd)

        def guarded(e_, g_):
            nc.regs_load(flag_regs, flg_i[0:1, g_ * NE + e_: g_ * NE + e_ + 1])
            snap_cnt = dict(S.cnt)
            snap_d = dict(S.dcnt)
            snap_seen = {k_: dict(v_) for k_, v_ in S.seen.items()}
            with nc.If_cmp(flag_regs, 0, "IS_NE"):
                pass_body(e_, g_)
                if g_ + 1 < n_groups:
                    guarded(e_, g_ + 1)
            with nc.Else():
                bump_skipped(snap_cnt, snap_d)
            S.seen = snap_seen

        for e_ in range(NE):
            S.dma("sp", bd_bc[:], b_down[e_, :].partition_broadcast(128), chan="bd_bc", writes=["bd_bc"])
            S.dma("sp", bg_sb[:], b_gate[e_], chan="bg_sb", writes=["bg_sb"])
            S.dma("sp", bu_sb[:], b_up[e_], chan="bu_sb", writes=["bu_sb"])
            guarded(e_, 0)
        S.barrier()
        ph.close()

        ph = ExitStack()
        cur[0] = ph
        g2_bc = bc_tile("g2_bc", moddram[0, 5 * D:6 * D])
        l2g_bc = bc_tile("l2g_bc", ln2_g[0, :])
        l2b_bc = bc_tile("l2b_bc", ln2_b[0, :])
        YA = [sb("ya%d" % i, [128, D]) for i in range(2)]
        X1 = [sb("x1_%d" % i, [128, D]) for i in range(2)]
        OT = [sb("ot%d" % i, [128, D]) for i in range(2)]
        stats3 = sb("stats3", [128, 4, 6])
        mv3 = sb("mv3", [128, 2])
        rstd3 = sb("rstd3", [128, 1])
        for ti in range(16):
            b2 = ti % 2
            yk, xk, ok = "ya%d" % b2, "x1_%d" % b2, "ot%d" % b2
            S.dma("sp", YA[b2][:], yacc[ti * 128:(ti + 1) * 128, :], chan=yk, reads=["yacc"], writes=[yk])
            S.dma("sp", X1[b2][:], x1buf[ti * 128:(ti + 1) * 128, :], chan=xk, reads=[("x1buf", ti)], writes=[xk])
            S.op("pool", lambda e, b2=b2: e.tensor_tensor(out=YA[b2][:], in0=YA[b2][:], in1=g2_bc[:], op=ALU.mult), reads=[yk, "g2_bc"], writes=[yk])
            S.op("dve", lambda e, b2=b2: e.scalar_tensor_tensor(out=YA[b2][:], in0=X1[b2][:], scalar=ALPHA, in1=YA[b2][:], op0=ALU.mult, op1=ALU.add),
                 reads=[xk, yk], writes=[yk])
            ln_stats(YA[b2], yk, stats3, mv3, rstd3, "c")
            S.op("dve", lambda e, b2=b2: e.tensor_scalar(out=OT[b2][:], in0=YA[b2][:], scalar1=mv3[:, 0:1], scalar2=rstd3[:, 0:1], op0=ALU.subtract, op1=ALU.mult),
                 reads=[yk, "cmv", "crs"], writes=[ok])
            S.op("pool", lambda e, b2=b2: e.tensor_tensor(out=OT[b2][:], in0=OT[b2][:], in1=l2g_bc[:], op=ALU.mult), reads=[ok, "l2g_bc"], writes=[ok])
            S.op("pool", lambda e, b2=b2: e.tensor_tensor(out=OT[b2][:], in0=OT[b2][:], in1=l2b_bc[:], op=ALU.add), reads=[ok, "l2b_bc"], writes=[ok])
            S.dma("sp", out[ti * 128:(ti + 1) * 128, :], OT[b2][:], chan=ok, reads=[ok], writes=["out"])
        S.barrier()
        ph.close()
        print("sems", len(S.dsem), "inst", S.n_inst, "waits", S.n_wait)
    return nc


def host_consts():
    ident = np.eye(128, dtype=np.float32)
    triu2 = np.zeros((128, 128), np.float32)
    for b in range(2):
        triu2[b * 64:(b + 1) * 64, b * 64:(b + 1) * 64] = np.triu(np.ones((64, 64), np.float32))
    rmask = np.ones((128, SEG), np.float32)
    rmask[:, ::64] = 0.0
    ltri = np.triu(np.ones((128, 128), np.float32), 1)
    iota_e = np.tile((np.arange(NE, dtype=np.float32) * CAP)[None, :], (128, 1))
    tokid = (np.arange(16, dtype=np.int32)[None, :] * 128 + np.arange(128, dtype=np.int32)[:, None]).astype(np.int32)
    iota_n = np.tile(np.arange(NE, dtype=np.float32)[None, :], (128, 1))
    list_init = np.zeros((NE * CAP, 2), np.int32)
    list_init[:, 0] = T + (np.arange(NE * CAP) % 128)
    return dict(ident_f=ident, triu2=triu2, rmask=rmask, ltri=ltri, iota_e=iota_e, tokid=tokid, iota_n=iota_n, list_init=list_init)


def make_in_maps(inp):
    f = lambda a: np.ascontiguousarray(a, dtype=np.float32)
    x = inp["x"]
    consts = host_consts()
    shared = dict(
        w_ada=f(inp["w_ada"][0]), b_ada=f(inp["b_ada"][0][None, :]), w_in=f(inp["w_in"][0]), w_gk=f(inp["w_gk"][0]),
        b_gk=f(inp["b_gk"][0].reshape(4, 128).T), w_pool=f(inp["w_pool"][0]),
        b_pool=f(inp["b_pool"][0].reshape(8, 128).T), pool_scale=f(inp["pool_scale"][0].reshape(8, 128).T),
        gla_norm_w=f(inp["gla_norm_w"][0][None, :]), w_out=f(inp["w_out"][0]),
        ln1_g=f(inp["ln1_g"][0][None, :]), ln1_b=f(inp["ln1_b"][0][None, :]),
        w_router=f(inp["w_router"][0]), b_router=f(inp["b_router"][0][None, :]),
        w_gate=f(inp["w_gate"][0]), b_gate=f(inp["b_gate"][0].reshape(NE, 16, 128).transpose(0, 2, 1)),
        w_up=f(inp["w_up"][0]), b_up=f(inp["b_up"][0].reshape(NE, 16, 128).transpose(0, 2, 1)),
        w_down=f(inp["w_down"][0]), b_down=f(inp["b_down"][0]),
        ln2_g=f(inp["ln2_g"][0][None, :]), ln2_b=f(inp["ln2_b"][0][None, :]),
    )
    shared.update(consts)
    maps = []
    for core in range(8):
        b, half = core // 2, core % 2
        m = dict(shared)
        m["x_own"] = f(x[b, half * T:(half + 1) * T, :])
        m["x_pre"] = f(x[b, 0:T, :])
        m["c_l"] = f(inp["c"][b].reshape(16, 128).T)
        m["flag"] = np.full((128, 1), float(half), np.float32)
        ic = np.zeros((128, 4, 16), np.float32)
        for g in range(4):
            w = 2 ** (g + 1)
            pos = np.arange(1, 17, dtype=np.float32)
            ic[:, g, :] = (1.0 / np.minimum(pos, w) if half == 0 else np.full(16, 1.0 / w, np.float32))[None, :]
        m["invcnt"] = ic
        maps.append(m)
    return maps


N_GROUPS = 4


def kernel(**inputs):
    nc = build_program(n_groups=N_GROUPS)
    in_maps = make_in_maps(inputs)
    res = run_bass_kernel_spmd(nc, in_maps, core_ids=list(range(8)))
    outs = [res.results[i]["out"] for i in range(8)]
    full = np.stack([np.concatenate([outs[2 * b], outs[2 * b + 1]], axis=0) for b in range(4)], axis=0)
    return full.astype(np.float32)
```
